# Optimizing a Trainium2 kernel written in Bass

```python
import math
import jax
import jax.numpy as jnp
from jax import lax
import numpy as np

D_MODEL = 1024
BATCH = 4
SEQ = 8192
DEPTH = 4

GRID_W = 64
CTX_LEN = 256

GLA_HEADS = 4
GLA_DK = 64
GLA_DV = 128
GLA_RANK = 16
GLA_TAU = 16.0
GLA_CHUNK = 64
GQA_HEADS = 8
GQA_KV_HEADS = 2
GQA_HD = 64
Q_BLOCK = 128
ROPE_THETA = 10000.0
SSD_HEADS = 12
SSD_HD = 64
SSD_GROUPS = 2
SSD_HPG = SSD_HEADS // SSD_GROUPS
SSD_STATE = 128
SSD_CONV = 5
SSD_CHUNK = 64
FNET_GROUPS = 4
FNET_GC = 64
N_EXPERTS = 16
N_EXPERT_GROUPS = 4
EXPERTS_PER_GROUP = N_EXPERTS // N_EXPERT_GROUPS
TOP_K = 2
D_EXPERT = 768
MOE_BLOCK = 256

GLA_QK = GLA_HEADS * GLA_DK
GLA_V = GLA_HEADS * GLA_DV
GQA_Q = GQA_HEADS * GQA_HD
GQA_KV = GQA_KV_HEADS * GQA_HD
EVEN_SPLITS = (GLA_QK, GLA_QK, GLA_V, GLA_V, GLA_RANK, GLA_RANK, GQA_Q, GQA_KV, GQA_KV)
EVEN_IN = sum(EVEN_SPLITS)
EVEN_OUT = GLA_V + GQA_Q
SSD_INNER = SSD_HEADS * SSD_HD
SSD_BC = SSD_GROUPS * SSD_STATE
SSD_CONV_CH = SSD_INNER + 2 * SSD_BC
FNET_W = FNET_GROUPS * FNET_GC
ODD_SPLITS = (SSD_INNER, SSD_INNER, SSD_BC, SSD_BC, SSD_HEADS, SSD_HEADS, FNET_W)
ODD_IN = sum(ODD_SPLITS)
ODD_OUT = SSD_INNER + FNET_W

kernel_name = 'hybrid_gla_gqa_ssd_fnet_moe_dit'

F32 = jnp.float32


def layer_norm(x, g, b, eps=1e-6):
    xf = x.astype(F32)
    mu = jnp.mean(xf, -1, keepdims=True)
    var = jnp.mean(jnp.square(xf - mu), -1, keepdims=True)
    return ((xf - mu) * lax.rsqrt(var + eps)).astype(x.dtype) * g + b


def rms_norm(x, g, eps=1e-6):
    xf = x.astype(F32)
    return (xf * lax.rsqrt(jnp.mean(xf * xf, -1, keepdims=True) + eps)).astype(x.dtype) * g


def modulate(x, shift, scale):
    return x * (1 + scale) + shift


def split_cols(t, sizes):
    return jnp.split(t, [int(s) for s in np.cumsum(sizes)[:-1]], axis=-1)


def to_heads(t, n_heads):
    b, s, _ = t.shape
    return t.reshape(b, s, n_heads, -1).transpose(0, 2, 1, 3)


def from_heads(t):
    b, h, s, d = t.shape
    return t.transpose(0, 2, 1, 3).reshape(b, s, h * d)


def axial_rope_tables(rows, head_dim):
    r = jnp.repeat(jnp.arange(rows, dtype=F32), GRID_W)
    col = jnp.tile(jnp.arange(GRID_W, dtype=F32), rows)
    half = head_dim // 2
    inv = ROPE_THETA ** (-jnp.arange(0, half, 2, dtype=F32) / half)
    ar = r[:, None] * inv
    ac = col[:, None] * inv
    ang = jnp.concatenate([ar, ar, ac, ac], -1)
    return jnp.cos(ang), jnp.sin(ang)


def apply_axial_rope(x, cos, sin):
    x1, x2, x3, x4 = jnp.split(x, 4, axis=-1)
    rot = jnp.concatenate([-x2, x1, -x4, x3], -1)
    return (x * cos + rot * sin).astype(x.dtype)


def gla_scan(q, k, v, log_a, s0):
    bsz, h, t, dk = q.shape
    dv = v.shape[-1]
    L = GLA_CHUNK
    n = t // L
    qf = q.astype(F32).reshape(bsz, h, n, L, dk)
    kf = k.astype(F32).reshape(bsz, h, n, L, dk)
    vf = v.astype(F32).reshape(bsz, h, n, L, dv)
    b = jnp.cumsum(log_a.astype(F32).reshape(bsz, h, n, L, dk), axis=3)
    b_mid = b[:, :, :, L // 2 - 1:L // 2]
    b_end = b[:, :, :, -1:]
    mask = jnp.tril(jnp.ones((L, L), bool))
    att = jnp.einsum('bhnld,bhnmd->bhnlm', qf * jnp.exp(b - b_mid), kf * jnp.exp(b_mid - b))
    o_intra = jnp.einsum('bhnlm,bhnmv->bhnlv', jnp.where(mask, att, 0.0), vf)
    ds = jnp.einsum('bhnld,bhnlv->bhndv', kf * jnp.exp(b_end - b), vf)
    decay = jnp.exp(b_end[:, :, :, 0])

    def step(s, inp):
        d, dsn = inp
        return d[..., None] * s + dsn, s

    s_fin, s_starts = lax.scan(step, s0.astype(F32), (jnp.moveaxis(decay, 2, 0), jnp.moveaxis(ds, 2, 0)))
    s_starts = jnp.moveaxis(s_starts, 0, 2)
    o_inter = jnp.einsum('bhnld,bhndv->bhnlv', qf * jnp.exp(b), s_starts)
    return (o_intra + o_inter).reshape(bsz, h, t, dv).astype(v.dtype), s_fin


def ssd_scan(x, a, bm, cm, h0):
    bsz, t, g, hg, p = x.shape
    ns = bm.shape[-1]
    L = SSD_CHUNK
    n = t // L
    x = x.astype(F32).reshape(bsz, n, L, g, hg, p)
    a = a.astype(F32).reshape(bsz, n, L, g, hg)
    bm = bm.astype(F32).reshape(bsz, n, L, g, ns)
    cm = cm.astype(F32).reshape(bsz, n, L, g, ns)
    cum = jnp.cumsum(a, axis=2)
    seg = cum[:, :, :, None] - cum[:, :, None, :]
    mask = jnp.tril(jnp.ones((L, L), bool))[None, None, :, :, None, None]
    decay_ts = jnp.exp(jnp.where(mask, seg, -jnp.inf))
    cb = jnp.einsum('bnlgk,bnmgk->bnlmg', cm, bm)
    y_intra = jnp.einsum('bnlmgh,bnmghp->bnlghp', cb[..., None] * decay_ts, x)
    x_end = x * jnp.exp(cum[:, :, -1:] - cum)[..., None]
    dh = jnp.einsum('bnlgk,bnlghp->bnghpk', bm, x_end)
    chunk_decay = jnp.exp(cum[:, :, -1])

    def step(hs, inp):
        d, dhn = inp
        return d[..., None, None] * hs + dhn, hs

    h_fin, h_starts = lax.scan(step, h0.astype(F32), (jnp.moveaxis(chunk_decay, 1, 0), jnp.moveaxis(dh, 1, 0)))
    h_starts = jnp.moveaxis(h_starts, 0, 1)
    y_inter = jnp.einsum('bnlgk,bnghpk->bnlghp', cm, h_starts) * jnp.exp(cum)[..., None]
    return (y_intra + y_inter).reshape(bsz, t, g, hg, p), h_fin


def attend(q, k, v):
    s = jnp.einsum('bgrqd,bgkd->bgrqk', q, k).astype(F32) * (q.shape[-1] ** -0.5)
    p = jax.nn.softmax(s, axis=-1).astype(v.dtype)
    return jnp.einsum('bgrqk,bgkd->bgrqd', p, v)


def gqa_attention(q, k, v, block):
    b, hq, t, hd = q.shape
    hkv = k.shape[1]
    nb = t // block
    qb = jnp.moveaxis(q.reshape(b, hkv, hq // hkv, nb, block, hd), 3, 0)
    o = lax.map(lambda qi: attend(qi, k, v), qb)
    return jnp.moveaxis(o, 0, 3).reshape(b, hq, t, hd)


def dwconv_centered(x, w, bias):
    kw, ch = w.shape
    y = lax.conv_general_dilated(x, w[:, None, :].astype(x.dtype), window_strides=(1,),
                                 padding=[((kw - 1) // 2, kw // 2)],
                                 dimension_numbers=('NWC', 'WIO', 'NWC'), feature_group_count=ch)
    return y + bias


def fourier_mix(u, g):
    b, t, _ = u.shape
    un = rms_norm(u.reshape(b, t, FNET_GROUPS, FNET_GC), g.reshape(FNET_GROUPS, FNET_GC))
    f = jnp.fft.fft2(un.astype(F32), axes=(1, 3), norm='ortho').real
    return f.reshape(b, t, FNET_W).astype(u.dtype)


def even_mixer(u_ctx, u_lat, w_in, w_o, w_dec, b_dec, gla_g, qn_g, kn_g, cos, sin, last):
    pc = split_cols(u_ctx @ w_in, EVEN_SPLITS)
    pl = split_cols(u_lat @ w_in, EVEN_SPLITS)

    def gla_inputs(p):
        q, k, v, r, lf, lb = p[:6]
        ld = [to_heads(jax.nn.log_sigmoid((lr @ w_dec[di] + b_dec[di]).astype(F32)) / GLA_TAU, GLA_HEADS)
              for di, lr in enumerate((lf, lb))]
        return (to_heads(q, GLA_HEADS) * (GLA_DK ** -0.5), to_heads(k, GLA_HEADS), to_heads(v, GLA_HEADS),
                ld[0], ld[1], r)

    qc, kc, vc, lfc, lbc, rc = gla_inputs(pc)
    ql, kl, vl, lfl, lbl, rl = gla_inputs(pl)
    fl = lambda t: jnp.flip(t, 2)
    zero = jnp.zeros((qc.shape[0], GLA_HEADS, GLA_DK, GLA_DV), F32)
    oc_f, sc_f = gla_scan(qc, kc, vc, lfc, zero)
    oc_b, sc_b = gla_scan(fl(qc), fl(kc), fl(vc), fl(lbc), zero)
    ol_f, _ = gla_scan(ql, kl, vl, lfl, sc_f)
    ol_b, _ = gla_scan(fl(ql), fl(kl), fl(vl), fl(lbl), sc_b)

    def gla_out(of, ob_rev, r):
        return from_heads(rms_norm(of + fl(ob_rev), gla_g)) * jax.nn.silu(r)

    def gqa_inputs(p, rope):
        q, k, v = p[6:]
        q = rms_norm(to_heads(q, GQA_HEADS), qn_g)
        k = rms_norm(to_heads(k, GQA_KV_HEADS), kn_g)
        if rope:
            q = apply_axial_rope(q, cos, sin)
            k = apply_axial_rope(k, cos, sin)
        return q, k, to_heads(v, GQA_KV_HEADS)

    qgc, kgc, vgc = gqa_inputs(pc, False)
    qgl, kgl, vgl = gqa_inputs(pl, True)
    b_lat = from_heads(gqa_attention(qgl, jnp.concatenate([kgc, kgl], 2), jnp.concatenate([vgc, vgl], 2), Q_BLOCK))
    o_lat = jnp.concatenate([gla_out(ol_f, ol_b, rl), b_lat], -1) @ w_o
    if last:
        return None, o_lat
    b_ctx = from_heads(gqa_attention(qgc, kgc, vgc, qgc.shape[2]))
    o_ctx = jnp.concatenate([gla_out(oc_f, oc_b, rc), b_ctx], -1) @ w_o
    return o_ctx, o_lat


def odd_mixer(u_ctx, u_lat, w_in, w_o, conv_w, conv_b, dt_bias, a_log, d_skip, ssd_g, fnet_g, last):
    def prep(u):
        bsz, t, _ = u.shape
        z, xs, bs, cs, dtf, dtb, f = split_cols(u @ w_in, ODD_SPLITS)
        xbc = jax.nn.silu(dwconv_centered(jnp.concatenate([xs, bs, cs], -1), conv_w, conv_b))
        xs, bs, cs = split_cols(xbc, (SSD_INNER, SSD_BC, SSD_BC))
        xs = xs.reshape(bsz, t, SSD_GROUPS, SSD_HPG, SSD_HD)
        bs = bs.reshape(bsz, t, SSD_GROUPS, SSD_STATE)
        cs = cs.reshape(bsz, t, SSD_GROUPS, SSD_STATE)
        dirs = []
        for di, dt_raw in enumerate((dtf, dtb)):
            dt = jax.nn.softplus(dt_raw.astype(F32) + dt_bias[di]).reshape(bsz, t, SSD_GROUPS, SSD_HPG)
            a = dt * (-jnp.exp(a_log[di].astype(F32))).reshape(SSD_GROUPS, SSD_HPG)
            dirs.append((xs * dt[..., None], a))
        return z, xs, bs, cs, dirs, f

    zc, xc, bc, cc, dc, fc = prep(u_ctx)
    zl, xl, bl, cl, dl, fl_ = prep(u_lat)
    fl = lambda t: jnp.flip(t, 1)
    h0 = jnp.zeros((xc.shape[0], SSD_GROUPS, SSD_HPG, SSD_HD, SSD_STATE), F32)
    yc_f, hc_f = ssd_scan(dc[0][0], dc[0][1], bc, cc, h0)
    yc_b, hc_b = ssd_scan(fl(dc[1][0]), fl(dc[1][1]), fl(bc), fl(cc), h0)
    yl_f, _ = ssd_scan(dl[0][0], dl[0][1], bl, cl, hc_f)
    yl_b, _ = ssd_scan(fl(dl[1][0]), fl(dl[1][1]), fl(bl), fl(cl), hc_b)

    def ssd_out(yf, yb_rev, xs, z):
        bsz, t, _ = z.shape
        y = yf + fl(yb_rev) + xs.astype(F32) * d_skip.astype(F32).reshape(SSD_GROUPS, SSD_HPG, 1)
        y = y.reshape(bsz, t, SSD_INNER).astype(z.dtype) * jax.nn.silu(z)
        return rms_norm(y.reshape(bsz, t, SSD_GROUPS, -1), ssd_g.reshape(SSD_GROUPS, -1)).reshape(bsz, t, SSD_INNER)

    o_lat = jnp.concatenate([ssd_out(yl_f, yl_b, xl, zl), fourier_mix(fl_, fnet_g)], -1) @ w_o
    if last:
        return None, o_lat
    o_ctx = jnp.concatenate([ssd_out(yc_f, yc_b, xc, zc), fourier_mix(fc, fnet_g)], -1) @ w_o
    return o_ctx, o_lat


def route(h, router_w, router_b):
    n = h.shape[0]
    s = jax.nn.sigmoid((h @ router_w).astype(F32))
    sel = (s + router_b).reshape(n, N_EXPERT_GROUPS, EXPERTS_PER_GROUP)
    g_idx = jnp.argmax(lax.top_k(sel, TOP_K)[0].sum(-1), axis=-1)
    in_group = jnp.take_along_axis(sel, g_idx[:, None, None], axis=1)[:, 0]
    _, loc = lax.top_k(in_group, TOP_K)
    idx = (g_idx[:, None] * EXPERTS_PER_GROUP + loc).astype(jnp.int32)
    w = jnp.take_along_axis(s, idx, axis=1)
    return idx, w / jnp.sum(w, -1, keepdims=True)


def moe_ffn(h, idx, w, w_gate, w_up, w_down):
    n, d = h.shape
    e = w_gate.shape[0]
    m = MOE_BLOCK
    na = n * TOP_K
    flat_e = idx.reshape(-1)
    order = jnp.argsort(flat_e)
    e_sorted = flat_e[order]
    counts = jnp.zeros((e,), jnp.int32).at[flat_e].add(1)
    padded = (counts + m - 1) // m * m
    pad_end = jnp.cumsum(padded)
    pad_start = pad_end - padded
    start = jnp.cumsum(counts) - counts
    dest = pad_start[e_sorted] + jnp.arange(na, dtype=jnp.int32) - start[e_sorted]
    n_blocks = -(-na // m) + e
    tok = order // TOP_K
    buf = jnp.zeros((n_blocks * m, d), h.dtype).at[dest].set(h[tok])
    block_e = jnp.minimum(jnp.searchsorted(pad_end, jnp.arange(n_blocks, dtype=jnp.int32) * m, side='right'), e - 1)

    def expert_block(args):
        xb, ei = args
        return (jax.nn.silu(xb @ w_gate[ei]) * (xb @ w_up[ei])) @ w_down[ei]

    yb = lax.map(expert_block, (buf.reshape(n_blocks, m, d), block_e)).reshape(n_blocks * m, d)
    y = yb[dest] * w.reshape(-1)[order][:, None].astype(h.dtype)
    return jnp.zeros_like(h).at[tok].add(y)


def setup_inputs(seed: int = 0) -> dict:
    key = jax.random.key(seed)
    ks = iter(jax.random.split(key, 40))
    nrm = lambda shape, scale: jax.random.normal(next(ks), shape, F32) * scale
    beta = (8.0 * DEPTH) ** -0.25
    ne, no = (DEPTH + 1) // 2, DEPTH // 2
    D = D_MODEL
    dt0 = jnp.exp(jax.random.uniform(next(ks), (no, 2, SSD_HEADS), F32, math.log(1e-3), math.log(1e-1)))
    return {
        'x': nrm((BATCH, SEQ, D), 1.0),
        'c': nrm((BATCH, D), 1.0),
        'ctx': nrm((BATCH, CTX_LEN, D), 1.0),
        'c_ctx': nrm((D,), 1.0),
        'ada_w': nrm((DEPTH, D, 6 * D), 0.5 * D ** -0.5),
        'ada_b': nrm((DEPTH, 6 * D), 0.02),
        'ln_g': 1.0 + nrm((DEPTH, 2, D), 0.02),
        'ln_b': nrm((DEPTH, 2, D), 0.02),
        'ev_w_in': nrm((ne, D, EVEN_IN), D ** -0.5),
        'ev_w_o': nrm((ne, EVEN_OUT, D), beta * EVEN_OUT ** -0.5),
        'gla_w_decay': nrm((ne, 2, GLA_RANK, GLA_QK), GLA_RANK ** -0.5),
        'gla_b_decay': 1.0 + nrm((ne, 2, GLA_QK), 0.1),
        'gla_norm_g': 1.0 + nrm((ne, GLA_DV), 0.02),
        'gqa_q_norm_g': 1.0 + nrm((ne, GQA_HD), 0.02),
        'gqa_k_norm_g': 1.0 + nrm((ne, GQA_HD), 0.02),
        'od_w_in': nrm((no, D, ODD_IN), D ** -0.5),
        'od_w_o': nrm((no, ODD_OUT, D), beta * ODD_OUT ** -0.5),
        'ssd_conv_w': nrm((no, SSD_CONV, SSD_CONV_CH), SSD_CONV ** -0.5),
        'ssd_conv_b': nrm((no, SSD_CONV_CH), 0.02),
        'ssd_dt_bias': dt0 + jnp.log(-jnp.expm1(-dt0)),
        'ssd_a_log': jnp.log(jax.random.uniform(next(ks), (no, 2, SSD_HEADS), F32, 1.0, 16.0)),
        'ssd_d': 1.0 + nrm((no, SSD_HEADS), 0.02),
        'ssd_norm_g': 1.0 + nrm((no, SSD_INNER), 0.02),
        'fnet_norm_g': 1.0 + nrm((no, FNET_W), 0.02),
        'router_w': nrm((D, N_EXPERTS), D ** -0.5),
        'router_b': nrm((N_EXPERTS,), 0.01),
        'exp_w_gate': nrm((DEPTH, N_EXPERTS, D, D_EXPERT), D ** -0.5),
        'exp_w_up': nrm((DEPTH, N_EXPERTS, D, D_EXPERT), D ** -0.5),
        'exp_w_down': nrm((DEPTH, N_EXPERTS, D_EXPERT, D), beta * D_EXPERT ** -0.5),
    }


def reference(x, c, ctx, c_ctx, ada_w, ada_b, ln_g, ln_b, ev_w_in, ev_w_o, gla_w_decay, gla_b_decay,
              gla_norm_g, gqa_q_norm_g, gqa_k_norm_g, od_w_in, od_w_o, ssd_conv_w, ssd_conv_b,
              ssd_dt_bias, ssd_a_log, ssd_d, ssd_norm_g, fnet_norm_g, router_w, router_b,
              exp_w_gate, exp_w_up, exp_w_down):
    alpha = (2.0 * DEPTH) ** 0.25
    bsz, seq, d = x.shape
    rows = seq // GRID_W
    cos, sin = axial_rope_tables(rows, GQA_HD)
    xc = ctx
    for layer in range(DEPTH):
        last = layer == DEPTH - 1
        m_lat = jnp.split((jax.nn.silu(c) @ ada_w[layer] + ada_b[layer])[:, None, :], 6, axis=-1)
        m_ctx = jnp.split(jax.nn.silu(c_ctx) @ ada_w[layer] + ada_b[layer], 6, axis=-1)
        u_lat = modulate(x, m_lat[0], m_lat[1])
        u_ctx = modulate(xc, m_ctx[0], m_ctx[1])
        i = layer // 2
        if layer % 2 == 0:
            o_ctx, o_lat = even_mixer(u_ctx, u_lat, ev_w_in[i], ev_w_o[i], gla_w_decay[i], gla_b_decay[i],
                                      gla_norm_g[i], gqa_q_norm_g[i], gqa_k_norm_g[i], cos, sin, last)
        else:
            o_ctx, o_lat = odd_mixer(u_ctx, u_lat, od_w_in[i], od_w_o[i], ssd_conv_w[i], ssd_conv_b[i],
                                     ssd_dt_bias[i], ssd_a_log[i], ssd_d[i], ssd_norm_g[i], fnet_norm_g[i], last)
        x = layer_norm(alpha * x + m_lat[2] * o_lat, ln_g[layer, 0], ln_b[layer, 0])
        v_lat = modulate(x, m_lat[3], m_lat[4]).reshape(-1, d)
        if last:
            tokens = v_lat
            n_ctx = 0
        else:
            xc = layer_norm(alpha * xc + m_ctx[2] * o_ctx, ln_g[layer, 0], ln_b[layer, 0])
            tokens = jnp.concatenate([modulate(xc, m_ctx[3], m_ctx[4]).reshape(-1, d), v_lat], 0)
            n_ctx = xc.shape[0] * xc.shape[1]
        idx, gates = route(tokens, router_w, router_b)
        y = moe_ffn(tokens, idx, gates, exp_w_gate[layer], exp_w_up[layer], exp_w_down[layer])
        x = layer_norm(alpha * x + m_lat[5] * y[n_ctx:].reshape(bsz, seq, d), ln_g[layer, 1], ln_b[layer, 1])
        if not last:
            xc = layer_norm(alpha * xc + m_ctx[5] * y[:n_ctx].reshape(xc.shape), ln_g[layer, 1], ln_b[layer, 1])
    return x
```

```python
import numpy as np
from contextlib import ExitStack
import concourse.bass as bass
import concourse.mybir as mybir
from concourse.bass_utils import run_bass_kernel_spmd

F32 = mybir.dt.float32
BF16 = mybir.dt.bfloat16
I32 = mybir.dt.int32
AF = mybir.ActivationFunctionType
ALU = mybir.AluOpType
AX = mybir.AxisListType


class Buf:
    __slots__ = ("t", "name", "lw", "rd")

    def __init__(self, t, name):
        self.t = t
        self.name = name
        self.lw = None
        self.rd = {}


class KB:
    ENG = ("pe", "dve", "act", "pool", "sp")

    def __init__(self, nc, st, ndma=12):
        self.nc = nc
        self.st = st
        self.st0 = st
        self.pfx = ""
        self.e = {"pe": nc.tensor, "dve": nc.vector, "act": nc.scalar, "pool": nc.gpsimd, "sp": nc.sync}
        self.sem = {}
        self.cnt = {}
        for k in ("pe", "dve", "act", "pool"):
            self.sem[k] = st.enter_context(nc.semaphore("s_" + k))
            self.cnt[k] = 0
        self.dsem = [st.enter_context(nc.semaphore("d%d" % i)) for i in range(ndma)]
        self.dcnt = [0] * ndma
        self.drr = 0
        self.waited = {k: {} for k in self.ENG}
        self.out_tickets = []
        self.n_inst = 0

    def sb(self, name, shape, dt):
        return Buf(self.st.enter_context(self.nc.sbuf_tensor("sb_" + self.pfx + name, list(shape), dt)), name)

    def ps(self, name, shape, dt=F32):
        return Buf(self.st.enter_context(self.nc.psum_tensor("ps_" + self.pfx + name, list(shape), dt)), name)

    def _semobj(self, key):
        return self.sem[key] if isinstance(key, str) else self.dsem[key]

    def _wait(self, eng, key, val):
        if key == eng and eng == "pe":
            return
        w = self.waited[eng]
        if w.get(key, 0) >= val:
            return
        self.e[eng].wait_ge(self._semobj(key), val)
        w[key] = val

    def _deps(self, eng, reads, writes):
        deps = {}
        for b in reads:
            if b.lw is not None and deps.get(b.lw[0], 0) < b.lw[1]:
                deps[b.lw[0]] = b.lw[1]
        for b in writes:
            if b.lw is not None and deps.get(b.lw[0], 0) < b.lw[1]:
                deps[b.lw[0]] = b.lw[1]
            for k, v in b.rd.items():
                if deps.get(k, 0) < v:
                    deps[k] = v
        for k, v in deps.items():
            self._wait(eng, k, v)

    def _mark(self, ticket, reads, writes):
        k, v = ticket
        for b in writes:
            b.lw = ticket
            b.rd = {}
        for b in reads:
            if b.rd.get(k, 0) < v:
                b.rd[k] = v

    def op(self, eng, fn, reads=(), writes=(), sig=True):
        self._deps(eng, reads, writes)
        inst = fn(self.e[eng])
        self.n_inst += 1
        if sig:
            self.cnt[eng] += 1
            inst.then_inc(self.sem[eng], 1)
            ticket = (eng, self.cnt[eng])
        else:
            ticket = (eng, self.cnt[eng] + 1)
        self._mark(ticket, reads, writes)
        return ticket

    def dma(self, q, out, in_, reads=(), writes=(), is_out=False):
        i = self.drr
        self.drr = (self.drr + 1) % len(self.dsem)
        if self.dcnt[i] > 0:
            self._wait(q, i, self.dcnt[i])
        self._deps(q, reads, writes)
        self.dcnt[i] += 16
        self.e[q].dma_start(out=out, in_=in_).then_inc(self.dsem[i], 16)
        self.n_inst += 1
        ticket = (i, self.dcnt[i])
        self._mark(ticket, reads, writes)
        if is_out:
            self.out_tickets.append(ticket)
        return ticket

    def collective(self, kind, ins_ap, outs_ap, groups, reads=(), writes=()):
        if "cc" not in self.sem:
            self.sem["cc"] = self.st0.enter_context(self.nc.semaphore("s_cc"))
            self.cnt["cc"] = 0
        self._deps("pool", reads, writes)
        self.cnt["cc"] += 1
        self.nc.gpsimd.collective_compute(kind, ALU.bypass, replica_groups=groups, ins=[ins_ap], outs=[outs_ap]).then_inc(self.sem["cc"], 1)
        self.n_inst += 1
        ticket = ("cc", self.cnt["cc"])
        self._mark(ticket, reads, writes)
        return ticket

    def barrier(self):
        for eng in self.ENG:
            for i, c in enumerate(self.dcnt):
                if c > 0:
                    self._wait(eng, i, c)
            for k in self.cnt:
                if self.cnt[k] > 0 and k != eng:
                    self._wait(eng, k, self.cnt[k])

    def sb_in(self, st, name, shape, dt):
        return Buf(st.enter_context(self.nc.sbuf_tensor("sb_" + self.pfx + name, list(shape), dt)), name)

    def finish(self):
        for i, c in enumerate(self.dcnt):
            if c > 0:
                self._wait("sp", i, c)
        for k in self.cnt:
            if self.cnt[k] > 0:
                self._wait("sp", k, self.cnt[k])


ALPHA = (2.0 * 4) ** 0.25
LN_EPS = 1e-6
TB = 384
TC = 4224


def _din(nc, name, shape, dt=F32):
    return nc.dram_tensor(name, list(shape), dt, kind="ExternalInput").ap()


def _dout(nc, name, shape, dt=F32):
    return nc.dram_tensor(name, list(shape), dt, kind="ExternalOutput").ap()


def _segs(bi):
    return [(0, 128, 0), (128, TB, 1)] if bi == 0 else [(0, TB, 1)]


class LNCtx:
    def __init__(self, kb, pS1, pS2):
        self.kb = kb
        self.pS1, self.pS2 = pS1, pS2
        self.rb = kb.sb("ln_rb", [128, 8, TB], BF16)
        self.rsq = kb.sb("ln_rsq", [128, 8, TB], BF16)
        self.mean = kb.sb("ln_mean", [128, TB], F32)
        self.m2 = kb.sb("ln_m2", [128, TB], F32)
        self.rstd = kb.sb("ln_rstd", [128, TB], F32)
        self.nmr = kb.sb("ln_nmr", [128, TB], F32)
        self.ones = kb.sb("ln_ones", [128, 128], BF16)
        self.eps = kb.sb("ln_eps", [128, 1], F32)
        kb.op("pool", lambda e: e.memset(self.ones.t[:], 1.0 / 1024.0), writes=[self.ones])
        kb.op("pool", lambda e: e.memset(self.eps.t[:], LN_EPS / (ALPHA * ALPHA)), writes=[self.eps])

    def normalize(self, r):
        kb = self.kb
        for c in range(8):
            kb.op("act", lambda e, c=c: e.activation(out=self.rb.t[:, c, :], in_=r.t[:, c, :], func=AF.Copy),
                  reads=[r], writes=[self.rb])
            kb.op("act", lambda e, c=c: e.activation(out=self.rsq.t[:, c, :], in_=r.t[:, c, :], func=AF.Square),
                  reads=[r], writes=[self.rsq])
        for c in range(8):
            kb.op("pe", lambda e, c=c: e.matmul(self.pS1.t[:, 0:TB], self.ones.t[:], self.rb.t[:, c, :],
                                                start=(c == 0), stop=(c == 7)),
                  reads=[self.ones, self.rb], writes=[self.pS1], sig=(c == 7))
        for c in range(8):
            kb.op("pe", lambda e, c=c: e.matmul(self.pS2.t[:, 0:TB], self.ones.t[:], self.rsq.t[:, c, :],
                                                start=(c == 0), stop=(c == 7)),
                  reads=[self.ones, self.rsq], writes=[self.pS2], sig=(c == 7))
        kb.op("act", lambda e: e.activation(out=self.mean.t[:], in_=self.pS1.t[:, 0:TB], func=AF.Copy),
              reads=[self.pS1], writes=[self.mean])
        kb.op("dve", lambda e: e.tensor_tensor(out=self.m2.t[:], in0=self.mean.t[:], in1=self.mean.t[:], op=ALU.mult),
              reads=[self.mean], writes=[self.m2])
        kb.op("dve", lambda e: e.tensor_tensor(out=self.m2.t[:], in0=self.pS2.t[:, 0:TB], in1=self.m2.t[:],
                                               op=ALU.subtract),
              reads=[self.pS2, self.m2], writes=[self.m2])
        kb.op("act", lambda e: e.activation(out=self.m2.t[:], in_=self.m2.t[:], func=AF.Sqrt, bias=self.eps.t[:, 0:1]),
              reads=[self.m2, self.eps], writes=[self.m2])
        kb.op("dve", lambda e: e.reciprocal(out=self.rstd.t[:], in_=self.m2.t[:]), reads=[self.m2], writes=[self.rstd])
        kb.op("dve", lambda e: e.scalar_tensor_tensor(out=self.nmr.t[:], in0=self.mean.t[:], scalar=-1.0,
                                                      in1=self.rstd.t[:], op0=ALU.mult, op1=ALU.mult),
              reads=[self.mean, self.rstd], writes=[self.nmr])
        for c in range(8):
            kb.op("dve", lambda e, c=c: e.tensor_tensor(out=r.t[:, c, :], in0=r.t[:, c, :], in1=self.rstd.t[:],
                                                        op=ALU.mult), reads=[r, self.rstd], writes=[r])
            kb.op("dve", lambda e, c=c: e.tensor_tensor(out=r.t[:, c, :], in0=r.t[:, c, :], in1=self.nmr.t[:],
                                                        op=ALU.add), reads=[r, self.nmr], writes=[r])


def emit_stageC(kb, nc, P, D, nblk=11, sbs=(3, 3, 3, 2), nexp=16, mods_buf=None, load_cat=None, xo_tok=None):
    catT, xT, xo, w_o, mods, lnp, rw, rbias, sel_in, ident_in, wg, wu, wd, xmid_d = (
        D.get(k) for k in ("catT", "xT", "xo", "w_o", "mods", "lnp", "rw", "rbias", "sel", "ident", "wg", "wu", "wd", "xmid"))
    if xo_tok is None:
        xo_tok = Buf(None, "xo_tok")
    with ExitStack() as st:
        old_st = kb.st
        kb.st = st
        ln = LNCtx(kb, P[2], P[3])
        SBT = max(sbs) * TB
        wo_sb = kb.sb("wo", [128, 8, 1024], BF16)
        mods_sb = mods_buf if mods_buf is not None else kb.sb("mods", [128, 2, 6, 8], F32)
        lnp_sb = kb.sb("lnp", [128, 2, 2, 8], F32)
        rw_sb = kb.sb("rw", [128, 8, 16], F32)
        rb_sb = kb.sb("rbias", [128, 16], F32)
        sel_sb = kb.sb("sel", [16, 16 * 128], F32)
        id_sb = kb.sb("ident", [128, 128], F32)
        ga = kb.sb("ga", [128, 2, 2, 8], F32)
        gp = kb.sb("gp", [128, 2, 8], F32)
        bp = kb.sb("bp", [128, 2, 8], F32)
        kb.dma("pool", wo_sb.t[:], w_o, writes=[wo_sb])
        if mods_buf is None:
            kb.dma("sp", mods_sb.t[:], mods, writes=[mods_sb])
        kb.dma("sp", lnp_sb.t[:], lnp, writes=[lnp_sb])
        kb.dma("sp", rw_sb.t[:], rw, writes=[rw_sb])
        kb.dma("sp", rb_sb.t[:], rbias, writes=[rb_sb])
        kb.dma("sp", sel_sb.t[:], sel_in, writes=[sel_sb])
        kb.dma("sp", id_sb.t[:], ident_in, writes=[id_sb])
        for w in range(2):
            kb.op("dve", lambda e, w=w: e.tensor_scalar(out=ga.t[:, 0, w, :], in0=mods_sb.t[:, w, 2, :], scalar1=1.0 / ALPHA,
                                                        scalar2=None, op0=ALU.mult), reads=[mods_sb], writes=[ga])
            kb.op("dve", lambda e, w=w: e.tensor_scalar(out=ga.t[:, 1, w, :], in0=mods_sb.t[:, w, 5, :], scalar1=1.0 / ALPHA,
                                                        scalar2=None, op0=ALU.mult), reads=[mods_sb], writes=[ga])
            kb.op("dve", lambda e, w=w: e.scalar_tensor_tensor(out=gp.t[:, w, :], in0=mods_sb.t[:, w, 4, :], scalar=1.0,
                                                               in1=lnp_sb.t[:, 0, 0, :], op0=ALU.add, op1=ALU.mult),
                  reads=[mods_sb, lnp_sb], writes=[gp])
            kb.op("dve", lambda e, w=w: e.scalar_tensor_tensor(out=bp.t[:, w, :], in0=mods_sb.t[:, w, 4, :], scalar=1.0,
                                                               in1=lnp_sb.t[:, 1, 0, :], op0=ALU.add, op1=ALU.mult),
                  reads=[mods_sb, lnp_sb], writes=[bp])
            kb.op("dve", lambda e, w=w: e.tensor_tensor(out=bp.t[:, w, :], in0=bp.t[:, w, :], in1=mods_sb.t[:, w, 3, :],
                                                        op=ALU.add), reads=[bp, mods_sb], writes=[bp])
        xin = kb.sb("xin", [128, 8, TB], F32)
        catb = kb.sb("catb", [128, 8, TB], BF16)
        r = kb.sb("r", [128, 8, TB], F32)
        vf = kb.sb("vf", [128, 8, TB], F32)
        vb = kb.sb("vb", [128, 8, SBT], BF16)
        yacc = kb.sb("yacc", [128, 8, SBT], F32)
        gT = kb.sb("gT", [16, SBT], F32)
        NW = 3
        wbuf = [kb.sb("wbuf%d" % i, [128, 8 * 768], BF16) for i in range(NW)]
        wrr = [0]
        rt = {n: kb.sb("rt_" + n, [128, 16], F32) for n in ("s", "sel", "c1", "c2", "c3", "min", "selm", "mask", "sg", "gates")}
        rgs = kb.sb("rt_gs", [128, 4], F32)
        rgm = kb.sb("rt_gm", [128, 4], F32)
        rmx = kb.sb("rt_mx", [128, 1], F32)
        rden = kb.sb("rt_den", [128, 1], F32)
        gb = [kb.sb("gb%d" % i, [128, TB], F32) for i in range(2)]
        sg_t = [kb.sb("sgt%d" % i, [128, TB], F32) for i in range(2)]
        aj = [kb.sb("aj%d" % i, [128, TB], BF16) for i in range(6)]
        xmid_tok = [Buf(None, "xmid%d" % i) for i in range(nblk)]

        def v4(b):
            return b.t[:].rearrange("p (g e) -> p g e", e=4)

        def route_tile(vcols, gcol0):
            pR = P[4]
            for k in range(8):
                kb.op("pe", lambda e, k=k: e.matmul(pR.t[:, 0:16], vf.t[:, k, vcols], rw_sb.t[:, k, :],
                                                    start=(k == 0), stop=(k == 7)),
                      reads=[vf, rw_sb], writes=[pR], sig=(k == 7))
            kb.op("act", lambda e: e.activation(out=rt["s"].t[:], in_=pR.t[:, 0:16], func=AF.Sigmoid),
                  reads=[pR], writes=[rt["s"]])
            kb.op("dve", lambda e: e.tensor_tensor(out=rt["sel"].t[:], in0=rt["s"].t[:], in1=rb_sb.t[:], op=ALU.add),
                  reads=[rt["s"], rb_sb], writes=[rt["sel"]])
            sel4 = v4(rt["sel"])
            for k, cn in ((1, "c1"), (2, "c2"), (3, "c3")):
                c4 = v4(rt[cn])
                kb.op("dve", lambda e, k=k, c4=c4: e.tensor_tensor(out=c4[:, :, 0:4 - k], in0=sel4[:, :, k:4],
                                                                   in1=sel4[:, :, 0:4 - k], op=ALU.is_gt),
                      reads=[rt["sel"]], writes=[rt[cn]])
                kb.op("dve", lambda e, k=k, c4=c4: e.tensor_tensor(out=c4[:, :, 4 - k:4], in0=sel4[:, :, 0:k],
                                                                   in1=sel4[:, :, 4 - k:4], op=ALU.is_gt),
                      reads=[rt["sel"]], writes=[rt[cn]])
            kb.op("dve", lambda e: e.tensor_tensor(out=rt["c1"].t[:], in0=rt["c1"].t[:], in1=rt["c2"].t[:], op=ALU.add),
                  reads=[rt["c1"], rt["c2"]], writes=[rt["c1"]])
            kb.op("dve", lambda e: e.tensor_tensor(out=rt["c1"].t[:], in0=rt["c1"].t[:], in1=rt["c3"].t[:], op=ALU.add),
                  reads=[rt["c1"], rt["c3"]], writes=[rt["c1"]])
            kb.op("dve", lambda e: e.tensor_single_scalar(out=rt["min"].t[:], in_=rt["c1"].t[:], scalar=1.5, op=ALU.is_lt),
                  reads=[rt["c1"]], writes=[rt["min"]])
            kb.op("dve", lambda e: e.tensor_tensor(out=rt["selm"].t[:], in0=rt["sel"].t[:], in1=rt["min"].t[:], op=ALU.mult),
                  reads=[rt["sel"], rt["min"]], writes=[rt["selm"]])
            kb.op("dve", lambda e: e.tensor_reduce(out=rgs.t[:], in_=v4(rt["selm"]), axis=AX.X, op=ALU.add),
                  reads=[rt["selm"]], writes=[rgs])
            kb.op("dve", lambda e: e.tensor_reduce(out=rmx.t[:], in_=rgs.t[:], axis=AX.X, op=ALU.max),
                  reads=[rgs], writes=[rmx])
            kb.op("dve", lambda e: e.tensor_scalar(out=rgm.t[:], in0=rgs.t[:], scalar1=rmx.t[:, 0:1], scalar2=None,
                                                   op0=ALU.is_ge), reads=[rgs, rmx], writes=[rgm])
            m4 = v4(rt["mask"])
            min4 = v4(rt["min"])
            for g in range(4):
                kb.op("dve", lambda e, g=g: e.tensor_scalar(out=m4[:, g, :], in0=min4[:, g, :], scalar1=rgm.t[:, g:g + 1],
                                                            scalar2=None, op0=ALU.mult),
                      reads=[rt["min"], rgm], writes=[rt["mask"]])
            kb.op("dve", lambda e: e.tensor_tensor(out=rt["sg"].t[:], in0=rt["s"].t[:], in1=rt["mask"].t[:], op=ALU.mult),
                  reads=[rt["s"], rt["mask"]], writes=[rt["sg"]])
            kb.op("dve", lambda e: e.tensor_reduce(out=rden.t[:], in_=rt["sg"].t[:], axis=AX.X, op=ALU.add),
                  reads=[rt["sg"]], writes=[rden])
            kb.op("dve", lambda e: e.reciprocal(out=rden.t[:], in_=rden.t[:]), reads=[rden], writes=[rden])
            kb.op("dve", lambda e: e.tensor_scalar(out=rt["gates"].t[:], in0=rt["sg"].t[:], scalar1=rden.t[:, 0:1],
                                                   scalar2=None, op0=ALU.mult), reads=[rt["sg"], rden], writes=[rt["gates"]])
            kb.op("pe", lambda e: e.transpose(pR.t[0:16, 128:256], rt["gates"].t[:], id_sb.t[:]),
                  reads=[rt["gates"], id_sb], writes=[pR])
            kb.op("act", lambda e: e.activation(out=gT.t[:, gcol0:gcol0 + 128], in_=pR.t[0:16, 128:256], func=AF.Copy),
                  reads=[pR], writes=[gT])

        def phase1(bi, lb):
            cols = slice(bi * TB, (bi + 1) * TB)
            kb.dma("sp", xin.t[:], xT[:, :, cols], writes=[xin])
            if load_cat is None:
                kb.dma("pool", catb.t[:], catT[:, :, cols], writes=[catb])
            else:
                load_cat(bi, catb, r, vf)
            for d in range(8):
                pA = P[d % 2]
                for k in range(8):
                    kb.op("pe", lambda e, k=k, d=d, pA=pA: e.matmul(pA.t[:, 0:TB], wo_sb.t[:, k, d * 128:(d + 1) * 128],
                                                                    catb.t[:, k, :], start=(k == 0), stop=(k == 7)),
                          reads=[wo_sb, catb], writes=[pA], sig=(k == 7))
                for (lo, hi, w) in _segs(bi):
                    kb.op("dve", lambda e, d=d, lo=lo, hi=hi, w=w, pA=pA: e.scalar_tensor_tensor(
                        out=r.t[:, d, lo:hi], in0=pA.t[:, lo:hi], scalar=ga.t[:, 0, w, d:d + 1], in1=xin.t[:, d, lo:hi],
                        op0=ALU.mult, op1=ALU.add), reads=[pA, ga, xin], writes=[r])
            ln.normalize(r)
            for c in range(8):
                for (lo, hi, w) in _segs(bi):
                    kb.op("act", lambda e, c=c, lo=lo, hi=hi, w=w: e.activation(
                        out=vf.t[:, c, lo:hi], in_=r.t[:, c, lo:hi], func=AF.Identity,
                        scale=gp.t[:, w, c:c + 1], bias=bp.t[:, w, c:c + 1]), reads=[r, gp, bp], writes=[vf])
                kb.op("pool", lambda e, c=c: e.tensor_copy(out=vb.t[:, c, lb * TB:(lb + 1) * TB], in_=vf.t[:, c, :]),
                      reads=[vf], writes=[vb])
                kb.op("act", lambda e, c=c: e.activation(out=r.t[:, c, :], in_=r.t[:, c, :], func=AF.Identity,
                                                         scale=lnp_sb.t[:, 0, 0, c:c + 1], bias=lnp_sb.t[:, 1, 0, c:c + 1]),
                      reads=[r, lnp_sb], writes=[r])
            kb.dma("sp", xmid_d[:, :, cols], r.t[:], reads=[r], writes=[xmid_tok[bi]])
            for tt in range(TB // 128):
                route_tile(slice(tt * 128, (tt + 1) * 128), lb * TB + tt * 128)

        def load_w(src, nchunk, ncol):
            wb = wbuf[wrr[0] % NW]
            wrr[0] += 1
            view = wb.t[:, 0:nchunk * ncol].rearrange("p (c n) -> p c n", n=ncol)
            kb.dma("pool", view, src.rearrange("(c p) n -> p c n", p=128), writes=[wb])
            return wb, view

        def moe(nb_):
            for ex in range(nexp):
                wgb, wgv = load_w(wg[ex], 8, 768)
                wub, wuv = load_w(wu[ex], 8, 768)
                wdb, wdv = load_w(wd[ex], 6, 1024)
                for lb in range(nb_):
                    cs = slice(lb * TB, (lb + 1) * TB)
                    g_ = gb[lb % 2]
                    kb.op("pe", lambda e, ex=ex, cs=cs: e.matmul(P[5].t[:, 0:TB], sel_sb.t[:, ex * 128:(ex + 1) * 128],
                                                                 gT.t[:, cs], start=True, stop=True),
                          reads=[sel_sb, gT], writes=[P[5]])
                    kb.op("act", lambda e, g_=g_: e.activation(out=g_.t[:], in_=P[5].t[:, 0:TB], func=AF.Copy),
                          reads=[P[5]], writes=[g_])
                    ajs = []
                    for j in range(6):
                        pG, pU = P[0 + (j % 2)], P[2 + (j % 2)]
                        for k in range(8):
                            kb.op("pe", lambda e, k=k, j=j, pG=pG: e.matmul(pG.t[:, 0:TB], wgv[:, k, j * 128:(j + 1) * 128],
                                                                            vb.t[:, k, cs], start=(k == 0), stop=(k == 7)),
                                  reads=[wgb, vb], writes=[pG], sig=(k == 7))
                        for k in range(8):
                            kb.op("pe", lambda e, k=k, j=j, pU=pU: e.matmul(pU.t[:, 0:TB], wuv[:, k, j * 128:(j + 1) * 128],
                                                                            vb.t[:, k, cs], start=(k == 0), stop=(k == 7)),
                                  reads=[wub, vb], writes=[pU], sig=(k == 7))
                        s_ = sg_t[j % 2]
                        a_ = aj[j]
                        kb.op("act", lambda e, s_=s_, pG=pG: e.activation(out=s_.t[:], in_=pG.t[:, 0:TB], func=AF.Silu),
                              reads=[pG], writes=[s_])
                        kb.op("dve", lambda e, s_=s_, g_=g_: e.tensor_tensor(out=s_.t[:], in0=s_.t[:], in1=g_.t[:], op=ALU.mult),
                              reads=[s_, g_], writes=[s_])
                        kb.op("dve", lambda e, s_=s_, a_=a_, pU=pU: e.tensor_tensor(out=a_.t[:], in0=pU.t[:, 0:TB], in1=s_.t[:],
                                                                                    op=ALU.mult),
                              reads=[pU, s_], writes=[a_])
                        ajs.append(a_)
                    for d in range(8):
                        pY = P[6 + (d % 2)]
                        for j in range(6):
                            kb.op("pe", lambda e, j=j, d=d, pY=pY: e.matmul(pY.t[:, 0:TB], wdv[:, j, d * 128:(d + 1) * 128],
                                                                            ajs[j].t[:], start=(j == 0), stop=(j == 5)),
                                  reads=[wdb, ajs[j]], writes=[pY], sig=(j == 5))
                        if ex == 0:
                            kb.op("act", lambda e, d=d, pY=pY: e.activation(out=yacc.t[:, d, cs], in_=pY.t[:, 0:TB], func=AF.Copy),
                                  reads=[pY], writes=[yacc])
                        else:
                            kb.op("dve", lambda e, d=d, pY=pY: e.tensor_tensor(out=yacc.t[:, d, cs], in0=pY.t[:, 0:TB],
                                                                               in1=yacc.t[:, d, cs], op=ALU.add),
                                  reads=[pY, yacc], writes=[yacc])

        def phase3(bi, lb):
            cols = slice(bi * TB, (bi + 1) * TB)
            cs = slice(lb * TB, (lb + 1) * TB)
            kb.dma("sp", xin.t[:], xmid_d[:, :, cols], reads=[xmid_tok[bi]], writes=[xin])
            for d in range(8):
                for (lo, hi, w) in _segs(bi):
                    kb.op("dve", lambda e, d=d, lo=lo, hi=hi, w=w: e.scalar_tensor_tensor(
                        out=r.t[:, d, lo:hi], in0=yacc.t[:, d, lb * TB + lo:lb * TB + hi], scalar=ga.t[:, 1, w, d:d + 1],
                        in1=xin.t[:, d, lo:hi], op0=ALU.mult, op1=ALU.add), reads=[yacc, ga, xin], writes=[r])
            ln.normalize(r)
            for c in range(8):
                kb.op("act", lambda e, c=c: e.activation(out=r.t[:, c, :], in_=r.t[:, c, :], func=AF.Identity,
                                                         scale=lnp_sb.t[:, 0, 1, c:c + 1], bias=lnp_sb.t[:, 1, 1, c:c + 1]),
                      reads=[r, lnp_sb], writes=[r])
            kb.dma("sp", xo[:, :, cols], r.t[:], reads=[r], writes=[xo_tok], is_out=True)

        b0 = 0
        for nb_ in sbs:
            for lb in range(nb_):
                phase1(b0 + lb, lb)
            moe(nb_)
            for lb in range(nb_):
                phase3(b0 + lb, lb)
            b0 += nb_
        assert b0 == nblk


        kb.barrier()
        kb.st = old_st


def build_stageC(nblk=11, sbs=(3, 3, 3, 2), nexp=16):
    nc = bass.Bass("TRN2", target_bir_lowering=False)
    T = nblk * TB
    catT = _din(nc, "catT", [1024, T]).rearrange("(c p) t -> p c t", p=128)
    xT = _din(nc, "xT", [1024, T]).rearrange("(c p) t -> p c t", p=128)
    w_o = _din(nc, "w_o", [1024, 1024]).rearrange("(c p) n -> p c n", p=128)
    mods = _din(nc, "mods", [128, 2, 6, 8])
    lnp = _din(nc, "lnp", [128, 2, 2, 8])
    rw = _din(nc, "rw", [1024, 16]).rearrange("(c p) n -> p c n", p=128)
    rbias = _din(nc, "rbias", [128, 16])
    sel_in = _din(nc, "sel", [16, 16 * 128])
    ident_in = _din(nc, "ident", [128, 128])
    wg = _din(nc, "wg", [nexp, 1024, 768])
    wu = _din(nc, "wu", [nexp, 1024, 768])
    wd = _din(nc, "wd", [nexp, 768, 1024])
    xo = _dout(nc, "xo", [1024, T]).rearrange("(c p) t -> p c t", p=128)
    xmid_d = nc.dram_tensor("xmid", [1024, T], F32, kind="Internal").ap().rearrange("(c p) t -> p c t", p=128)
    D = dict(catT=catT, xT=xT, xo=xo, w_o=w_o, mods=mods, lnp=lnp, rw=rw, rbias=rbias, sel=sel_in, ident=ident_in, wg=wg, wu=wu, wd=wd,
             xmid=xmid_d)
    with ExitStack() as st:
        kb = KB(nc, st)
        P = [kb.ps("P%d" % i, [128, 512]) for i in range(8)]
        emit_stageC(kb, nc, P, D, nblk, sbs, nexp)
        kb.finish()
        print("stageC instructions:", kb.n_inst, dict(kb.cnt))
    return nc


def pack_vec(v):
    v = np.asarray(v, np.float32)
    n = v.shape[-1] // 128
    return np.ascontiguousarray(np.moveaxis(v.reshape(v.shape[:-1] + (n, 128)), -1, 0))


def pack_mods(m_ctx, m_lat):
    a = np.stack([np.asarray(m_ctx, np.float32).reshape(6, 8, 128), np.asarray(m_lat, np.float32).reshape(6, 8, 128)], 0)
    return np.ascontiguousarray(a.transpose(3, 0, 1, 2))


def const_sel():
    s = np.zeros((16, 16, 128), np.float32)
    for e in range(16):
        s[e, e, :] = 1.0
    return s.reshape(16, 16 * 128)


SE = 8448
NBE = SE // TB
NCTX = 256
FM_E = 864
TM_E = 320


def _segs_h(bi):
    return [(0, 256, 0), (256, TB, 1)] if bi == 0 else [(0, TB, 1)]


def _bwd_segs(bi):
    s0 = bi * TB
    if s0 + TB <= 8192:
        return [(0, TB, 256 + s0)]
    nlat = 8192 - s0
    return [(0, nlat, 256 + s0), (nlat, TB, 0)]


def emit_inproj(kb, nc, xT, w_in, mods_sb, midx, pT, vtm, nfm, ntm, P, nblk=NBE, load_x=None):
    with ExitStack() as st:
        ncol = nfm + ntm
        w_sb = kb.sb_in(st, "ip_w", [128, 8, ncol], BF16)
        kb.dma("pool", w_sb.t[:], w_in, writes=[w_sb])
        sc1 = kb.sb_in(st, "ip_sc1", [128, 2, 8], F32)
        for w in range(2):
            kb.op("dve", lambda e, w=w: e.tensor_scalar(out=sc1.t[:, w, :], in0=mods_sb.t[:, w, midx + 1, :], scalar1=1.0,
                                                        scalar2=None, op0=ALU.add), reads=[mods_sb], writes=[sc1])
        xin = [kb.sb_in(st, "ip_xin%d" % i, [128, 8, TB], F32) for i in range(2)]
        u = [kb.sb_in(st, "ip_u%d" % i, [128, 8, TB], BF16) for i in range(2)]
        stg = [kb.sb_in(st, "ip_stg%d" % i, [128, TB], F32) for i in range(3)]
        stv = [kb.sb_in(st, "ip_stv%d" % i, [128, max(ntm, 1)], BF16) for i in range(2)]
        nch = (nfm + 127) // 128
        si = 0
        vi = 0
        for bi in range(nblk):
            cols = slice(bi * TB, (bi + 1) * TB)
            xi, ub = xin[bi % 2], u[bi % 2]
            if load_x is None:
                kb.dma("sp", xi.t[:], xT[:, :, cols], writes=[xi])
            else:
                load_x(bi, xi)
            for c in range(8):
                for (lo, hi, w) in _segs_h(bi):
                    kb.op("act", lambda e, c=c, lo=lo, hi=hi, w=w, xi=xi, ub=ub: e.activation(
                        out=ub.t[:, c, lo:hi], in_=xi.t[:, c, lo:hi], func=AF.Identity,
                        scale=sc1.t[:, w, c:c + 1], bias=mods_sb.t[:, w, midx, c:c + 1]), reads=[xi, sc1, mods_sb], writes=[ub])
            for cc in range(nch):
                n = min(128, nfm - cc * 128)
                pA = P[cc % 2]
                for k in range(8):
                    kb.op("pe", lambda e, k=k, cc=cc, n=n, pA=pA, ub=ub: e.matmul(
                        pA.t[0:n, 0:TB], w_sb.t[:, k, cc * 128:cc * 128 + n], ub.t[:, k, :], start=(k == 0), stop=(k == 7)),
                        reads=[w_sb, ub], writes=[pA], sig=(k == 7))
                sg = stg[si % 3]
                si += 1
                eng = "act" if cc % 2 == 0 else "dve"
                if eng == "act":
                    kb.op("act", lambda e, n=n, pA=pA, sg=sg: e.activation(out=sg.t[0:n, :], in_=pA.t[0:n, 0:TB], func=AF.Copy),
                          reads=[pA], writes=[sg])
                else:
                    kb.op("dve", lambda e, n=n, pA=pA, sg=sg: e.tensor_copy(out=sg.t[0:n, :], in_=pA.t[0:n, 0:TB]),
                          reads=[pA], writes=[sg])
                kb.dma("sp", pT[cc * 128:cc * 128 + n, cols], sg.t[0:n, :], reads=[sg])
            for tt in range(TB // 128 if ntm > 0 else 0):
                pV = P[2 + (tt % 2)]
                for k in range(8):
                    kb.op("pe", lambda e, k=k, tt=tt, pV=pV, ub=ub: e.matmul(
                        pV.t[:, 0:ntm], ub.t[:, k, tt * 128:(tt + 1) * 128], w_sb.t[:, k, nfm:nfm + ntm],
                        start=(k == 0), stop=(k == 7)), reads=[w_sb, ub], writes=[pV], sig=(k == 7))
                sv = stv[vi % 2]
                vi += 1
                kb.op("dve", lambda e, pV=pV, sv=sv: e.tensor_copy(out=sv.t[:], in_=pV.t[:, 0:ntm]), reads=[pV], writes=[sv])
                r0 = bi * TB + tt * 128
                kb.dma("sp", vtm[r0:r0 + 128, :], sv.t[:], reads=[sv])
        kb.barrier()


def emit_gla(kb, nc, pT, vtm, wdec, bdec, glag, masks_in, ident_in, ofs, obs, cat_out, P):
    INV_TAU = 1.0 / 16.0
    with ExitStack() as st:
        sbn = lambda n, sh, dt: kb.sb_in(st, "gl_" + n, sh, dt)
        wdec_sb = sbn("wdec", [16, 2, 128], F32)
        nbdec = sbn("nbdec", [64, 2, 2], F32)
        g_sb = sbn("g", [128, 1], F32)
        one = sbn("one", [128, 1], F32)
        eps = sbn("eps", [128, 1], F32)
        ones128 = sbn("ones128", [128, 128], BF16)
        cmask = sbn("cmask", [64, TB], F32)
        mk = sbn("mk", [128, 2, 128], F32)
        idb = sbn("idb", [64, 64], BF16)
        kb.dma("sp", wdec_sb.t[:], wdec, writes=[wdec_sb])
        kb.dma("sp", nbdec.t[:], bdec, writes=[nbdec])
        kb.dma("sp", g_sb.t[:], glag, writes=[g_sb])
        kb.dma("sp", mk.t[:], masks_in, writes=[mk])
        kb.dma("pool", idb.t[:], ident_in[0:64, 0:64], writes=[idb])
        kb.op("dve", lambda e: e.tensor_scalar(out=nbdec.t[:], in0=nbdec.t[:], scalar1=-1.0, scalar2=None, op0=ALU.mult),
              reads=[nbdec], writes=[nbdec])
        kb.op("pool", lambda e: e.memset(one.t[:], 1.0), writes=[one])
        kb.op("pool", lambda e: e.memset(eps.t[:], 1e-6), writes=[eps])
        kb.op("pool", lambda e: e.memset(ones128.t[:], 1.0 / 128.0), writes=[ones128])
        kb.op("pool", lambda e: e.memset(cmask.t[:], 1.0), writes=[cmask])
        for n in range(TB // 128):
            kb.op("pool", lambda e, n=n: e.memset(cmask.t[:, n * 128:n * 128 + 1], 0.0), writes=[cmask])
        S32 = {}
        Sbf = {}
        bufs = {}
        for hd in range(2):
            for dr in range(2):
                key = (hd, dr)
                S32[key] = sbn("S32_%d%d" % key, [64, 128], F32)
                Sbf[key] = sbn("Sbf_%d%d" % key, [64, 128], BF16)
                kb.op("pool", lambda e, key=key: e.memset(S32[key].t[:], 0.0), writes=[S32[key]])
                kb.op("pool", lambda e, key=key: e.memset(Sbf[key].t[:], 0.0), writes=[Sbf[key]])
        for dr in range(2):
            d = {}
            for n, sh, dt in (("q", [64, TB], F32), ("k", [64, TB], F32), ("lx", [16, TB], F32), ("v", [128, 3, 128], BF16),
                              ("sp", [64, TB], F32), ("X", [64, TB], F32), ("tmp", [64, TB], F32), ("D1", [64, TB], F32),
                              ("D4", [64, TB], F32), ("E", [64, TB], F32), ("dec", [64, 3], F32),
                              ("qt", [64, TB], BF16), ("qh", [64, TB], BF16), ("kt", [64, TB], BF16), ("kh", [64, TB], BF16),
                              ("attm", [128, 128], BF16), ("khs", [128, 64], BF16), ("ost", [128, TB], F32)):
                d[n] = sbn("%s_%d" % (n, dr), sh, dt)
            bufs[dr] = d
        PK = Buf(st.enter_context(nc.psum_tensor("ps_" + kb.pfx + "gl_pk", [128, 64], BF16)), "gl_pk")

        def gla_block(hd, dr, bi):
            B = bufs[dr]
            key = (hd, dr)
            segs = [(0, TB, bi * TB)] if dr == 0 else _bwd_segs(bi)
            for (lo, hi, src) in segs:
                n = hi - lo
                kb.dma("sp", B["q"].t[:, lo:hi], pT[hd * 64:(hd + 1) * 64, src:src + n], writes=[B["q"]])
                kb.dma("sp", B["k"].t[:, lo:hi], pT[128 + hd * 64:128 + (hd + 1) * 64, src:src + n], writes=[B["k"]])
                kb.dma("sp", B["lx"].t[:, lo:hi], pT[832 + dr * 16:848 + dr * 16, src:src + n], writes=[B["lx"]])
                kb.dma("sp", B["v"].t[:, lo // 128:hi // 128, :],
                       vtm[src:src + n, hd * 128:(hd + 1) * 128].rearrange("(n p) d -> p n d", p=128), writes=[B["v"]])
            pz = P[0]
            kb.op("pe", lambda e: e.matmul(pz.t[0:64, 0:TB], wdec_sb.t[:, dr, hd * 64:(hd + 1) * 64], B["lx"].t[:],
                                           start=True, stop=True), reads=[wdec_sb, B["lx"]], writes=[pz])
            kb.op("act", lambda e: e.activation(out=B["sp"].t[:], in_=pz.t[0:64, 0:TB], func=AF.Exp, scale=-1.0,
                                                bias=nbdec.t[:, dr, hd:hd + 1]), reads=[pz, nbdec], writes=[B["sp"]])
            kb.op("act", lambda e: e.activation(out=B["sp"].t[:], in_=B["sp"].t[:], func=AF.Ln, bias=one.t[0:64, 0:1]),
                  reads=[B["sp"], one], writes=[B["sp"]])
            kb.op("dve", lambda e: e.tensor_tensor_scan(out=B["X"].t[:], data0=cmask.t[:], data1=B["sp"].t[:], initial=0.0,
                                                        op0=ALU.mult, op1=ALU.add), reads=[cmask, B["sp"]], writes=[B["X"]])
            X3 = B["X"].t[:].rearrange("p (n l) -> p n l", l=128)
            tmp3 = B["tmp"].t[:].rearrange("p (n l) -> p n l", l=128)
            D13 = B["D1"].t[:].rearrange("p (n l) -> p n l", l=128)
            D43 = B["D4"].t[:].rearrange("p (n l) -> p n l", l=128)
            kb.op("act", lambda e: e.activation(out=B["dec"].t[:].unsqueeze(2), in_=X3[:, :, 127:128], func=AF.Exp,
                                                scale=-INV_TAU), reads=[B["X"]], writes=[B["dec"]])
            if dr == 0:
                kb.op("dve", lambda e: e.tensor_tensor(out=D43, in0=X3, in1=X3[:, :, 127:128].broadcast_to([64, 3, 128]),
                                                       op=ALU.subtract), reads=[B["X"]], writes=[B["D4"]])
                Xb = B["X"]
                Xb3 = X3
            else:
                kb.op("dve", lambda e: e.tensor_tensor(out=tmp3, in0=X3, in1=X3[:, :, 127:128].broadcast_to([64, 3, 128]),
                                                       op=ALU.subtract), reads=[B["X"]], writes=[B["tmp"]])
                kb.op("dve", lambda e: e.tensor_tensor(out=B["D1"].t[:], in0=B["sp"].t[:], in1=B["tmp"].t[:], op=ALU.subtract),
                      reads=[B["sp"], B["tmp"]], writes=[B["D1"]])
                kb.op("dve", lambda e: e.tensor_tensor(out=D43, in0=D13, in1=X3[:, :, 127:128].broadcast_to([64, 3, 128]),
                                                       op=ALU.subtract), reads=[B["D1"], B["X"]], writes=[B["D4"]])
                kb.op("dve", lambda e: e.tensor_copy(out=B["tmp"].t[:], in_=B["D1"].t[:]), reads=[B["D1"]], writes=[B["tmp"]])
                Xb = B["tmp"]
                Xb3 = tmp3
            kb.op("dve", lambda e: e.tensor_tensor(out=D13, in0=Xb3, in1=Xb3[:, :, 63:64].broadcast_to([64, 3, 128]),
                                                   op=ALU.subtract), reads=[Xb], writes=[B["D1"]])
            for (src, scale, dst, base, isq) in ((B["D1"], -INV_TAU, B["qt"], B["q"], True), (B["D1"], INV_TAU, B["kt"], B["k"], False),
                                                 (Xb, -INV_TAU, B["qh"], B["q"], True), (B["D4"], INV_TAU, B["kh"], B["k"], False)):
                kb.op("act", lambda e, src=src, scale=scale: e.activation(out=B["E"].t[:], in_=src.t[:], func=AF.Exp, scale=scale),
                      reads=[src], writes=[B["E"]])
                if isq:
                    kb.op("dve", lambda e, dst=dst, base=base: e.scalar_tensor_tensor(
                        out=dst.t[:], in0=base.t[:], scalar=0.125, in1=B["E"].t[:], op0=ALU.mult, op1=ALU.mult),
                        reads=[base, B["E"]], writes=[dst])
                else:
                    kb.op("dve", lambda e, dst=dst, base=base: e.tensor_tensor(out=dst.t[:], in0=base.t[:], in1=B["E"].t[:],
                                                                               op=ALU.mult), reads=[base, B["E"]], writes=[dst])
            order = range(3) if dr == 0 else range(2, -1, -1)
            for ci in order:
                cs = slice(ci * 128, (ci + 1) * 128)
                pA, pO, pD = P[1 + dr], P[3 + dr], P[5 + dr]
                kb.op("pe", lambda e: e.matmul(pA.t[:, 0:128], B["kt"].t[:, cs], B["qt"].t[:, cs], start=True, stop=True),
                      reads=[B["kt"], B["qt"]], writes=[pA])
                kb.op("dve", lambda e: e.tensor_tensor(out=B["attm"].t[:], in0=pA.t[:, 0:128], in1=mk.t[:, dr, :], op=ALU.mult),
                      reads=[pA, mk], writes=[B["attm"]])
                kb.op("pe", lambda e: e.matmul(pO.t[:, 0:128], B["v"].t[:, ci, :], B["attm"].t[:], start=True, stop=False),
                      reads=[B["v"], B["attm"]], writes=[pO], sig=False)
                kb.op("pe", lambda e: e.matmul(pO.t[:, 0:128], Sbf[key].t[:], B["qh"].t[:, cs], start=False, stop=True),
                      reads=[Sbf[key], B["qh"]], writes=[pO])
                kb.op("act", lambda e: e.activation(out=B["ost"].t[:, cs], in_=pO.t[:, 0:128], func=AF.Copy),
                      reads=[pO], writes=[B["ost"]])
                kb.op("pe", lambda e: e.transpose(PK.t[:, 0:64], B["kh"].t[:, cs], idb.t[:]), reads=[B["kh"], idb], writes=[PK])
                kb.op("act", lambda e: e.activation(out=B["khs"].t[:], in_=PK.t[:, 0:64], func=AF.Copy),
                      reads=[PK], writes=[B["khs"]])
                kb.op("pe", lambda e: e.matmul(pD.t[0:64, 0:128], B["khs"].t[:], B["v"].t[:, ci, :], start=True, stop=True),
                      reads=[B["khs"], B["v"]], writes=[pD])
                kb.op("dve", lambda e: e.scalar_tensor_tensor(out=S32[key].t[:], in0=S32[key].t[:], scalar=B["dec"].t[:, ci:ci + 1],
                                                              in1=pD.t[0:64, 0:128], op0=ALU.mult, op1=ALU.add),
                      reads=[S32[key], B["dec"], pD], writes=[S32[key]])
                kb.op("act", lambda e: e.activation(out=Sbf[key].t[:], in_=S32[key].t[:], func=AF.Copy),
                      reads=[S32[key]], writes=[Sbf[key]])
            dst = ofs if dr == 0 else obs
            for (lo, hi, src) in segs:
                kb.dma("sp", dst[hd * 128:(hd + 1) * 128, src:src + (hi - lo)], B["ost"].t[:, lo:hi], reads=[B["ost"]])

        for hd in range(2):
            for i in range(NBE):
                gla_block(hd, 0, i)
                gla_block(hd, 1, NBE - 1 - i)
        kb.barrier()
        fo = [sbn("fo%d" % i, [128, TB], F32) for i in range(2)]
        fb = [sbn("fb%d" % i, [128, TB], F32) for i in range(2)]
        fr = [sbn("fr%d" % i, [128, TB], F32) for i in range(2)]
        fsq = sbn("fsq", [128, TB], BF16)
        frs = sbn("frs", [128, TB], F32)
        fi = 0
        for hd in range(2):
            for bi in range(NBE):
                cols = slice(bi * TB, (bi + 1) * TB)
                o_, b_, r_ = fo[fi % 2], fb[fi % 2], fr[fi % 2]
                fi += 1
                kb.dma("sp", o_.t[:], ofs[hd * 128:(hd + 1) * 128, cols], writes=[o_])
                kb.dma("sp", b_.t[:], obs[hd * 128:(hd + 1) * 128, cols], writes=[b_])
                kb.dma("sp", r_.t[:], pT[256 + hd * 128:256 + (hd + 1) * 128, cols], writes=[r_])
                kb.op("dve", lambda e: e.tensor_tensor(out=o_.t[:], in0=o_.t[:], in1=b_.t[:], op=ALU.add), reads=[o_, b_], writes=[o_])
                kb.op("act", lambda e: e.activation(out=fsq.t[:], in_=o_.t[:], func=AF.Square), reads=[o_], writes=[fsq])
                kb.op("pe", lambda e: e.matmul(P[0].t[:, 0:TB], ones128.t[:], fsq.t[:], start=True, stop=True),
                      reads=[ones128, fsq], writes=[P[0]])
                kb.op("act", lambda e: e.activation(out=frs.t[:], in_=P[0].t[:, 0:TB], func=AF.Sqrt, bias=eps.t[:, 0:1]),
                      reads=[P[0], eps], writes=[frs])
                kb.op("dve", lambda e: e.reciprocal(out=frs.t[:], in_=frs.t[:]), reads=[frs], writes=[frs])
                kb.op("dve", lambda e: e.scalar_tensor_tensor(out=o_.t[:], in0=o_.t[:], scalar=g_sb.t[:, 0:1], in1=frs.t[:],
                                                              op0=ALU.mult, op1=ALU.mult), reads=[o_, g_sb, frs], writes=[o_])
                kb.op("act", lambda e: e.activation(out=r_.t[:], in_=r_.t[:], func=AF.Silu), reads=[r_], writes=[r_])
                kb.op("dve", lambda e: e.tensor_tensor(out=o_.t[:], in0=o_.t[:], in1=r_.t[:], op=ALU.mult), reads=[o_, r_], writes=[o_])
                kb.dma("sp", cat_out[hd * 128:(hd + 1) * 128, cols], o_.t[:], reads=[o_], is_out=True)
        kb.barrier()


def emit_gqa(kb, nc, pT, vtm, qkg, rope_R, cosT, sinT, cat_out, P):
    QB = 512
    with ExitStack() as st:
        sbn = lambda n, sh, dt: kb.sb_in(st, "gq_" + n, sh, dt)
        QT = [sbn("QT%d" % j, [64, SE], BF16) for j in range(4)]
        KT = sbn("KT", [64, SE], BF16)
        V1 = sbn("V1", [128, SE // 128, 65], BF16)
        g_sb = sbn("g", [64, 2], F32)
        R_sb = sbn("R", [64, 64], F32)
        ones64 = sbn("ones64", [64, 64], F32)
        onesr = sbn("onesr", [128, 64], F32)
        eps = sbn("eps", [64, 1], F32)
        kb.dma("sp", g_sb.t[:], qkg, writes=[g_sb])
        kb.dma("sp", R_sb.t[:], rope_R, writes=[R_sb])
        kb.op("pool", lambda e: e.memset(ones64.t[:], 1.0 / 64.0), writes=[ones64])
        kb.op("pool", lambda e: e.memset(onesr.t[:], 1.0), writes=[onesr])
        kb.op("pool", lambda e: e.memset(eps.t[:], 1e-6), writes=[eps])
        kb.op("pool", lambda e: e.memset(V1.t[:], 1.0), writes=[V1])
        kb.dma("sp", V1.t[:, :, 0:64], vtm[:, 256:320].rearrange("(n p) d -> p n d", p=128), writes=[V1])
        x = [sbn("x%d" % i, [64, TB], F32) for i in range(2)]
        sq = sbn("sq", [64, TB], F32)
        rs = sbn("rs", [64, TB], F32)
        xn = sbn("xn", [64, TB], F32)
        t1 = sbn("t1", [64, TB], F32)
        t2 = sbn("t2", [64, TB], F32)
        cs_sb = [sbn("cos%d" % i, [64, TB], F32) for i in range(2)]
        sn_sb = [sbn("sin%d" % i, [64, TB], F32) for i in range(2)]
        xi = 0
        for bi in range(NBE):
            cols = slice(bi * TB, (bi + 1) * TB)
            llo = 256 if bi == 0 else 0
            lat0 = bi * TB + llo - 256
            nl = TB - llo
            c_, s_ = cs_sb[bi % 2], sn_sb[bi % 2]
            kb.dma("sp", c_.t[:, llo:TB], cosT[:, lat0:lat0 + nl], writes=[c_])
            kb.dma("sp", s_.t[:, llo:TB], sinT[:, lat0:lat0 + nl], writes=[s_])
            for j in range(5):
                x_ = x[xi % 2]
                xi += 1
                row0 = 512 + j * 64
                dst = QT[j] if j < 4 else KT
                gcol = 0 if j < 4 else 1
                oscale = 0.125 if j < 4 else 1.0
                kb.dma("sp", x_.t[:], pT[row0:row0 + 64, cols], writes=[x_])
                kb.op("act", lambda e: e.activation(out=sq.t[:], in_=x_.t[:], func=AF.Square), reads=[x_], writes=[sq])
                kb.op("pe", lambda e: e.matmul(P[0].t[0:64, 0:TB], ones64.t[:], sq.t[:], start=True, stop=True),
                      reads=[ones64, sq], writes=[P[0]])
                kb.op("act", lambda e: e.activation(out=rs.t[:], in_=P[0].t[0:64, 0:TB], func=AF.Sqrt, bias=eps.t[:, 0:1]),
                      reads=[P[0], eps], writes=[rs])
                kb.op("dve", lambda e: e.reciprocal(out=rs.t[:], in_=rs.t[:]), reads=[rs], writes=[rs])
                kb.op("dve", lambda e: e.scalar_tensor_tensor(out=xn.t[:], in0=x_.t[:], scalar=g_sb.t[:, gcol:gcol + 1], in1=rs.t[:],
                                                              op0=ALU.mult, op1=ALU.mult), reads=[x_, g_sb, rs], writes=[xn])
                if llo > 0:
                    kb.op("act", lambda e: e.activation(out=dst.t[:, bi * TB:bi * TB + llo], in_=xn.t[:, 0:llo], func=AF.Copy,
                                                        scale=oscale), reads=[xn], writes=[dst])
                kb.op("pe", lambda e: e.matmul(P[1].t[0:64, 0:nl], R_sb.t[:], xn.t[:, llo:TB], start=True, stop=True),
                      reads=[R_sb, xn], writes=[P[1]])
                kb.op("dve", lambda e: e.tensor_tensor(out=t1.t[:, llo:TB], in0=xn.t[:, llo:TB], in1=c_.t[:, llo:TB], op=ALU.mult),
                      reads=[xn, c_], writes=[t1])
                kb.op("dve", lambda e: e.tensor_tensor(out=t2.t[:, llo:TB], in0=P[1].t[0:64, 0:nl], in1=s_.t[:, llo:TB], op=ALU.mult),
                      reads=[P[1], s_], writes=[t2])
                kb.op("dve", lambda e: e.tensor_tensor(out=t1.t[:, llo:TB], in0=t1.t[:, llo:TB], in1=t2.t[:, llo:TB], op=ALU.add),
                      reads=[t1, t2], writes=[t1])
                kb.op("act", lambda e: e.activation(out=dst.t[:, bi * TB + llo:(bi + 1) * TB], in_=t1.t[:, llo:TB], func=AF.Copy,
                                                    scale=oscale), reads=[t1], writes=[dst])
        pt = [sbn("pt%d" % i, [128, QB], BF16) for i in range(3)]
        osb = [sbn("osb%d" % i, [64, QB], F32) for i in range(2)]
        rsum = sbn("rsum", [128, QB], F32)
        pi = 0
        qi = 0
        jobs = []
        for j in range(4):
            jobs.append((j, 0, 256, 2))
            for qb in range(8192 // QB):
                jobs.append((j, 256 + qb * QB, QB, SE // 128))
        for (j, q0, nq, nkt) in jobs:
            pO = P[4 + (qi % 2)]
            o_ = osb[qi % 2]
            qi += 1
            for kt in range(nkt):
                pS = P[2 + (kt % 2)]
                p_ = pt[pi % 3]
                pi += 1
                kb.op("pe", lambda e: e.matmul(pS.t[:, 0:nq], KT.t[:, kt * 128:(kt + 1) * 128], QT[j].t[:, q0:q0 + nq],
                                               start=True, stop=True), reads=[KT, QT[j]], writes=[pS])
                kb.op("act", lambda e: e.activation(out=p_.t[:, 0:nq], in_=pS.t[:, 0:nq], func=AF.Exp), reads=[pS], writes=[p_])
                kb.op("pe", lambda e: e.matmul(pO.t[0:65, 0:nq], V1.t[:, kt, :], p_.t[:, 0:nq], start=(kt == 0), stop=(kt == nkt - 1)),
                      reads=[V1, p_], writes=[pO], sig=(kt == nkt - 1))
            kb.op("dve", lambda e: e.reciprocal(out=rsum.t[64:65, 0:nq], in_=pO.t[64:65, 0:nq]), reads=[pO], writes=[rsum])
            kb.op("act", lambda e: e.activation(out=o_.t[:, 0:nq], in_=pO.t[0:64, 0:nq], func=AF.Copy), reads=[pO], writes=[o_])
            kb.op("pe", lambda e: e.matmul(P[6].t[0:64, 0:nq], onesr.t[64:65, :], rsum.t[64:65, 0:nq], start=True, stop=True),
                  reads=[onesr, rsum], writes=[P[6]])
            kb.op("dve", lambda e: e.tensor_tensor(out=o_.t[:, 0:nq], in0=o_.t[:, 0:nq], in1=P[6].t[0:64, 0:nq], op=ALU.mult),
                  reads=[o_, P[6]], writes=[o_])
            kb.dma("sp", cat_out[256 + j * 64:256 + (j + 1) * 64, q0:q0 + nq], o_.t[:, 0:nq], reads=[o_], is_out=True)
        kb.barrier()


def build_stageB_even(parts=("inproj", "gla", "gqa")):
    nc = bass.Bass("TRN2", target_bir_lowering=False)
    xT = _din(nc, "xT", [1024, SE]).rearrange("(c p) t -> p c t", p=128)
    w_in = _din(nc, "w_in", [1024, FM_E + TM_E]).rearrange("(c p) n -> p c n", p=128)
    mods = _din(nc, "mods", [128, 2, 6, 8])
    wdec = _din(nc, "wdec", [16, 2, 128])
    bdec = _din(nc, "bdec", [64, 2, 2])
    glag = _din(nc, "glag", [128, 1])
    masks = _din(nc, "masks", [128, 2, 128])
    ident = _din(nc, "ident", [128, 128])
    qkg = _din(nc, "qkg", [64, 2])
    ropeR = _din(nc, "ropeR", [64, 64])
    cosT = _din(nc, "cosT", [64, 8192])
    sinT = _din(nc, "sinT", [64, 8192])
    cat = _dout(nc, "cat", [512, SE])
    pT = nc.dram_tensor("pT", [FM_E, SE], F32, kind="Internal").ap()
    vtm = nc.dram_tensor("vtm", [SE, TM_E], BF16, kind="Internal").ap()
    ofs = nc.dram_tensor("ofs", [256, SE], F32, kind="Internal").ap()
    obs = nc.dram_tensor("obs", [256, SE], F32, kind="Internal").ap()
    with ExitStack() as st:
        kb = KB(nc, st)
        P = [kb.ps("P%d" % i, [128, 512]) for i in range(7)]
        mods_sb = kb.sb("modsb", [128, 2, 6, 8], F32)
        kb.dma("sp", mods_sb.t[:], mods, writes=[mods_sb])
        if "inproj" in parts:
            emit_inproj(kb, nc, xT, w_in, mods_sb, 0, pT, vtm, FM_E, TM_E, P)
        if "gla" in parts:
            emit_gla(kb, nc, pT, vtm, wdec, bdec, glag, masks, ident, ofs, obs, cat, P)
        if "gqa" in parts:
            emit_gqa(kb, nc, pT, vtm, qkg, ropeR, cosT, sinT, cat, P)
        kb.finish()
        print("stageB_even instructions:", kb.n_inst, dict(kb.cnt))
    return nc


def const_masks():
    m = np.arange(128)[:, None]
    l = np.arange(128)[None, :]
    return np.ascontiguousarray(np.stack([(l >= m), (l <= m)], 1).astype(np.float32))


def const_ropeR():
    R = np.zeros((64, 64), np.float32)
    for dp in range(64):
        blk = dp // 16
        if blk % 2 == 0:
            R[dp + 16, dp] = -1.0
        else:
            R[dp - 16, dp] = 1.0
    return R


def const_rope_tables():
    rows = 8192 // 64
    r = np.repeat(np.arange(rows, dtype=np.float32), 64)
    col = np.tile(np.arange(64, dtype=np.float32), rows)
    half = 32
    inv = (np.float32(10000.0) ** (-np.arange(0, half, 2, dtype=np.float32) / np.float32(half))).astype(np.float32)
    ar = r[:, None] * inv
    ac = col[:, None] * inv
    ang = np.concatenate([ar, ar, ac, ac], -1)
    return np.ascontiguousarray(np.cos(ang).T.astype(np.float32)), np.ascontiguousarray(np.sin(ang).T.astype(np.float32))


def even_core_cols(g):
    o_q, o_k, o_v, o_r, o_lf, o_lb, o_gq, o_gk, o_gv = np.cumsum([0, 256, 256, 512, 512, 16, 16, 512, 128])
    fm = []
    for hd in (2 * g, 2 * g + 1):
        fm += list(range(o_q + hd * 64, o_q + (hd + 1) * 64))
    for hd in (2 * g, 2 * g + 1):
        fm += list(range(o_k + hd * 64, o_k + (hd + 1) * 64))
    for hd in (2 * g, 2 * g + 1):
        fm += list(range(o_r + hd * 128, o_r + (hd + 1) * 128))
    for j in range(4 * g, 4 * g + 4):
        fm += list(range(o_gq + j * 64, o_gq + (j + 1) * 64))
    fm += list(range(o_gk + g * 64, o_gk + (g + 1) * 64))
    fm += list(range(o_lf, o_lf + 16)) + list(range(o_lb, o_lb + 16))
    tm = []
    for hd in (2 * g, 2 * g + 1):
        tm += list(range(o_v + hd * 128, o_v + (hd + 1) * 128))
    tm += list(range(o_gv + g * 64, o_gv + (g + 1) * 64))
    assert len(fm) == FM_E and len(tm) == TM_E
    return np.array(fm + tm)


def even_core_inputs(g, xT_b, mods_b, w_in, w_dec, b_dec, gla_g, qg, kg, consts):
    hd = (2 * g, 2 * g + 1)
    wdec = np.stack([np.concatenate([w_dec[d][:, h * 64:(h + 1) * 64] for h in hd], 1) for d in range(2)], 1)
    bdec = np.stack([np.stack([b_dec[d][h * 64:(h + 1) * 64] for h in hd], 1) for d in range(2)], 1)
    return dict(xT=xT_b, w_in=np.ascontiguousarray(w_in[:, even_core_cols(g)]), mods=mods_b,
                wdec=np.ascontiguousarray(wdec, np.float32), bdec=np.ascontiguousarray(bdec, np.float32),
                glag=np.ascontiguousarray(gla_g.reshape(128, 1)), masks=consts["masks"], ident=consts["ident"],
                qkg=np.ascontiguousarray(np.stack([qg, kg], 1)), ropeR=consts["ropeR"], cosT=consts["cosT"], sinT=consts["sinT"])


FM_O = 1164
NEGBIG = -30000.0


def emit_conv(kb, nc, pT, cT, convw, convb, P):
    with ExitStack() as st:
        sbn = lambda n, sh, dt: kb.sb_in(st, "cv_" + n, sh, dt)
        w_sb = sbn("w", [128, 5, 5], F32)
        b_sb = sbn("b", [128, 5], F32)
        kb.dma("sp", w_sb.t[:], convw, writes=[w_sb])
        kb.dma("sp", b_sb.t[:], convb, writes=[b_sb])
        xin = [sbn("xin%d" % i, [128, TB + 4], F32) for i in range(3)]
        acc = [sbn("acc%d" % i, [128, TB], F32) for i in range(3)]
        k = 0
        segs = [(0, 256, 0, 256)]
        segs += [(256 + i * TB, min(256 + (i + 1) * TB, SE), 256, SE) for i in range((8192 + TB - 1) // TB)]
        for (a, b, s0, s1) in segs:
            n = b - a
            lo, hi = max(a - 2, s0), min(b + 2, s1)
            for c in range(5):
                xi, ac = xin[k % 3], acc[k % 3]
                k += 1
                kb.op("pool", lambda e: e.memset(xi.t[:], 0.0), writes=[xi])
                kb.dma("sp", xi.t[:, lo - (a - 2):hi - (a - 2)], pT[384 + c * 128:384 + (c + 1) * 128, lo:hi], writes=[xi])
                kb.op("dve", lambda e: e.tensor_scalar(out=ac.t[:, 0:n], in0=xi.t[:, 0:n], scalar1=w_sb.t[:, c, 0:1],
                                                       scalar2=b_sb.t[:, c:c + 1], op0=ALU.mult, op1=ALU.add),
                      reads=[xi, w_sb, b_sb], writes=[ac])
                for j in range(1, 5):
                    kb.op("dve", lambda e, j=j: e.scalar_tensor_tensor(out=ac.t[:, 0:n], in0=xi.t[:, j:j + n], scalar=w_sb.t[:, c, j:j + 1],
                                                                       in1=ac.t[:, 0:n], op0=ALU.mult, op1=ALU.add),
                          reads=[xi, w_sb, ac], writes=[ac])
                kb.op("act", lambda e: e.activation(out=ac.t[:, 0:n], in_=ac.t[:, 0:n], func=AF.Silu), reads=[ac], writes=[ac])
                kb.dma("sp", cT[c * 128:(c + 1) * 128, a:b], ac.t[:, 0:n], reads=[ac])
        kb.barrier()


def emit_ssd(kb, nc, pT, cT, dtb_in, alog_in, dsk_in, ng_in, masks_in, ident_in, yfs, ybs, cat_out, P):
    with ExitStack() as st:
        sbn = lambda n, sh, dt: kb.sb_in(st, "sd_" + n, sh, dt)
        dtbias = sbn("dtbias", [128, 12], F32)
        negA = sbn("negA", [128, 12], F32)
        mkb = sbn("mkb", [128, 2, 128], F32)
        onesr = sbn("onesr", [1, 128], F32)
        one1 = sbn("one1", [128, 1], F32)
        cmask = sbn("cmask", [128, TB], F32)
        idf = sbn("idf", [128, 128], BF16)
        kb.dma("sp", dtbias.t[:], dtb_in, writes=[dtbias])
        kb.dma("sp", negA.t[:], alog_in, writes=[negA])
        kb.dma("sp", mkb.t[:], masks_in, writes=[mkb])
        kb.dma("pool", idf.t[:], ident_in, writes=[idf])
        kb.op("act", lambda e: e.activation(out=negA.t[:], in_=negA.t[:], func=AF.Exp), reads=[negA], writes=[negA])
        kb.op("dve", lambda e: e.tensor_scalar(out=negA.t[:], in0=negA.t[:], scalar1=-1.0, scalar2=None, op0=ALU.mult),
              reads=[negA], writes=[negA])
        kb.op("dve", lambda e: e.tensor_scalar(out=mkb.t[:], in0=mkb.t[:], scalar1=-1.0, scalar2=-NEGBIG, op0=ALU.add, op1=ALU.mult),
              reads=[mkb], writes=[mkb])
        kb.op("pool", lambda e: e.memset(onesr.t[:], 1.0), writes=[onesr])
        kb.op("pool", lambda e: e.memset(one1.t[:], 1.0), writes=[one1])
        kb.op("pool", lambda e: e.memset(cmask.t[:], 1.0), writes=[cmask])
        for n in range(TB // 128):
            kb.op("pool", lambda e, n=n: e.memset(cmask.t[:, n * 128:n * 128 + 1], 0.0), writes=[cmask])
        H32, Hbf = {}, {}
        for h in range(6):
            for dr in range(2):
                key = (h, dr)
                H32[key] = sbn("H32_%d%d" % key, [128, 64], F32)
                Hbf[key] = sbn("Hbf_%d%d" % key, [128, 64], BF16)
                kb.op("pool", lambda e, key=key: e.memset(H32[key].t[:], 0.0), writes=[H32[key]])
                kb.op("pool", lambda e, key=key: e.memset(Hbf[key].t[:], 0.0), writes=[Hbf[key]])
        sh = {}
        for dr in range(2):
            d = {}
            for n, shp, dt in (("BT", [128, TB], BF16), ("CT", [128, TB], F32), ("xsT", [128, 3, TB], BF16), ("CB", [128, 3, 128], F32),
                               ("Btm", [128, 3, 128], BF16), ("xstm", [128, 3, 384], F32)):
                d[n] = sbn("%s_%d" % (n, dr), shp, dt)
            sh[dr] = d
        pd = {}
        for n, shp, dt in (("raw", [128, TB], F32), ("dt", [128, TB], F32), ("P", [128, TB], F32), ("X", [128, TB], F32),
                           ("nX", [128, TB], F32), ("EL", [128, TB], F32), ("w", [128, TB], F32), ("ED", [128, 3], F32),
                           ("dec", [128, 128], F32), ("G", [128, 128], BF16), ("cols", [128, 2], F32), ("xd", [128, 64], BF16),
                           ("xdw", [128, 64], BF16), ("Ct", [128, 128], BF16), ("yst", [64, TB], F32)):
            pd[n] = [sbn("%s_%d" % (n, i), shp, dt) for i in range(2)]
        PT = Buf(st.enter_context(nc.psum_tensor("ps_" + kb.pfx + "sd_pt", [128, 512], BF16)), "sd_pt")
        cnt = [0]

        def load_shared(dr, bi):
            Sd = sh[dr]
            segs = [(0, TB, bi * TB)] if dr == 0 else _bwd_segs(bi)
            for (lo, hi, src) in segs:
                n = hi - lo
                kb.dma("pool", Sd["BT"].t[:, lo:hi], cT[384:512, src:src + n], writes=[Sd["BT"]])
                kb.dma("sp", Sd["CT"].t[:, lo:hi], cT[512:640, src:src + n], writes=[Sd["CT"]])
                kb.dma("pool", Sd["xsT"].t[:, :, lo:hi], cT[0:384, src:src + n].rearrange("(c p) t -> p c t", p=128), writes=[Sd["xsT"]])
            ctb = pd["Ct"][0]
            for ci in range(3):
                cs = slice(ci * 128, (ci + 1) * 128)
                kb.op("act", lambda e: e.activation(out=ctb.t[:], in_=Sd["CT"].t[:, cs], func=AF.Copy), reads=[Sd["CT"]], writes=[ctb])
                kb.op("pe", lambda e: e.matmul(P[0].t[:, 0:128], Sd["BT"].t[:, cs], ctb.t[:], start=True, stop=True),
                      reads=[Sd["BT"], ctb], writes=[P[0]])
                kb.op("act", lambda e: e.activation(out=Sd["CB"].t[:, ci, :], in_=P[0].t[:, 0:128], func=AF.Copy),
                      reads=[P[0]], writes=[Sd["CB"]])
                kb.op("pe", lambda e: e.transpose(PT.t[:, 0:128], Sd["BT"].t[:, cs], idf.t[:]), reads=[Sd["BT"], idf], writes=[PT])
                kb.op("act", lambda e: e.activation(out=Sd["Btm"].t[:, ci, :], in_=PT.t[:, 0:128], func=AF.Copy),
                      reads=[PT], writes=[Sd["Btm"]])
                for c in range(3):
                    kb.op("pe", lambda e: e.transpose(PT.t[:, 128 + c * 128:256 + c * 128], Sd["xsT"].t[:, c, cs], idf.t[:]),
                          reads=[Sd["xsT"], idf], writes=[PT], sig=(c == 2))
                kb.op("dve", lambda e: e.tensor_copy(out=Sd["xstm"].t[:, ci, :], in_=PT.t[:, 128:512]), reads=[PT], writes=[Sd["xstm"]])

        def ssd_block(h, dr, bi):
            Sd = sh[dr]
            i = cnt[0] % 2
            cnt[0] += 1
            B = {k_: v_[i] for k_, v_ in pd.items()}
            key = (h, dr)
            hd = dr * 6 + h
            segs = [(0, TB, bi * TB)] if dr == 0 else _bwd_segs(bi)
            for (lo, hi, src) in segs:
                n = hi - lo
                row = 1152 + dr * 6 + h
                kb.dma("sp", B["raw"].t[:, lo:hi].unsqueeze(1), pT[row:row + 1, src:src + n].partition_broadcast(128), writes=[B["raw"]])
            kb.op("act", lambda e: e.activation(out=B["dt"].t[:], in_=B["raw"].t[:], func=AF.Exp, bias=dtbias.t[:, hd:hd + 1]),
                  reads=[B["raw"], dtbias], writes=[B["dt"]])
            kb.op("act", lambda e: e.activation(out=B["dt"].t[:], in_=B["dt"].t[:], func=AF.Ln, bias=one1.t[:, 0:1]),
                  reads=[B["dt"], one1], writes=[B["dt"]])
            kb.op("dve", lambda e: e.tensor_scalar(out=B["raw"].t[:], in0=B["dt"].t[:], scalar1=negA.t[:, hd:hd + 1], scalar2=None,
                                                   op0=ALU.mult), reads=[B["dt"], negA], writes=[B["raw"]])
            kb.op("dve", lambda e: e.tensor_tensor_scan(out=B["P"].t[:], data0=cmask.t[:], data1=B["raw"].t[:], initial=0.0,
                                                        op0=ALU.mult, op1=ALU.add), reads=[cmask, B["raw"]], writes=[B["P"]])
            P3 = B["P"].t[:].rearrange("p (n l) -> p n l", l=128)
            tot_bc = P3[:, :, 127:128].broadcast_to([128, 3, 128])
            r3 = lambda b_: b_.t[:].rearrange("p (n l) -> p n l", l=128)
            kb.op("act", lambda e: e.activation(out=B["ED"].t[:].unsqueeze(2), in_=P3[:, :, 127:128], func=AF.Exp),
                  reads=[B["P"]], writes=[B["ED"]])
            if dr == 0:
                kb.op("dve", lambda e: e.tensor_copy(out=B["X"].t[:], in_=B["P"].t[:]), reads=[B["P"]], writes=[B["X"]])
                kb.op("act", lambda e: e.activation(out=B["EL"].t[:], in_=B["P"].t[:], func=AF.Exp), reads=[B["P"]], writes=[B["EL"]])
                kb.op("dve", lambda e: e.tensor_tensor(out=r3(B["w"]), in0=tot_bc, in1=P3, op=ALU.subtract), reads=[B["P"]], writes=[B["w"]])
            else:
                kb.op("dve", lambda e: e.tensor_tensor(out=B["X"].t[:], in0=B["P"].t[:], in1=B["raw"].t[:], op=ALU.subtract),
                      reads=[B["P"], B["raw"]], writes=[B["X"]])
                kb.op("dve", lambda e: e.tensor_tensor(out=r3(B["EL"]), in0=tot_bc, in1=r3(B["X"]), op=ALU.subtract),
                      reads=[B["P"], B["X"]], writes=[B["EL"]])
                kb.op("act", lambda e: e.activation(out=B["EL"].t[:], in_=B["EL"].t[:], func=AF.Exp), reads=[B["EL"]], writes=[B["EL"]])
                kb.op("dve", lambda e: e.tensor_copy(out=B["w"].t[:], in_=B["X"].t[:]), reads=[B["X"]], writes=[B["w"]])
            kb.op("act", lambda e: e.activation(out=B["w"].t[:], in_=B["w"].t[:], func=AF.Exp), reads=[B["w"]], writes=[B["w"]])
            kb.op("dve", lambda e: e.tensor_tensor(out=B["w"].t[:], in0=B["w"].t[:], in1=B["dt"].t[:], op=ALU.mult),
                  reads=[B["w"], B["dt"]], writes=[B["w"]])
            kb.op("dve", lambda e: e.tensor_scalar(out=B["nX"].t[:], in0=B["X"].t[:], scalar1=-1.0, scalar2=None, op0=ALU.mult),
                  reads=[B["X"]], writes=[B["nX"]])
            order = range(3) if dr == 0 else range(2, -1, -1)
            for ci in order:
                cs = slice(ci * 128, (ci + 1) * 128)
                pS, pC, pY, pH = P[1], P[2], P[3 + dr], P[5 + dr]
                if dr == 0:
                    kb.op("pe", lambda e: e.matmul(pS.t[:, 0:128], onesr.t[0:1, :], B["X"].t[0:1, cs], start=True, stop=False),
                          reads=[onesr, B["X"]], writes=[pS], sig=False)
                    kb.op("pe", lambda e: e.matmul(pS.t[:, 0:128], B["nX"].t[0:1, cs], onesr.t[0:1, :], start=False, stop=True),
                          reads=[onesr, B["nX"]], writes=[pS])
                else:
                    kb.op("pe", lambda e: e.matmul(pS.t[:, 0:128], onesr.t[0:1, :], B["nX"].t[0:1, cs], start=True, stop=False),
                          reads=[onesr, B["nX"]], writes=[pS], sig=False)
                    kb.op("pe", lambda e: e.matmul(pS.t[:, 0:128], B["X"].t[0:1, cs], onesr.t[0:1, :], start=False, stop=True),
                          reads=[onesr, B["X"]], writes=[pS])
                kb.op("dve", lambda e: e.tensor_tensor(out=B["dec"].t[:], in0=pS.t[:, 0:128], in1=mkb.t[:, dr, :], op=ALU.add),
                      reads=[pS, mkb], writes=[B["dec"]])
                kb.op("act", lambda e: e.activation(out=B["dec"].t[:], in_=B["dec"].t[:], func=AF.Exp), reads=[B["dec"]], writes=[B["dec"]])
                kb.op("dve", lambda e: e.tensor_tensor(out=B["G"].t[:], in0=B["dec"].t[:], in1=Sd["CB"].t[:, ci, :], op=ALU.mult),
                      reads=[B["dec"], Sd["CB"]], writes=[B["G"]])
                kb.op("pe", lambda e: e.matmul(pC.t[:, 0:1], B["dt"].t[0:1, cs], onesr.t[0:1, 0:1], start=True, stop=True),
                      reads=[B["dt"], onesr], writes=[pC], sig=False)
                kb.op("pe", lambda e: e.matmul(pC.t[:, 1:2], B["w"].t[0:1, cs], onesr.t[0:1, 0:1], start=True, stop=True),
                      reads=[B["w"], onesr], writes=[pC])
                kb.op("act", lambda e: e.activation(out=B["cols"].t[:], in_=pC.t[:, 0:2], func=AF.Copy), reads=[pC], writes=[B["cols"]])
                xs_h = Sd["xstm"].t[:, ci, h * 64:(h + 1) * 64]
                kb.op("dve", lambda e: e.tensor_scalar(out=B["xd"].t[:], in0=xs_h, scalar1=B["cols"].t[:, 0:1], scalar2=None, op0=ALU.mult),
                      reads=[Sd["xstm"], B["cols"]], writes=[B["xd"]])
                kb.op("dve", lambda e: e.tensor_scalar(out=B["xdw"].t[:], in0=xs_h, scalar1=B["cols"].t[:, 1:2], scalar2=None, op0=ALU.mult),
                      reads=[Sd["xstm"], B["cols"]], writes=[B["xdw"]])
                kb.op("dve", lambda e: e.tensor_tensor(out=B["Ct"].t[:], in0=Sd["CT"].t[:, cs], in1=B["EL"].t[:, cs], op=ALU.mult),
                      reads=[Sd["CT"], B["EL"]], writes=[B["Ct"]])
                kb.op("pe", lambda e: e.matmul(pY.t[0:64, 0:128], B["xd"].t[:], B["G"].t[:], start=True, stop=False),
                      reads=[B["xd"], B["G"]], writes=[pY], sig=False)
                kb.op("pe", lambda e: e.matmul(pY.t[0:64, 0:128], Hbf[key].t[:], B["Ct"].t[:], start=False, stop=True),
                      reads=[Hbf[key], B["Ct"]], writes=[pY])
                kb.op("act", lambda e: e.activation(out=B["yst"].t[:, cs], in_=pY.t[0:64, 0:128], func=AF.Copy), reads=[pY], writes=[B["yst"]])
                kb.op("pe", lambda e: e.matmul(pH.t[:, 0:64], Sd["Btm"].t[:, ci, :], B["xdw"].t[:], start=True, stop=True),
                      reads=[Sd["Btm"], B["xdw"]], writes=[pH])
                kb.op("dve", lambda e: e.scalar_tensor_tensor(out=H32[key].t[:], in0=H32[key].t[:], scalar=B["ED"].t[:, ci:ci + 1],
                                                              in1=pH.t[:, 0:64], op0=ALU.mult, op1=ALU.add),
                      reads=[H32[key], B["ED"], pH], writes=[H32[key]])
                kb.op("act", lambda e: e.activation(out=Hbf[key].t[:], in_=H32[key].t[:], func=AF.Copy), reads=[H32[key]], writes=[Hbf[key]])
            dst = yfs if dr == 0 else ybs
            for (lo, hi, src) in segs:
                kb.dma("sp", dst[h * 64:(h + 1) * 64, src:src + (hi - lo)], B["yst"].t[:, lo:hi], reads=[B["yst"]])

        for i in range(NBE):
            load_shared(0, i)
            load_shared(1, NBE - 1 - i)
            for h in range(6):
                ssd_block(h, 0, i)
                ssd_block(h, 1, NBE - 1 - i)
        kb.barrier()
        dsk = sbn("dsk", [128, 3], F32)
        ng = sbn("ng", [128, 3], F32)
        ones384 = sbn("ones384", [128, 128], BF16)
        epsf = sbn("epsf", [128, 1], F32)
        kb.dma("sp", dsk.t[:], dsk_in, writes=[dsk])
        kb.dma("sp", ng.t[:], ng_in, writes=[ng])
        kb.op("pool", lambda e: e.memset(ones384.t[:], 1.0 / 384.0), writes=[ones384])
        kb.op("pool", lambda e: e.memset(epsf.t[:], 1e-6), writes=[epsf])
        fy = sbn("fy", [128, 3, TB], F32)
        fb = sbn("fb", [128, 3, TB], F32)
        fx = sbn("fx", [128, 3, TB], F32)
        fz = sbn("fz", [128, 3, TB], F32)
        fsq = sbn("fsq", [128, 3, TB], BF16)
        frs = sbn("frs", [128, TB], F32)
        v3 = lambda ap: ap.rearrange("(c p) t -> p c t", p=128)
        for bi in range(NBE):
            cols = slice(bi * TB, (bi + 1) * TB)
            kb.dma("sp", fy.t[:], v3(yfs[:, cols]), writes=[fy])
            kb.dma("sp", fb.t[:], v3(ybs[:, cols]), writes=[fb])
            kb.dma("sp", fx.t[:], v3(cT[0:384, cols]), writes=[fx])
            kb.dma("sp", fz.t[:], v3(pT[0:384, cols]), writes=[fz])
            for c in range(3):
                kb.op("dve", lambda e: e.tensor_tensor(out=fy.t[:, c, :], in0=fy.t[:, c, :], in1=fb.t[:, c, :], op=ALU.add),
                      reads=[fy, fb], writes=[fy])
                kb.op("dve", lambda e: e.scalar_tensor_tensor(out=fy.t[:, c, :], in0=fx.t[:, c, :], scalar=dsk.t[:, c:c + 1], in1=fy.t[:, c, :],
                                                              op0=ALU.mult, op1=ALU.add), reads=[fx, dsk, fy], writes=[fy])
                kb.op("act", lambda e: e.activation(out=fz.t[:, c, :], in_=fz.t[:, c, :], func=AF.Silu), reads=[fz], writes=[fz])
                kb.op("dve", lambda e: e.tensor_tensor(out=fy.t[:, c, :], in0=fy.t[:, c, :], in1=fz.t[:, c, :], op=ALU.mult),
                      reads=[fy, fz], writes=[fy])
                kb.op("act", lambda e: e.activation(out=fsq.t[:, c, :], in_=fy.t[:, c, :], func=AF.Square), reads=[fy], writes=[fsq])
            for c in range(3):
                kb.op("pe", lambda e: e.matmul(P[0].t[:, 0:TB], ones384.t[:], fsq.t[:, c, :], start=(c == 0), stop=(c == 2)),
                      reads=[ones384, fsq], writes=[P[0]], sig=(c == 2))
            kb.op("act", lambda e: e.activation(out=frs.t[:], in_=P[0].t[:, 0:TB], func=AF.Sqrt, bias=epsf.t[:, 0:1]),
                  reads=[P[0], epsf], writes=[frs])
            kb.op("dve", lambda e: e.reciprocal(out=frs.t[:], in_=frs.t[:]), reads=[frs], writes=[frs])
            for c in range(3):
                kb.op("dve", lambda e: e.scalar_tensor_tensor(out=fy.t[:, c, :], in0=fy.t[:, c, :], scalar=ng.t[:, c:c + 1], in1=frs.t[:],
                                                              op0=ALU.mult, op1=ALU.mult), reads=[fy, ng, frs], writes=[fy])
            kb.dma("sp", v3(cat_out[0:384, cols]), fy.t[:], reads=[fy], is_out=True)
        kb.barrier()


def emit_fnet(kb, nc, pT, fg_in, fconst, cat_out, P):
    with ExitStack() as st:
        sbn = lambda n, sh, dt: kb.sb_in(st, "fn_" + n, sh, dt)
        unT = sbn("unT", [128, SE], BF16)
        g_sb = sbn("g", [128, 1], F32)
        bd = sbn("bd", [128, 128], BF16)
        ccsc = sbn("ccsc", [128, 256], BF16)
        f64 = sbn("f64", [64, 2, 128], BF16)
        tw = sbn("tw", [128, 2, 64], F32)
        f128 = sbn("f128", [128, 2, 128], BF16)
        c256 = sbn("c256", [128, 2, 2, 256], BF16)
        eps = sbn("eps", [128, 1], F32)
        kb.dma("sp", g_sb.t[:], fg_in, writes=[g_sb])
        kb.dma("pool", bd.t[:], fconst["bd"], writes=[bd])
        kb.dma("pool", ccsc.t[:], fconst["ccsc"], writes=[ccsc])
        kb.dma("pool", f64.t[:], fconst["f64"], writes=[f64])
        kb.dma("sp", tw.t[:], fconst["tw"], writes=[tw])
        kb.dma("pool", f128.t[:], fconst["f128"], writes=[f128])
        kb.dma("pool", c256.t[:], fconst["c256"], writes=[c256])
        kb.op("pool", lambda e: e.memset(eps.t[:], 1e-6), writes=[eps])
        fin = [sbn("fin%d" % i, [128, TB], F32) for i in range(2)]
        fsq = sbn("fsq", [128, TB], BF16)
        frs = sbn("frs", [128, TB], F32)
        for bi in range(NBE):
            cols = slice(bi * TB, (bi + 1) * TB)
            f_ = fin[bi % 2]
            kb.dma("sp", f_.t[:], pT[1024:1152, cols], writes=[f_])
            kb.op("act", lambda e: e.activation(out=fsq.t[:], in_=f_.t[:], func=AF.Square), reads=[f_], writes=[fsq])
            kb.op("pe", lambda e: e.matmul(P[0].t[:, 0:TB], bd.t[:], fsq.t[:], start=True, stop=True), reads=[bd, fsq], writes=[P[0]])
            kb.op("act", lambda e: e.activation(out=frs.t[:], in_=P[0].t[:, 0:TB], func=AF.Sqrt, bias=eps.t[:, 0:1]),
                  reads=[P[0], eps], writes=[frs])
            kb.op("dve", lambda e: e.reciprocal(out=frs.t[:], in_=frs.t[:]), reads=[frs], writes=[frs])
            kb.op("dve", lambda e: e.scalar_tensor_tensor(out=unT.t[:, cols], in0=f_.t[:], scalar=g_sb.t[:, 0:1], in1=frs.t[:],
                                                          op0=ALU.mult, op1=ALU.mult), reads=[f_, g_sb, frs], writes=[unT])
        Actx = sbn("Actx", [128, 2, 2, 128], BF16)
        octx = sbn("octx", [128, 256], F32)
        for tile in range(2):
            kb.op("pe", lambda e: e.matmul(P[1].t[:, 0:256], unT.t[:, tile * 128:(tile + 1) * 128], ccsc.t[:, :],
                                           start=True, stop=True), reads=[unT, ccsc], writes=[P[1]])
            kb.op("act", lambda e: e.activation(
                out=Actx.t[:, tile, :, :].rearrange("p a (g c) -> p g a c", g=2),
                in_=P[1].t[:, 0:256].rearrange("p (g a c) -> p g a c", g=2, a=2), func=AF.Copy), reads=[P[1]], writes=[Actx])
        k = 0
        for tile in range(2):
            for ab in range(2):
                kb.op("pe", lambda e: e.matmul(P[2].t[:, 0:256], Actx.t[:, tile, ab, :], c256.t[:, tile, ab, :], start=(k == 0), stop=(k == 3)),
                      reads=[Actx, c256], writes=[P[2]], sig=(k == 3))
                k += 1
        kb.op("act", lambda e: e.activation(out=octx.t[:], in_=P[2].t[:, 0:256], func=AF.Copy), reads=[P[2]], writes=[octx])
        kb.dma("sp", cat_out[384:512, 0:256], octx.t[:], reads=[octx], is_out=True)
        Y = sbn("Y", [64, 2, 2, 64, 128], BF16)
        Zp = sbn("Zp", [128, 2, 64, 128], BF16)
        zs = [sbn("zs%d" % i, [128, 4, 2, 64], F32) for i in range(2)]
        ta = [sbn("ta%d" % i, [128, 4, 64], F32) for i in range(4)]
        ost = sbn("ost", [128, 8192], F32)
        unL = unT.t[:, 256:SE].rearrange("p (t1 t2) -> p t2 t1", t2=128)
        for t2 in range(128):
            pz = P[3 + (t2 % 2)]
            kb.op("pe", lambda e: e.matmul(pz.t[0:64, 0:256], unL[:, t2, :], ccsc.t[:, :], start=True, stop=True),
                  reads=[unT, ccsc], writes=[pz])
            src = pz.t[0:64, 0:256].rearrange("p (g a c) -> p g a c", g=2, a=2)
            dst = Y.t[:, :, :, :, t2].rearrange("p a g c -> p g a c")
            if t2 % 2 == 0:
                kb.op("act", lambda e: e.activation(out=dst, in_=src, func=AF.Copy), reads=[pz], writes=[Y])
            else:
                kb.op("dve", lambda e: e.tensor_copy(out=dst, in_=src), reads=[pz], writes=[Y])
        tcb = tw.t[:, 0, :].unsqueeze(1).broadcast_to([128, 4, 64])
        tsb = tw.t[:, 1, :].unsqueeze(1).broadcast_to([128, 4, 64])
        for b4 in range(32):
            pz = P[3 + (b4 % 2)]
            z_ = zs[b4 % 2]
            for q in range(4):
                gc = b4 * 4 + q
                g, c = gc // 64, gc % 64
                kb.op("pe", lambda e: e.matmul(pz.t[:, q * 128:(q + 1) * 128], Y.t[:, 0, g, c, :], f64.t[:, 0, :], start=True, stop=False),
                      reads=[Y, f64], writes=[pz], sig=False)
                kb.op("pe", lambda e: e.matmul(pz.t[:, q * 128:(q + 1) * 128], Y.t[:, 1, g, c, :], f64.t[:, 1, :], start=False, stop=True),
                      reads=[Y, f64], writes=[pz], sig=(q == 3))
            kb.op("act", lambda e: e.activation(out=z_.t[:], in_=pz.t[:, 0:512].rearrange("p (q r t) -> p q r t", q=4, r=2), func=AF.Copy),
                  reads=[pz], writes=[z_])
            zr, zi = z_.t[:, :, 0, :], z_.t[:, :, 1, :]
            gsl = slice(b4 * 4, b4 * 4 + 4)
            kb.op("dve", lambda e: e.tensor_tensor(out=ta[0].t[:], in0=zr, in1=tcb, op=ALU.mult), reads=[z_, tw], writes=[ta[0]])
            kb.op("dve", lambda e: e.tensor_tensor(out=ta[1].t[:], in0=zi, in1=tsb, op=ALU.mult), reads=[z_, tw], writes=[ta[1]])
            kb.op("dve", lambda e: e.tensor_tensor(out=Zp.t[:, 0, :, gsl].rearrange("p t g -> p g t"), in0=ta[0].t[:], in1=ta[1].t[:], op=ALU.add),
                  reads=[ta[0], ta[1]], writes=[Zp])
            kb.op("pool", lambda e: e.tensor_tensor(out=ta[2].t[:], in0=zi, in1=tcb, op=ALU.mult), reads=[z_, tw], writes=[ta[2]])
            kb.op("pool", lambda e: e.tensor_tensor(out=ta[3].t[:], in0=zr, in1=tsb, op=ALU.mult), reads=[z_, tw], writes=[ta[3]])
            kb.op("pool", lambda e: e.tensor_tensor(out=Zp.t[:, 1, :, gsl].rearrange("p t g -> p g t"), in0=ta[2].t[:], in1=ta[3].t[:],
                                                    op=ALU.subtract), reads=[ta[2], ta[3]], writes=[Zp])
        ostv = ost.t[:].rearrange("p (t2 t1) -> p t1 t2", t1=64)
        for b4 in range(16):
            pz = P[3 + (b4 % 2)]
            for q in range(4):
                t1p = b4 * 4 + q
                kb.op("pe", lambda e: e.matmul(pz.t[:, q * 128:(q + 1) * 128], Zp.t[:, 0, t1p, :], f128.t[:, 0, :], start=True, stop=False),
                      reads=[Zp, f128], writes=[pz], sig=False)
                kb.op("pe", lambda e: e.matmul(pz.t[:, q * 128:(q + 1) * 128], Zp.t[:, 1, t1p, :], f128.t[:, 1, :], start=False, stop=True),
                      reads=[Zp, f128], writes=[pz], sig=(q == 3))
            kb.op("act", lambda e: e.activation(out=ostv[:, b4 * 4:b4 * 4 + 4, :], in_=pz.t[:, 0:512].rearrange("p (q t) -> p q t", q=4),
                                                func=AF.Copy), reads=[pz], writes=[ost])
        for i in range(4):
            kb.dma("sp", cat_out[384:512, 256 + i * 2048:256 + (i + 1) * 2048], ost.t[:, i * 2048:(i + 1) * 2048], reads=[ost], is_out=True)
        kb.barrier()


def build_stageB_odd(parts=("inproj", "conv", "ssd", "fnet")):
    nc = bass.Bass("TRN2", target_bir_lowering=False)
    xT = _din(nc, "xT", [1024, SE]).rearrange("(c p) t -> p c t", p=128)
    w_in = _din(nc, "w_in", [1024, FM_O]).rearrange("(c p) n -> p c n", p=128)
    mods = _din(nc, "mods", [128, 2, 6, 8])
    convw = _din(nc, "convw", [128, 5, 5])
    convb = _din(nc, "convb", [128, 5])
    dtb = _din(nc, "dtb", [128, 12])
    alog = _din(nc, "alog", [128, 12])
    dsk = _din(nc, "dsk", [128, 3])
    ng = _din(nc, "ng", [128, 3])
    masks = _din(nc, "masks", [128, 2, 128])
    ident = _din(nc, "ident", [128, 128])
    fg = _din(nc, "fg", [128, 1])
    fconst = dict(bd=_din(nc, "fc_bd", [128, 128]), ccsc=_din(nc, "fc_ccsc", [128, 256]), f64=_din(nc, "fc_f64", [64, 2, 128]),
                  tw=_din(nc, "fc_tw", [128, 2, 64]), f128=_din(nc, "fc_f128", [128, 2, 128]), c256=_din(nc, "fc_c256", [128, 2, 2, 256]))
    cat = _dout(nc, "cat", [512, SE])
    pT = nc.dram_tensor("pT", [FM_O, SE], F32, kind="Internal").ap()
    cT = nc.dram_tensor("cT", [640, SE], F32, kind="Internal").ap()
    yfs = nc.dram_tensor("yfs", [384, SE], F32, kind="Internal").ap()
    ybs = nc.dram_tensor("ybs", [384, SE], F32, kind="Internal").ap()
    with ExitStack() as st:
        kb = KB(nc, st)
        P = [kb.ps("P%d" % i, [128, 512]) for i in range(7)]
        mods_sb = kb.sb("modsb", [128, 2, 6, 8], F32)
        kb.dma("sp", mods_sb.t[:], mods, writes=[mods_sb])
        if "inproj" in parts:
            emit_inproj(kb, nc, xT, w_in, mods_sb, 0, pT, None, FM_O, 0, P)
        if "conv" in parts:
            emit_conv(kb, nc, pT, cT, convw, convb, P)
        if "ssd" in parts:
            emit_ssd(kb, nc, pT, cT, dtb, alog, dsk, ng, masks, ident, yfs, ybs, cat, P)
        if "fnet" in parts:
            emit_fnet(kb, nc, pT, fg, fconst, cat, P)
        kb.finish()
        print("stageB_odd instructions:", kb.n_inst, dict(kb.cnt))
    return nc


def const_fnet():
    c = np.arange(64)
    ang = 2 * np.pi * np.outer(c, c) / 64.0
    C64, S64 = np.cos(ang), np.sin(ang)
    ccsc = np.concatenate([C64, S64], 1) / 8.0
    z_ = np.zeros_like(ccsc)
    ccsc = np.concatenate([np.concatenate([ccsc, z_], 1), np.concatenate([z_, ccsc], 1)], 0)
    f64 = np.stack([np.concatenate([C64, -S64], 1), np.concatenate([-S64, -C64], 1)], 1) / 8.0
    t2 = np.arange(128)
    phi = 2 * np.pi * np.outer(t2, c) / 8192.0
    tw = np.stack([np.cos(phi), np.sin(phi)], 1)
    psi = 2 * np.pi * np.outer(t2, t2) / 128.0
    f128 = np.stack([np.cos(psi), np.sin(psi)], 1) / np.sqrt(128.0)
    t = np.arange(256)
    a256 = 2 * np.pi * np.outer(t, t) / 256.0
    c256 = np.stack([np.cos(a256), -np.sin(a256)], 1) / 16.0
    c256 = c256.reshape(2, 128, 2, 256).transpose(1, 0, 2, 3)
    bd = np.zeros((128, 128))
    bd[:64, :64] = 1.0 / 64.0
    bd[64:, 64:] = 1.0 / 64.0
    f32 = lambda a: np.ascontiguousarray(a, np.float32)
    return dict(fc_bd=f32(bd), fc_ccsc=f32(ccsc), fc_f64=f32(f64), fc_tw=f32(tw), fc_f128=f32(f128), fc_c256=f32(c256))


def odd_core_cols(g):
    o_z, o_x, o_b, o_c, o_dtf, o_dtb, o_f = np.cumsum([0, 768, 768, 256, 256, 12, 12])
    cols = list(range(o_z + g * 384, o_z + (g + 1) * 384)) + list(range(o_x + g * 384, o_x + (g + 1) * 384))
    cols += list(range(o_b + g * 128, o_b + (g + 1) * 128)) + list(range(o_c + g * 128, o_c + (g + 1) * 128))
    cols += list(range(o_f + g * 128, o_f + (g + 1) * 128))
    cols += list(range(o_dtf + g * 6, o_dtf + (g + 1) * 6)) + list(range(o_dtb + g * 6, o_dtb + (g + 1) * 6))
    assert len(cols) == FM_O
    return np.array(cols)


def odd_core_inputs(g, xT_b, mods_b, w_in, conv_w, conv_b, dt_bias, a_log, d_skip, ssd_g, fnet_g, consts):
    ch = np.concatenate([768 * 0 + np.arange(g * 384, (g + 1) * 384), 768 + np.arange(g * 128, (g + 1) * 128),
                         1024 + np.arange(g * 128, (g + 1) * 128)])
    cw = conv_w[:, ch].reshape(5, 5, 128).transpose(2, 1, 0)
    cb = conv_b[ch].reshape(5, 128).T
    rep = lambda v: np.ascontiguousarray(np.broadcast_to(np.asarray(v, np.float32)[None, :], (128, len(v))))
    dtb = rep(np.concatenate([dt_bias[0][g * 6:(g + 1) * 6], dt_bias[1][g * 6:(g + 1) * 6]]))
    alog = rep(np.concatenate([a_log[0][g * 6:(g + 1) * 6], a_log[1][g * 6:(g + 1) * 6]]))
    dsk = np.repeat(d_skip[g * 6:(g + 1) * 6], 64).reshape(3, 128).T
    ng = ssd_g[g * 384:(g + 1) * 384].reshape(3, 128).T
    fg = fnet_g[g * 128:(g + 1) * 128].reshape(128, 1)
    f32 = lambda a: np.ascontiguousarray(a, np.float32)
    im = dict(xT=xT_b, w_in=f32(w_in[:, odd_core_cols(g)]), mods=mods_b, convw=f32(cw), convb=f32(cb), dtb=dtb, alog=alog,
              dsk=f32(dsk), ng=f32(ng), masks=consts["masks"], ident=consts["ident"], fg=f32(fg))
    im.update(consts["fnet"])
    return im


def build_adaln():
    nc = bass.Bass("TRN2", target_bir_lowering=False)
    cin = _din(nc, "cin", [128, 8, 5])
    w = _din(nc, "w", [1024, 3072]).rearrange("(c p) n -> p c n", p=128)
    b = _din(nc, "b", [128, 24])
    out = _dout(nc, "out", [128, 24, 5])
    with ExitStack() as st:
        kb = KB(nc, st)
        P = [kb.ps("P%d" % i, [128, 512]) for i in range(4)]
        c_sb = kb.sb("c", [128, 8, 5], F32)
        b_sb = kb.sb("b", [128, 24], F32)
        o_sb = kb.sb("o", [128, 24, 5], F32)
        w_sb = [kb.sb("w%d" % i, [128, 8, 768], F32) for i in range(4)]
        kb.dma("sp", c_sb.t[:], cin, writes=[c_sb])
        kb.dma("sp", b_sb.t[:], b, writes=[b_sb])
        for i in range(4):
            kb.dma("sp", w_sb[i].t[:], w[:, :, i * 768:(i + 1) * 768], writes=[w_sb[i]])
        kb.op("act", lambda e: e.activation(out=c_sb.t[:], in_=c_sb.t[:], func=AF.Silu), reads=[c_sb], writes=[c_sb])
        for j in range(24):
            pj = P[j % 4]
            wi, wo = j // 6, (j % 6) * 128
            for k in range(8):
                kb.op("pe", lambda e, k=k: e.matmul(pj.t[:, 0:5], w_sb[wi].t[:, k, wo:wo + 128], c_sb.t[:, k, :], start=(k == 0), stop=(k == 7)),
                      reads=[w_sb[wi], c_sb], writes=[pj], sig=(k == 7))
            kb.op("dve", lambda e: e.tensor_scalar(out=o_sb.t[:, j, :], in0=pj.t[:, 0:5], scalar1=b_sb.t[:, j:j + 1], scalar2=None, op0=ALU.add),
                  reads=[pj, b_sb], writes=[o_sb])
        kb.dma("sp", out, o_sb.t[:], reads=[o_sb], is_out=True)
        kb.finish()
    return nc


_PROGS = {}


def _prog(name, fn):
    if name not in _PROGS:
        _PROGS[name] = fn()
    return _PROGS[name]


def kernel_unfused(x, c, ctx, c_ctx, ada_w, ada_b, ln_g, ln_b, ev_w_in, ev_w_o, gla_w_decay, gla_b_decay, gla_norm_g, gqa_q_norm_g,
           gqa_k_norm_g, od_w_in, od_w_o, ssd_conv_w, ssd_conv_b, ssd_dt_bias, ssd_a_log, ssd_d, ssd_norm_g, fnet_norm_g,
           router_w, router_b, exp_w_gate, exp_w_up, exp_w_down):
    f32 = lambda a: np.ascontiguousarray(np.asarray(a), np.float32)
    x, c, ctx, c_ctx = f32(x), f32(c), f32(ctx), f32(c_ctx)
    ada_w, ada_b = f32(ada_w), f32(ada_b)
    NB, DEPTH = 4, 4
    cores = list(range(8))
    call = np.concatenate([c, c_ctx[None, :]], 0)
    cin = np.ascontiguousarray(call.reshape(5, 8, 128).transpose(2, 1, 0))
    ims = []
    for core in cores:
        layer, half = core // 2, core % 2
        ims.append(dict(cin=cin, w=np.ascontiguousarray(ada_w[layer][:, half * 3072:(half + 1) * 3072]),
                        b=np.ascontiguousarray(ada_b[layer][half * 3072:(half + 1) * 3072].reshape(24, 128).T)))
    res = run_bass_kernel_spmd(_prog("adaln", build_adaln), ims, core_ids=cores)
    mods_all = np.zeros((DEPTH, 5, 6144), np.float32)
    for core in cores:
        layer, half = core // 2, core % 2
        o = np.asarray(res.results[core]["out"])
        mods_all[layer][:, half * 3072:(half + 1) * 3072] = o.transpose(2, 1, 0).reshape(5, 3072)
    XT = [np.ascontiguousarray(np.concatenate([ctx[b], x[b]], 0).T) for b in range(NB)]
    cosT, sinT = const_rope_tables()
    consts = dict(masks=const_masks(), ident=np.eye(128, dtype=np.float32), ropeR=const_ropeR(), cosT=cosT, sinT=sinT,
                  fnet=const_fnet())
    sel = const_sel()
    rbias = np.ascontiguousarray(np.broadcast_to(f32(router_b)[None, :], (128, 16)))
    for layer in range(DEPTH):
        i = layer // 2
        mods = [pack_mods(mods_all[layer][4], mods_all[layer][b]) for b in range(NB)]
        ims = []
        for core in cores:
            b, g = core // 2, core % 2
            if layer % 2 == 0:
                ims.append(even_core_inputs(g, XT[b], mods[b], f32(ev_w_in[i]), f32(gla_w_decay[i]), f32(gla_b_decay[i]),
                                            f32(gla_norm_g[i]), f32(gqa_q_norm_g[i]), f32(gqa_k_norm_g[i]), consts))
            else:
                ims.append(odd_core_inputs(g, XT[b], mods[b], f32(od_w_in[i]), f32(ssd_conv_w[i]), f32(ssd_conv_b[i]),
                                           f32(ssd_dt_bias[i]), f32(ssd_a_log[i]), f32(ssd_d[i]), f32(ssd_norm_g[i]),
                                           f32(fnet_norm_g[i]), consts))
        if layer % 2 == 0:
            res = run_bass_kernel_spmd(_prog("B_even", build_stageB_even), ims, core_ids=cores)
        else:
            res = run_bass_kernel_spmd(_prog("B_odd", build_stageB_odd), ims, core_ids=cores)
        CAT = []
        for b in range(NB):
            cat = np.empty((1024, SE), np.float32)
            for g in range(2):
                cg = np.asarray(res.results[2 * b + g]["cat"])
                if layer % 2 == 0:
                    cat[g * 256:(g + 1) * 256] = cg[0:256]
                    cat[512 + g * 256:512 + (g + 1) * 256] = cg[256:512]
                else:
                    cat[g * 384:(g + 1) * 384] = cg[0:384]
                    cat[768 + g * 128:768 + (g + 1) * 128] = cg[384:512]
            CAT.append(cat)
        w_o = f32(ev_w_o[i]) if layer % 2 == 0 else f32(od_w_o[i])
        lnp = np.ascontiguousarray(np.stack([pack_vec(f32(ln_g[layer])), pack_vec(f32(ln_b[layer]))], 1))
        wg, wu, wd = f32(exp_w_gate[layer]), f32(exp_w_up[layer]), f32(exp_w_down[layer])
        ims = []
        for core in cores:
            b, h = core // 2, core % 2
            colsel = np.concatenate([np.arange(h * 128, (h + 1) * 128), 256 + np.arange(h * 4096, (h + 1) * 4096)])
            ims.append(dict(catT=np.ascontiguousarray(CAT[b][:, colsel]), xT=np.ascontiguousarray(XT[b][:, colsel]), w_o=w_o,
                            mods=mods[b], lnp=lnp, rw=f32(router_w), rbias=rbias, sel=sel, ident=consts["ident"],
                            wg=wg, wu=wu, wd=wd))
        res = run_bass_kernel_spmd(_prog("C", build_stageC), ims, core_ids=cores)
        for core in cores:
            b, h = core // 2, core % 2
            colsel = np.concatenate([np.arange(h * 128, (h + 1) * 128), 256 + np.arange(h * 4096, (h + 1) * 4096)])
            XT[b][:, colsel] = np.asarray(res.results[core]["xo"])
    return np.ascontiguousarray(np.stack([XT[b][:, 256:].T for b in range(NB)], 0)).astype(np.float32)


EVEN_CHUNK_ROWS = [0, 128, 512, 640, 256, 384, 768, 896]
ODD_CHUNK_ROWS = [0, 128, 256, 512, 640, 768, 384, 896]
PAIRS = [[0, 1], [2, 3], [4, 5], [6, 7]]


def _gsegs(lo, hi):
    out = []
    for (a, b, j, t0) in ((0, 128, 0, 0), (128, 256, 1, 0), (256, 4352, 0, 128), (4352, 8448, 1, 128)):
        s, e = max(lo, a), min(hi, b)
        if s < e:
            out.append((s, e, j, t0 + s - a))
    return out


def _csegs(bi, j):
    if bi == 0:
        return [(0, 128, j * 128), (128, TB, 256 + j * 4096)]
    return [(0, TB, 256 + j * 4096 + bi * TB - 128)]


def emit_adaln(kb, nc, cin2, ada_w, ada_b48, mods_bufs, P):
    with ExitStack() as st:
        c_sb = kb.sb_in(st, "ad_c", [128, 8, 2], F32)
        b_sb = kb.sb_in(st, "ad_b", [128, 4, 48], F32)
        wt = [kb.sb_in(st, "ad_w%d" % i, [128, 8, 768], F32) for i in range(2)]
        kb.dma("sp", c_sb.t[:], cin2, writes=[c_sb])
        for L in range(4):
            kb.dma("sp", b_sb.t[:, L, :], ada_b48[L], writes=[b_sb])
        kb.op("act", lambda e: e.activation(out=c_sb.t[:], in_=c_sb.t[:], func=AF.Silu), reads=[c_sb], writes=[c_sb])
        n = 0
        for L in range(len(mods_bufs)):
            wv = ada_w[L].rearrange("(c p) n -> p c n", p=128)
            for piece in range(8):
                w_ = wt[n % 2]
                n += 1
                kb.dma("sp", w_.t[:], wv[:, :, piece * 768:(piece + 1) * 768], writes=[w_])
                for jj in range(6):
                    j = piece * 6 + jj
                    m, c = divmod(j, 8)
                    pj = P[jj % 2]
                    for k in range(8):
                        kb.op("pe", lambda e, k=k: e.matmul(pj.t[:, 0:2], w_.t[:, k, jj * 128:(jj + 1) * 128], c_sb.t[:, k, :],
                                                            start=(k == 0), stop=(k == 7)), reads=[w_, c_sb], writes=[pj], sig=(k == 7))
                    kb.op("dve", lambda e: e.tensor_scalar(out=mods_bufs[L].t[:, :, m, c], in0=pj.t[:, 0:2], scalar1=b_sb.t[:, L, j:j + 1],
                                                           scalar2=None, op0=ALU.add), reads=[pj, b_sb], writes=[mods_bufs[L]])
        kb.barrier()


def build_fused(depth=4):
    nc = bass.Bass("TRN2", target_bir_lowering=False)
    I = {}

    def din(name, shape):
        I[name] = _din(nc, name, shape)
        return I[name]

    cin2 = din("cin2", [128, 8, 2])
    ada_w = din("ada_w", [4, 1024, 6144])
    ada_b48 = din("ada_b48", [4, 128, 48])
    XTg0 = din("XTg0", [2048, 4224])
    xown0 = din("xown0", [1024, 4224])
    msel_in = din("msel", [128, 2])
    masks = din("masks", [128, 2, 128])
    ident = din("ident", [128, 128])
    ropeR = din("ropeR", [64, 64])
    cosT = din("cosT", [64, 8192])
    sinT = din("sinT", [64, 8192])
    fconst = dict(bd=din("fc_bd", [128, 128]), ccsc=din("fc_ccsc", [128, 256]), f64=din("fc_f64", [64, 2, 128]),
                  tw=din("fc_tw", [128, 2, 64]), f128=din("fc_f128", [128, 2, 128]), c256=din("fc_c256", [128, 2, 2, 256]))
    rw = din("rw", [1024, 16]).rearrange("(c p) n -> p c n", p=128)
    rbias = din("rbias", [128, 16])
    sel_in = din("sel", [16, 16 * 128])
    LW = []
    for L in range(depth):
        d = {}
        if L % 2 == 0:
            d["w_in"] = din("w_in%d" % L, [1024, FM_E + TM_E]).rearrange("(c p) n -> p c n", p=128)
            d["wdec"] = din("wdec%d" % L, [16, 2, 128])
            d["bdec"] = din("bdec%d" % L, [64, 2, 2])
            d["glag"] = din("glag%d" % L, [128, 1])
            d["qkg"] = din("qkg%d" % L, [64, 2])
        else:
            d["w_in"] = din("w_in%d" % L, [1024, FM_O]).rearrange("(c p) n -> p c n", p=128)
            for nme, shp in (("convw", [128, 5, 5]), ("convb", [128, 5]), ("dtb", [128, 12]), ("alog", [128, 12]), ("dsk", [128, 3]),
                             ("ng", [128, 3]), ("fg", [128, 1])):
                d[nme] = din("%s%d" % (nme, L), shp)
        d["w_o"] = din("w_o%d" % L, [1024, 1024]).rearrange("(c p) n -> p c n", p=128)
        d["lnp"] = din("lnp%d" % L, [128, 2, 2, 8])
        d["wg"] = din("wg%d" % L, [16, 1024, 768])
        d["wu"] = din("wu%d" % L, [16, 1024, 768])
        d["wd"] = din("wd%d" % L, [16, 768, 1024])
        LW.append(d)
    xout = _dout(nc, "xout", [1024, TC])
    dint = lambda name, shape, dt=F32: nc.dram_tensor(name, list(shape), dt, kind="Internal").ap()
    pT = dint("pT", [FM_O, SE])
    vtm = dint("vtm", [SE, TM_E], BF16)
    ofs, obs = dint("ofs", [256, SE]), dint("obs", [256, SE])
    cT = dint("cT", [640, SE])
    yfs, ybs = dint("yfs", [384, SE]), dint("ybs", [384, SE])
    xmid = dint("xmid", [1024, TC]).rearrange("(c p) t -> p c t", p=128)
    catp = dint("catp", [512, SE])
    catg = dint("catg", [1024, SE])
    xo_buf = [dint("xoA", [1024, TC]), dint("xoB", [1024, TC])]
    XTg = dint("XTg", [2048, TC])
    v3 = lambda ap: ap.rearrange("(c p) t -> p c t", p=128)
    with ExitStack() as st:
        kb = KB(nc, st)
        P = [kb.ps("P%d" % i, [128, 512]) for i in range(7)]
        mods_bufs = [kb.sb("mods%d" % L, [128, 2, 6, 8], F32) for L in range(depth)]
        msel = kb.sb("msel", [128, 2], F32)
        kb.dma("sp", msel.t[:], msel_in, writes=[msel])
        kb.pfx = "ad_"
        emit_adaln(kb, nc, cin2, ada_w, ada_b48, mods_bufs, P)
        catg_tok = Buf(None, "catg_tok")
        xtg_tok = Buf(None, "xtg_tok")
        for L in range(depth):
            W = LW[L]
            xsrc = XTg0 if L == 0 else XTg
            xv = xsrc.rearrange("(c ph s i) t -> ph s i c t", ph=2, s=2, i=64)

            def load_x(bi, xi, xv=xv):
                for (s, e, j, t0) in _gsegs(bi * TB, (bi + 1) * TB):
                    for ph in range(2):
                        kb.dma("sp", xi.t[ph * 64:(ph + 1) * 64, :, s - bi * TB:e - bi * TB], xv[ph, j][:, :, t0:t0 + (e - s)],
                               reads=[xtg_tok], writes=[xi])

            kb.pfx = "L%dB_" % L
            if L % 2 == 0:
                emit_inproj(kb, nc, None, W["w_in"], mods_bufs[L], 0, pT, vtm, FM_E, TM_E, P, load_x=load_x)
                emit_gla(kb, nc, pT, vtm, W["wdec"], W["bdec"], W["glag"], masks, ident, ofs, obs, catp, P)
                emit_gqa(kb, nc, pT, vtm, W["qkg"], ropeR, cosT, sinT, catp, P)
                rows = EVEN_CHUNK_ROWS
            else:
                emit_inproj(kb, nc, None, W["w_in"], mods_bufs[L], 0, pT, None, FM_O, 0, P, load_x=load_x)
                emit_conv(kb, nc, pT, cT, W["convw"], W["convb"], P)
                emit_ssd(kb, nc, pT, cT, W["dtb"], W["alog"], W["dsk"], W["ng"], masks, ident, yfs, ybs, catp, P)
                emit_fnet(kb, nc, pT, W["fg"], fconst, catp, P)
                rows = ODD_CHUNK_ROWS
            for k in range(16):
                kb.collective("AllGather", catp[k * 32:(k + 1) * 32, :], catg[k * 64:(k + 1) * 64, :], PAIRS, writes=[catg_tok])

            def load_cat(bi, catb, r, vf, rows=rows):
                for j, dst in ((0, r), (1, vf)):
                    for c in range(8):
                        srank, rho0 = rows[c] // 512, rows[c] % 512
                        for (lo, hi, gc) in _csegs(bi, j):
                            for q in range(4):
                                g0 = (rho0 // 32 + q) * 64 + srank * 32
                                kb.dma("sp", dst.t[q * 32:(q + 1) * 32, c, lo:hi], catg[g0:g0 + 32, gc:gc + (hi - lo)],
                                       reads=[catg_tok], writes=[dst])
                kb.op("dve", lambda e: e.tensor_scalar(out=r.t[:], in0=r.t[:], scalar1=msel.t[:, 0:1], scalar2=None, op0=ALU.mult),
                      reads=[r, msel], writes=[r])
                kb.op("dve", lambda e: e.scalar_tensor_tensor(out=catb.t[:], in0=vf.t[:], scalar=msel.t[:, 1:2], in1=r.t[:],
                                                              op0=ALU.mult, op1=ALU.add), reads=[vf, msel, r], writes=[catb])

            last = L == depth - 1
            xo_ap = xout if last else xo_buf[L % 2]
            xin_ap = xown0 if L == 0 else xo_buf[(L - 1) % 2]
            D = dict(xT=v3(xin_ap), xo=v3(xo_ap), w_o=W["w_o"], lnp=W["lnp"], rw=rw, rbias=rbias, sel=sel_in, ident=ident,
                     wg=W["wg"], wu=W["wu"], wd=W["wd"], xmid=xmid)
            kb.pfx = "L%dC_" % L
            with ExitStack() as stc:
                p7 = Buf(stc.enter_context(nc.psum_tensor("ps_L%dC_P7" % L, [128, 512], F32)), "P7")
                xo_tok = Buf(None, "xo_tok")
                emit_stageC(kb, nc, P + [p7], D, mods_buf=mods_bufs[L], load_cat=load_cat, xo_tok=xo_tok)
            if not last:
                for k in range(16):
                    kb.collective("AllGather", xo_ap[k * 64:(k + 1) * 64, :], XTg[k * 128:(k + 1) * 128, :], PAIRS,
                                  reads=[xo_tok], writes=[xtg_tok])
        kb.finish()
        print("fused instructions:", kb.n_inst, dict(kb.cnt))
    return nc


def kernel(x, c, ctx, c_ctx, ada_w, ada_b, ln_g, ln_b, ev_w_in, ev_w_o, gla_w_decay, gla_b_decay, gla_norm_g, gqa_q_norm_g,
                 gqa_k_norm_g, od_w_in, od_w_o, ssd_conv_w, ssd_conv_b, ssd_dt_bias, ssd_a_log, ssd_d, ssd_norm_g, fnet_norm_g,
                 router_w, router_b, exp_w_gate, exp_w_up, exp_w_down, _depth=4):
    f32 = lambda a: np.ascontiguousarray(np.asarray(a), np.float32)
    x, c, ctx, c_ctx = f32(x), f32(c), f32(ctx), f32(c_ctx)
    NB, DEPTH = 4, _depth
    cores = list(range(8))
    cosT, sinT = const_rope_tables()
    shared = dict(ada_w=f32(ada_w), ada_b48=np.ascontiguousarray(f32(ada_b).reshape(4, 48, 128).transpose(0, 2, 1)),
                  masks=const_masks(), ident=np.eye(128, dtype=np.float32), ropeR=const_ropeR(), cosT=cosT, sinT=sinT,
                  rw=f32(router_w), rbias=np.ascontiguousarray(np.broadcast_to(f32(router_b)[None, :], (128, 16))), sel=const_sel())
    shared.update(const_fnet())
    for L in range(DEPTH):
        i = L // 2
        shared["w_o%d" % L] = f32(ev_w_o[i]) if L % 2 == 0 else f32(od_w_o[i])
        shared["lnp%d" % L] = np.ascontiguousarray(np.stack([pack_vec(f32(ln_g[L])), pack_vec(f32(ln_b[L]))], 1))
        shared["wg%d" % L], shared["wu%d" % L], shared["wd%d" % L] = f32(exp_w_gate[L]), f32(exp_w_up[L]), f32(exp_w_down[L])
    dummy_c = dict(masks=None, ident=None, ropeR=None, cosT=None, sinT=None, fnet={})
    ims = []
    for core in cores:
        b, r = core // 2, core % 2
        im = dict(shared)
        cc = np.stack([c_ctx, c[b]], 0)
        im["cin2"] = np.ascontiguousarray(cc.reshape(2, 8, 128).transpose(2, 1, 0))
        halves = [np.concatenate([ctx[b][j * 128:(j + 1) * 128], x[b][j * 4096:(j + 1) * 4096]], 0).T for j in range(2)]
        im["XTg0"] = np.ascontiguousarray(np.stack([h_.reshape(16, 64, TC) for h_ in halves], 1).reshape(2048, TC))
        im["xown0"] = np.ascontiguousarray(halves[r])
        im["msel"] = np.ascontiguousarray(np.broadcast_to(np.array([1.0 - r, float(r)], np.float32)[None, :], (128, 2)))
        for L in range(DEPTH):
            i = L // 2
            if L % 2 == 0:
                e = even_core_inputs(r, None, None, f32(ev_w_in[i]), f32(gla_w_decay[i]), f32(gla_b_decay[i]), f32(gla_norm_g[i]),
                                     f32(gqa_q_norm_g[i]), f32(gqa_k_norm_g[i]), dummy_c)
                for k in ("w_in", "wdec", "bdec", "glag", "qkg"):
                    im["%s%d" % (k, L)] = e[k]
            else:
                o = odd_core_inputs(r, None, None, f32(od_w_in[i]), f32(ssd_conv_w[i]), f32(ssd_conv_b[i]), f32(ssd_dt_bias[i]),
                                    f32(ssd_a_log[i]), f32(ssd_d[i]), f32(ssd_norm_g[i]), f32(fnet_norm_g[i]), dummy_c)
                for k in ("w_in", "convw", "convb", "dtb", "alog", "dsk", "ng", "fg"):
                    im["%s%d" % (k, L)] = o[k]
        ims.append(im)
    res = run_bass_kernel_spmd(_prog("fused%d" % DEPTH, lambda: build_fused(DEPTH)), ims, core_ids=cores)
    out = np.empty((NB, 8192, 1024), np.float32)
    for core in cores:
        b, r = core // 2, core % 2
        out[b, r * 4096:(r + 1) * 4096] = np.asarray(res.results[core]["xout"])[:, 128:].T
    return out
```

```python
import numpy as np
from contextlib import ExitStack
import concourse.bass as bass
import concourse.mybir as mybir
from concourse.bass_utils import run_bass_kernel_spmd

F32 = mybir.dt.float32
BF16 = mybir.dt.bfloat16
I32 = mybir.dt.int32
AF = mybir.ActivationFunctionType
ALU = mybir.AluOpType
AX = mybir.AxisListType


SAME_ENG_SYNC = True


class Buf:
    __slots__ = ("t", "name", "lw", "rd")

    def __init__(self, t, name):
        self.t = t
        self.name = name
        self.lw = None
        self.rd = {}


class KB:
    ENG = ("pe", "dve", "act", "pool", "sp")

    def __init__(self, nc, st, ndma=12):
        self.nc = nc
        self.st = st
        self.st0 = st
        self.pfx = ""
        self.e = {"pe": nc.tensor, "dve": nc.vector, "act": nc.scalar, "pool": nc.gpsimd, "sp": nc.sync}
        self.sem = {}
        self.cnt = {}
        for k in ("pe", "dve", "act", "pool"):
            self.sem[k] = st.enter_context(nc.semaphore("s_" + k))
            self.cnt[k] = 0
        self.dsem = [st.enter_context(nc.semaphore("d%d" % i)) for i in range(ndma)]
        self.dcnt = [0] * ndma
        self.drr = 0
        self.waited = {k: {} for k in self.ENG}
        self.out_tickets = []
        self.n_inst = 0

    def sb(self, name, shape, dt):
        return Buf(self.st.enter_context(self.nc.sbuf_tensor("sb_" + self.pfx + name, list(shape), dt)), name)

    def ps(self, name, shape, dt=F32):
        return Buf(self.st.enter_context(self.nc.psum_tensor("ps_" + self.pfx + name, list(shape), dt)), name)

    def _semobj(self, key):
        return self.sem[key] if isinstance(key, str) else self.dsem[key]

    def _wait(self, eng, key, val):
        if key == eng and (eng == "pe" or not SAME_ENG_SYNC):
            return
        w = self.waited[eng]
        if w.get(key, 0) >= val:
            return
        self.e[eng].wait_ge(self._semobj(key), val)
        w[key] = val

    def _deps(self, eng, reads, writes):
        deps = {}
        for b in reads:
            if b.lw is not None and deps.get(b.lw[0], 0) < b.lw[1]:
                deps[b.lw[0]] = b.lw[1]
        for b in writes:
            if b.lw is not None and deps.get(b.lw[0], 0) < b.lw[1]:
                deps[b.lw[0]] = b.lw[1]
            for k, v in b.rd.items():
                if deps.get(k, 0) < v:
                    deps[k] = v
        for k, v in deps.items():
            self._wait(eng, k, v)

    def _mark(self, ticket, reads, writes):
        k, v = ticket
        for b in writes:
            b.lw = ticket
            b.rd = {}
        for b in reads:
            if b.rd.get(k, 0) < v:
                b.rd[k] = v

    def op(self, eng, fn, reads=(), writes=(), sig=True):
        self._deps(eng, reads, writes)
        inst = fn(self.e[eng])
        self.n_inst += 1
        if sig:
            self.cnt[eng] += 1
            inst.then_inc(self.sem[eng], 1)
            ticket = (eng, self.cnt[eng])
        else:
            ticket = (eng, self.cnt[eng] + 1)
        self._mark(ticket, reads, writes)
        return ticket

    def dma(self, q, out, in_, reads=(), writes=(), is_out=False):
        i = self.drr
        self.drr = (self.drr + 1) % len(self.dsem)
        if self.dcnt[i] > 0:
            self._wait(q, i, self.dcnt[i])
        self._deps(q, reads, writes)
        self.dcnt[i] += 16
        self.e[q].dma_start(out=out, in_=in_).then_inc(self.dsem[i], 16)
        self.n_inst += 1
        ticket = (i, self.dcnt[i])
        self._mark(ticket, reads, writes)
        if is_out:
            self.out_tickets.append(ticket)
        return ticket

    def collective(self, kind, ins_ap, outs_ap, groups, reads=(), writes=()):
        if "cc" not in self.sem:
            self.sem["cc"] = self.st0.enter_context(self.nc.semaphore("s_cc"))
            self.cnt["cc"] = 0
        self._deps("pool", reads, writes)
        self.cnt["cc"] += 1
        self.nc.gpsimd.collective_compute(kind, ALU.bypass, replica_groups=groups, ins=[ins_ap], outs=[outs_ap]).then_inc(self.sem["cc"], 1)
        self.n_inst += 1
        ticket = ("cc", self.cnt["cc"])
        self._mark(ticket, reads, writes)
        return ticket

    def barrier(self):
        for eng in self.ENG:
            for i, c in enumerate(self.dcnt):
                if c > 0:
                    self._wait(eng, i, c)
            for k in self.cnt:
                if self.cnt[k] > 0 and k != eng:
                    self._wait(eng, k, self.cnt[k])

    def sb_in(self, st, name, shape, dt):
        return Buf(st.enter_context(self.nc.sbuf_tensor("sb_" + self.pfx + name, list(shape), dt)), name)

    def finish(self):
        for i, c in enumerate(self.dcnt):
            if c > 0:
                self._wait("sp", i, c)
        for k in self.cnt:
            if self.cnt[k] > 0:
                self._wait("sp", k, self.cnt[k])


ALPHA = (2.0 * 4) ** 0.25
LN_EPS = 1e-6
TB = 384
TC = 4224


def _din(nc, name, shape, dt=F32):
    return nc.dram_tensor(name, list(shape), dt, kind="ExternalInput").ap()


def _dout(nc, name, shape, dt=F32):
    return nc.dram_tensor(name, list(shape), dt, kind="ExternalOutput").ap()


def _segs(bi):
    return [(0, 128, 0), (128, TB, 1)] if bi == 0 else [(0, TB, 1)]


class LNCtx:
    def __init__(self, kb, pS1, pS2):
        self.kb = kb
        self.pS1, self.pS2 = pS1, pS2
        self.rb = kb.sb("ln_rb", [128, 8, TB], BF16)
        self.rsq = kb.sb("ln_rsq", [128, 8, TB], BF16)
        self.mean = kb.sb("ln_mean", [128, TB], F32)
        self.m2 = kb.sb("ln_m2", [128, TB], F32)
        self.rstd = kb.sb("ln_rstd", [128, TB], F32)
        self.nmr = kb.sb("ln_nmr", [128, TB], F32)
        self.ones = kb.sb("ln_ones", [128, 128], BF16)
        self.eps = kb.sb("ln_eps", [128, 1], F32)
        kb.op("pool", lambda e: e.memset(self.ones.t[:], 1.0 / 1024.0), writes=[self.ones])
        kb.op("pool", lambda e: e.memset(self.eps.t[:], LN_EPS / (ALPHA * ALPHA)), writes=[self.eps])

    def normalize(self, r):
        kb = self.kb
        for c in range(8):
            kb.op("act", lambda e, c=c: e.activation(out=self.rb.t[:, c, :], in_=r.t[:, c, :], func=AF.Copy),
                  reads=[r], writes=[self.rb])
            kb.op("act", lambda e, c=c: e.activation(out=self.rsq.t[:, c, :], in_=r.t[:, c, :], func=AF.Square),
                  reads=[r], writes=[self.rsq])
        for c in range(8):
            kb.op("pe", lambda e, c=c: e.matmul(self.pS1.t[:, 0:TB], self.ones.t[:], self.rb.t[:, c, :],
                                                start=(c == 0), stop=(c == 7)),
                  reads=[self.ones, self.rb], writes=[self.pS1], sig=(c == 7))
        for c in range(8):
            kb.op("pe", lambda e, c=c: e.matmul(self.pS2.t[:, 0:TB], self.ones.t[:], self.rsq.t[:, c, :],
                                                start=(c == 0), stop=(c == 7)),
                  reads=[self.ones, self.rsq], writes=[self.pS2], sig=(c == 7))
        kb.op("act", lambda e: e.activation(out=self.mean.t[:], in_=self.pS1.t[:, 0:TB], func=AF.Copy),
              reads=[self.pS1], writes=[self.mean])
        kb.op("dve", lambda e: e.tensor_tensor(out=self.m2.t[:], in0=self.mean.t[:], in1=self.mean.t[:], op=ALU.mult),
              reads=[self.mean], writes=[self.m2])
        kb.op("dve", lambda e: e.tensor_tensor(out=self.m2.t[:], in0=self.pS2.t[:, 0:TB], in1=self.m2.t[:],
                                               op=ALU.subtract),
              reads=[self.pS2, self.m2], writes=[self.m2])
        kb.op("act", lambda e: e.activation(out=self.m2.t[:], in_=self.m2.t[:], func=AF.Sqrt, bias=self.eps.t[:, 0:1]),
              reads=[self.m2, self.eps], writes=[self.m2])
        kb.op("dve", lambda e: e.reciprocal(out=self.rstd.t[:], in_=self.m2.t[:]), reads=[self.m2], writes=[self.rstd])
        kb.op("dve", lambda e: e.scalar_tensor_tensor(out=self.nmr.t[:], in0=self.mean.t[:], scalar=-1.0,
                                                      in1=self.rstd.t[:], op0=ALU.mult, op1=ALU.mult),
              reads=[self.mean, self.rstd], writes=[self.nmr])
        for c in range(8):
            kb.op("dve", lambda e, c=c: e.tensor_tensor(out=r.t[:, c, :], in0=r.t[:, c, :], in1=self.rstd.t[:],
                                                        op=ALU.mult), reads=[r, self.rstd], writes=[r])
            kb.op("dve", lambda e, c=c: e.tensor_tensor(out=r.t[:, c, :], in0=r.t[:, c, :], in1=self.nmr.t[:],
                                                        op=ALU.add), reads=[r, self.nmr], writes=[r])


def emit_stageC(kb, nc, P, D, nblk=11, sbs=(3, 3, 3, 2), nexp=16, mods_buf=None, load_cat=None, xo_tok=None):
    catT, xT, xo, w_o, mods, lnp, rw, rbias, sel_in, ident_in, wg, wu, wd, xmid_d = (
        D.get(k) for k in ("catT", "xT", "xo", "w_o", "mods", "lnp", "rw", "rbias", "sel", "ident", "wg", "wu", "wd", "xmid"))
    if xo_tok is None:
        xo_tok = Buf(None, "xo_tok")
    with ExitStack() as st:
        old_st = kb.st
        kb.st = st
        ln = LNCtx(kb, P[2], P[3])
        SBT = max(sbs) * TB
        wo_sb = kb.sb("wo", [128, 8, 1024], BF16)
        mods_sb = mods_buf if mods_buf is not None else kb.sb("mods", [128, 2, 6, 8], F32)
        lnp_sb = kb.sb("lnp", [128, 2, 2, 8], F32)
        rw_sb = kb.sb("rw", [128, 8, 16], F32)
        rb_sb = kb.sb("rbias", [128, 16], F32)
        sel_sb = kb.sb("sel", [128, 16 * 128], F32)
        kb.op("pool", lambda e: e.memset(sel_sb.t[:], 0.0), writes=[sel_sb])
        id_sb = kb.sb("ident", [128, 128], F32)
        ga = kb.sb("ga", [128, 2, 2, 8], F32)
        gp = kb.sb("gp", [128, 2, 8], F32)
        bp = kb.sb("bp", [128, 2, 8], F32)
        kb.dma("pool", wo_sb.t[:], w_o, writes=[wo_sb])
        if mods_buf is None:
            kb.dma("sp", mods_sb.t[:], mods, writes=[mods_sb])
        kb.dma("sp", lnp_sb.t[:], lnp, writes=[lnp_sb])
        kb.dma("sp", rw_sb.t[:], rw, writes=[rw_sb])
        kb.dma("sp", rb_sb.t[:], rbias, writes=[rb_sb])
        kb.dma("sp", sel_sb.t[0:16, :], sel_in, writes=[sel_sb])
        kb.dma("sp", id_sb.t[:], ident_in, writes=[id_sb])
        for w in range(2):
            kb.op("dve", lambda e, w=w: e.tensor_scalar(out=ga.t[:, 0, w, :], in0=mods_sb.t[:, w, 2, :], scalar1=1.0 / ALPHA,
                                                        scalar2=None, op0=ALU.mult), reads=[mods_sb], writes=[ga])
            kb.op("dve", lambda e, w=w: e.tensor_scalar(out=ga.t[:, 1, w, :], in0=mods_sb.t[:, w, 5, :], scalar1=1.0 / ALPHA,
                                                        scalar2=None, op0=ALU.mult), reads=[mods_sb], writes=[ga])
            kb.op("dve", lambda e, w=w: e.scalar_tensor_tensor(out=gp.t[:, w, :], in0=mods_sb.t[:, w, 4, :], scalar=1.0,
                                                               in1=lnp_sb.t[:, 0, 0, :], op0=ALU.add, op1=ALU.mult),
                  reads=[mods_sb, lnp_sb], writes=[gp])
            kb.op("dve", lambda e, w=w: e.scalar_tensor_tensor(out=bp.t[:, w, :], in0=mods_sb.t[:, w, 4, :], scalar=1.0,
                                                               in1=lnp_sb.t[:, 1, 0, :], op0=ALU.add, op1=ALU.mult),
                  reads=[mods_sb, lnp_sb], writes=[bp])
            kb.op("dve", lambda e, w=w: e.tensor_tensor(out=bp.t[:, w, :], in0=bp.t[:, w, :], in1=mods_sb.t[:, w, 3, :],
                                                        op=ALU.add), reads=[bp, mods_sb], writes=[bp])
        xin = kb.sb("xin", [128, 8, TB], F32)
        catb = kb.sb("catb", [128, 8, TB], BF16)
        r = kb.sb("r", [128, 8, TB], F32)
        vf = kb.sb("vf", [128, 8, TB], F32)
        vb = kb.sb("vb", [128, 8, SBT], BF16)
        yacc = kb.sb("yacc", [128, 8, SBT], F32)
        gT = kb.sb("gT", [128, SBT], F32)
        kb.op("pool", lambda e: e.memset(gT.t[:], 0.0), writes=[gT])
        NW = 3
        wbuf = [kb.sb("wbuf%d" % i, [128, 8 * 768], BF16) for i in range(NW)]
        wrr = [0]
        rt = {n: kb.sb("rt_" + n, [128, 16], F32) for n in ("s", "sel", "c1", "c2", "c3", "min", "selm", "mask", "sg", "gates")}
        rgs = kb.sb("rt_gs", [128, 4], F32)
        rgm = kb.sb("rt_gm", [128, 4], F32)
        rmx = kb.sb("rt_mx", [128, 1], F32)
        rden = kb.sb("rt_den", [128, 1], F32)
        gb = [kb.sb("gb%d" % i, [128, TB], F32) for i in range(2)]
        sg_t = [kb.sb("sgt%d" % i, [128, TB], F32) for i in range(2)]
        aj = [kb.sb("aj%d" % i, [128, TB], BF16) for i in range(6)]
        xmid_tok = [Buf(None, "xmid%d" % i) for i in range(nblk)]

        def v4(b):
            return b.t[:].rearrange("p (g e) -> p g e", e=4)

        def route_tile(vcols, gcol0):
            pR = P[4]
            for k in range(8):
                kb.op("pe", lambda e, k=k: e.matmul(pR.t[:, 0:16], vf.t[:, k, vcols], rw_sb.t[:, k, :],
                                                    start=(k == 0), stop=(k == 7)),
                      reads=[vf, rw_sb], writes=[pR], sig=(k == 7))
            kb.op("act", lambda e: e.activation(out=rt["s"].t[:], in_=pR.t[:, 0:16], func=AF.Sigmoid),
                  reads=[pR], writes=[rt["s"]])
            kb.op("dve", lambda e: e.tensor_tensor(out=rt["sel"].t[:], in0=rt["s"].t[:], in1=rb_sb.t[:], op=ALU.add),
                  reads=[rt["s"], rb_sb], writes=[rt["sel"]])
            sel4 = v4(rt["sel"])
            for k, cn in ((1, "c1"), (2, "c2"), (3, "c3")):
                c4 = v4(rt[cn])
                kb.op("dve", lambda e, k=k, c4=c4: e.tensor_tensor(out=c4[:, :, 0:4 - k], in0=sel4[:, :, k:4],
                                                                   in1=sel4[:, :, 0:4 - k], op=ALU.is_gt),
                      reads=[rt["sel"]], writes=[rt[cn]])
                kb.op("dve", lambda e, k=k, c4=c4: e.tensor_tensor(out=c4[:, :, 4 - k:4], in0=sel4[:, :, 0:k],
                                                                   in1=sel4[:, :, 4 - k:4], op=ALU.is_gt),
                      reads=[rt["sel"]], writes=[rt[cn]])
            kb.op("dve", lambda e: e.tensor_tensor(out=rt["c1"].t[:], in0=rt["c1"].t[:], in1=rt["c2"].t[:], op=ALU.add),
                  reads=[rt["c1"], rt["c2"]], writes=[rt["c1"]])
            kb.op("dve", lambda e: e.tensor_tensor(out=rt["c1"].t[:], in0=rt["c1"].t[:], in1=rt["c3"].t[:], op=ALU.add),
                  reads=[rt["c1"], rt["c3"]], writes=[rt["c1"]])
            kb.op("dve", lambda e: e.tensor_single_scalar(out=rt["min"].t[:], in_=rt["c1"].t[:], scalar=1.5, op=ALU.is_lt),
                  reads=[rt["c1"]], writes=[rt["min"]])
            kb.op("dve", lambda e: e.tensor_tensor(out=rt["selm"].t[:], in0=rt["sel"].t[:], in1=rt["min"].t[:], op=ALU.mult),
                  reads=[rt["sel"], rt["min"]], writes=[rt["selm"]])
            kb.op("dve", lambda e: e.tensor_reduce(out=rgs.t[:], in_=v4(rt["selm"]), axis=AX.X, op=ALU.add),
                  reads=[rt["selm"]], writes=[rgs])
            kb.op("dve", lambda e: e.tensor_reduce(out=rmx.t[:], in_=rgs.t[:], axis=AX.X, op=ALU.max),
                  reads=[rgs], writes=[rmx])
            kb.op("dve", lambda e: e.tensor_scalar(out=rgm.t[:], in0=rgs.t[:], scalar1=rmx.t[:, 0:1], scalar2=None,
                                                   op0=ALU.is_ge), reads=[rgs, rmx], writes=[rgm])
            m4 = v4(rt["mask"])
            min4 = v4(rt["min"])
            for g in range(4):
                kb.op("dve", lambda e, g=g: e.tensor_scalar(out=m4[:, g, :], in0=min4[:, g, :], scalar1=rgm.t[:, g:g + 1],
                                                            scalar2=None, op0=ALU.mult),
                      reads=[rt["min"], rgm], writes=[rt["mask"]])
            kb.op("dve", lambda e: e.tensor_tensor(out=rt["sg"].t[:], in0=rt["s"].t[:], in1=rt["mask"].t[:], op=ALU.mult),
                  reads=[rt["s"], rt["mask"]], writes=[rt["sg"]])
            kb.op("dve", lambda e: e.tensor_reduce(out=rden.t[:], in_=rt["sg"].t[:], axis=AX.X, op=ALU.add),
                  reads=[rt["sg"]], writes=[rden])
            kb.op("dve", lambda e: e.reciprocal(out=rden.t[:], in_=rden.t[:]), reads=[rden], writes=[rden])
            kb.op("dve", lambda e: e.tensor_scalar(out=rt["gates"].t[:], in0=rt["sg"].t[:], scalar1=rden.t[:, 0:1],
                                                   scalar2=None, op0=ALU.mult), reads=[rt["sg"], rden], writes=[rt["gates"]])
            kb.op("pe", lambda e: e.transpose(pR.t[0:16, 128:256], rt["gates"].t[:], id_sb.t[:]),
                  reads=[rt["gates"], id_sb], writes=[pR])
            kb.op("act", lambda e: e.activation(out=gT.t[0:16, gcol0:gcol0 + 128], in_=pR.t[0:16, 128:256], func=AF.Copy),
                  reads=[pR], writes=[gT])

        def phase1(bi, lb):
            cols = slice(bi * TB, (bi + 1) * TB)
            kb.dma("sp", xin.t[:], xT[:, :, cols], writes=[xin])
            if load_cat is None:
                kb.dma("pool", catb.t[:], catT[:, :, cols], writes=[catb])
            else:
                load_cat(bi, catb, r, vf)
            for d in range(8):
                pA = P[d % 2]
                for k in range(8):
                    kb.op("pe", lambda e, k=k, d=d, pA=pA: e.matmul(pA.t[:, 0:TB], wo_sb.t[:, k, d * 128:(d + 1) * 128],
                                                                    catb.t[:, k, :], start=(k == 0), stop=(k == 7)),
                          reads=[wo_sb, catb], writes=[pA], sig=(k == 7))
                for (lo, hi, w) in _segs(bi):
                    kb.op("dve", lambda e, d=d, lo=lo, hi=hi, w=w, pA=pA: e.scalar_tensor_tensor(
                        out=r.t[:, d, lo:hi], in0=pA.t[:, lo:hi], scalar=ga.t[:, 0, w, d:d + 1], in1=xin.t[:, d, lo:hi],
                        op0=ALU.mult, op1=ALU.add), reads=[pA, ga, xin], writes=[r])
            ln.normalize(r)
            for c in range(8):
                for (lo, hi, w) in _segs(bi):
                    kb.op("act", lambda e, c=c, lo=lo, hi=hi, w=w: e.activation(
                        out=vf.t[:, c, lo:hi], in_=r.t[:, c, lo:hi], func=AF.Identity,
                        scale=gp.t[:, w, c:c + 1], bias=bp.t[:, w, c:c + 1]), reads=[r, gp, bp], writes=[vf])
                kb.op("pool", lambda e, c=c: e.tensor_copy(out=vb.t[:, c, lb * TB:(lb + 1) * TB], in_=vf.t[:, c, :]),
                      reads=[vf], writes=[vb])
                kb.op("act", lambda e, c=c: e.activation(out=r.t[:, c, :], in_=r.t[:, c, :], func=AF.Identity,
                                                         scale=lnp_sb.t[:, 0, 0, c:c + 1], bias=lnp_sb.t[:, 1, 0, c:c + 1]),
                      reads=[r, lnp_sb], writes=[r])
            kb.dma("sp", xmid_d[:, :, cols], r.t[:], reads=[r], writes=[xmid_tok[bi]])
            for tt in range(TB // 128):
                route_tile(slice(tt * 128, (tt + 1) * 128), lb * TB + tt * 128)

        def load_w(src, nchunk, ncol):
            wb = wbuf[wrr[0] % NW]
            wrr[0] += 1
            view = wb.t[:, 0:nchunk * ncol].rearrange("p (c n) -> p c n", n=ncol)
            kb.dma("pool", view, src.rearrange("(c p) n -> p c n", p=128), writes=[wb])
            return wb, view

        def moe(nb_):
            for ex in range(nexp):
                wgb, wgv = load_w(wg[ex], 8, 768)
                wub, wuv = load_w(wu[ex], 8, 768)
                wdb, wdv = load_w(wd[ex], 6, 1024)
                for lb in range(nb_):
                    cs = slice(lb * TB, (lb + 1) * TB)
                    g_ = gb[lb % 2]
                    kb.op("pe", lambda e, ex=ex, cs=cs: e.matmul(P[5].t[:, 0:TB], sel_sb.t[:, ex * 128:(ex + 1) * 128],
                                                                 gT.t[:, cs], start=True, stop=True),
                          reads=[sel_sb, gT], writes=[P[5]])
                    kb.op("act", lambda e, g_=g_: e.activation(out=g_.t[:], in_=P[5].t[:, 0:TB], func=AF.Copy),
                          reads=[P[5]], writes=[g_])
                    ajs = []
                    for j in range(6):
                        pG, pU = P[0 + (j % 2)], P[2 + (j % 2)]
                        for k in range(8):
                            kb.op("pe", lambda e, k=k, j=j, pG=pG: e.matmul(pG.t[:, 0:TB], wgv[:, k, j * 128:(j + 1) * 128],
                                                                            vb.t[:, k, cs], start=(k == 0), stop=(k == 7)),
                                  reads=[wgb, vb], writes=[pG], sig=(k == 7))
                        for k in range(8):
                            kb.op("pe", lambda e, k=k, j=j, pU=pU: e.matmul(pU.t[:, 0:TB], wuv[:, k, j * 128:(j + 1) * 128],
                                                                            vb.t[:, k, cs], start=(k == 0), stop=(k == 7)),
                                  reads=[wub, vb], writes=[pU], sig=(k == 7))
                        s_ = sg_t[j % 2]
                        a_ = aj[j]
                        kb.op("act", lambda e, s_=s_, pG=pG: e.activation(out=s_.t[:], in_=pG.t[:, 0:TB], func=AF.Silu),
                              reads=[pG], writes=[s_])
                        kb.op("dve", lambda e, s_=s_, g_=g_: e.tensor_tensor(out=s_.t[:], in0=s_.t[:], in1=g_.t[:], op=ALU.mult),
                              reads=[s_, g_], writes=[s_])
                        kb.op("dve", lambda e, s_=s_, a_=a_, pU=pU: e.tensor_tensor(out=a_.t[:], in0=pU.t[:, 0:TB], in1=s_.t[:],
                                                                                    op=ALU.mult),
                              reads=[pU, s_], writes=[a_])
                        ajs.append(a_)
                    for d in range(8):
                        pY = P[6 + (d % 2)]
                        for j in range(6):
                            kb.op("pe", lambda e, j=j, d=d, pY=pY: e.matmul(pY.t[:, 0:TB], wdv[:, j, d * 128:(d + 1) * 128],
                                                                            ajs[j].t[:], start=(j == 0), stop=(j == 5)),
                                  reads=[wdb, ajs[j]], writes=[pY], sig=(j == 5))
                        if ex == 0:
                            kb.op("act", lambda e, d=d, pY=pY: e.activation(out=yacc.t[:, d, cs], in_=pY.t[:, 0:TB], func=AF.Copy),
                                  reads=[pY], writes=[yacc])
                        else:
                            kb.op("dve", lambda e, d=d, pY=pY: e.tensor_tensor(out=yacc.t[:, d, cs], in0=pY.t[:, 0:TB],
                                                                               in1=yacc.t[:, d, cs], op=ALU.add),
                                  reads=[pY, yacc], writes=[yacc])

        def phase3(bi, lb):
            cols = slice(bi * TB, (bi + 1) * TB)
            cs = slice(lb * TB, (lb + 1) * TB)
            kb.dma("sp", xin.t[:], xmid_d[:, :, cols], reads=[xmid_tok[bi]], writes=[xin])
            for d in range(8):
                for (lo, hi, w) in _segs(bi):
                    kb.op("dve", lambda e, d=d, lo=lo, hi=hi, w=w: e.scalar_tensor_tensor(
                        out=r.t[:, d, lo:hi], in0=yacc.t[:, d, lb * TB + lo:lb * TB + hi], scalar=ga.t[:, 1, w, d:d + 1],
                        in1=xin.t[:, d, lo:hi], op0=ALU.mult, op1=ALU.add), reads=[yacc, ga, xin], writes=[r])
            ln.normalize(r)
            for c in range(8):
                kb.op("act", lambda e, c=c: e.activation(out=r.t[:, c, :], in_=r.t[:, c, :], func=AF.Identity,
                                                         scale=lnp_sb.t[:, 0, 1, c:c + 1], bias=lnp_sb.t[:, 1, 1, c:c + 1]),
                      reads=[r, lnp_sb], writes=[r])
            kb.dma("sp", xo[:, :, cols], r.t[:], reads=[r], writes=[xo_tok], is_out=True)

        b0 = 0
        for nb_ in sbs:
            for lb in range(nb_):
                phase1(b0 + lb, lb)
            moe(nb_)
            for lb in range(nb_):
                phase3(b0 + lb, lb)
            b0 += nb_
        assert b0 == nblk


        kb.barrier()
        kb.st = old_st


def build_stageC(nblk=11, sbs=(3, 3, 3, 2), nexp=16):
    nc = bass.Bass("TRN2", target_bir_lowering=False)
    T = nblk * TB
    catT = _din(nc, "catT", [1024, T]).rearrange("(c p) t -> p c t", p=128)
    xT = _din(nc, "xT", [1024, T]).rearrange("(c p) t -> p c t", p=128)
    w_o = _din(nc, "w_o", [1024, 1024]).rearrange("(c p) n -> p c n", p=128)
    mods = _din(nc, "mods", [128, 2, 6, 8])
    lnp = _din(nc, "lnp", [128, 2, 2, 8])
    rw = _din(nc, "rw", [1024, 16]).rearrange("(c p) n -> p c n", p=128)
    rbias = _din(nc, "rbias", [128, 16])
    sel_in = _din(nc, "sel", [16, 16 * 128])
    ident_in = _din(nc, "ident", [128, 128])
    wg = _din(nc, "wg", [nexp, 1024, 768])
    wu = _din(nc, "wu", [nexp, 1024, 768])
    wd = _din(nc, "wd", [nexp, 768, 1024])
    xo = _dout(nc, "xo", [1024, T]).rearrange("(c p) t -> p c t", p=128)
    xmid_d = nc.dram_tensor("xmid", [1024, T], F32, kind="Internal").ap().rearrange("(c p) t -> p c t", p=128)
    D = dict(catT=catT, xT=xT, xo=xo, w_o=w_o, mods=mods, lnp=lnp, rw=rw, rbias=rbias, sel=sel_in, ident=ident_in, wg=wg, wu=wu, wd=wd,
             xmid=xmid_d)
    with ExitStack() as st:
        kb = KB(nc, st)
        P = [kb.ps("P%d" % i, [128, 512]) for i in range(8)]
        emit_stageC(kb, nc, P, D, nblk, sbs, nexp)
        kb.finish()
        print("stageC instructions:", kb.n_inst, dict(kb.cnt))
    return nc


def pack_vec(v):
    v = np.asarray(v, np.float32)
    n = v.shape[-1] // 128
    return np.ascontiguousarray(np.moveaxis(v.reshape(v.shape[:-1] + (n, 128)), -1, 0))


def pack_mods(m_ctx, m_lat):
    a = np.stack([np.asarray(m_ctx, np.float32).reshape(6, 8, 128), np.asarray(m_lat, np.float32).reshape(6, 8, 128)], 0)
    return np.ascontiguousarray(a.transpose(3, 0, 1, 2))


def const_sel():
    s = np.zeros((16, 16, 128), np.float32)
    for e in range(16):
        s[e, e, :] = 1.0
    return s.reshape(16, 16 * 128)


SE = 8448
NBE = SE // TB
NCTX = 256
FM_E = 864
TM_E = 320


def _segs_h(bi):
    return [(0, 256, 0), (256, TB, 1)] if bi == 0 else [(0, TB, 1)]


def _bwd_segs(bi):
    s0 = bi * TB
    if s0 + TB <= 8192:
        return [(0, TB, 256 + s0)]
    nlat = 8192 - s0
    return [(0, nlat, 256 + s0), (nlat, TB, 0)]


def emit_inproj(kb, nc, xT, w_in, mods_sb, midx, pT, vtm, nfm, ntm, P, nblk=NBE, load_x=None):
    with ExitStack() as st:
        ncol = nfm + ntm
        w_sb = kb.sb_in(st, "ip_w", [128, 8, ncol], BF16)
        kb.dma("pool", w_sb.t[:], w_in, writes=[w_sb])
        sc1 = kb.sb_in(st, "ip_sc1", [128, 2, 8], F32)
        for w in range(2):
            kb.op("dve", lambda e, w=w: e.tensor_scalar(out=sc1.t[:, w, :], in0=mods_sb.t[:, w, midx + 1, :], scalar1=1.0,
                                                        scalar2=None, op0=ALU.add), reads=[mods_sb], writes=[sc1])
        xin = [kb.sb_in(st, "ip_xin%d" % i, [128, 8, TB], F32) for i in range(2)]
        u = [kb.sb_in(st, "ip_u%d" % i, [128, 8, TB], BF16) for i in range(2)]
        stg = [kb.sb_in(st, "ip_stg%d" % i, [128, TB], F32) for i in range(3)]
        stv = [kb.sb_in(st, "ip_stv%d" % i, [128, max(ntm, 1)], BF16) for i in range(2)]
        nch = (nfm + 127) // 128
        si = 0
        vi = 0
        for bi in range(nblk):
            cols = slice(bi * TB, (bi + 1) * TB)
            xi, ub = xin[bi % 2], u[bi % 2]
            if load_x is None:
                kb.dma("sp", xi.t[:], xT[:, :, cols], writes=[xi])
            else:
                load_x(bi, xi)
            for c in range(8):
                for (lo, hi, w) in _segs_h(bi):
                    kb.op("act", lambda e, c=c, lo=lo, hi=hi, w=w, xi=xi, ub=ub: e.activation(
                        out=ub.t[:, c, lo:hi], in_=xi.t[:, c, lo:hi], func=AF.Identity,
                        scale=sc1.t[:, w, c:c + 1], bias=mods_sb.t[:, w, midx, c:c + 1]), reads=[xi, sc1, mods_sb], writes=[ub])
            for cc in range(nch):
                n = min(128, nfm - cc * 128)
                pA = P[cc % 2]
                for k in range(8):
                    kb.op("pe", lambda e, k=k, cc=cc, n=n, pA=pA, ub=ub: e.matmul(
                        pA.t[0:n, 0:TB], w_sb.t[:, k, cc * 128:cc * 128 + n], ub.t[:, k, :], start=(k == 0), stop=(k == 7)),
                        reads=[w_sb, ub], writes=[pA], sig=(k == 7))
                sg = stg[si % 3]
                si += 1
                eng = "act" if cc % 2 == 0 else "dve"
                if eng == "act":
                    kb.op("act", lambda e, n=n, pA=pA, sg=sg: e.activation(out=sg.t[0:n, :], in_=pA.t[0:n, 0:TB], func=AF.Copy),
                          reads=[pA], writes=[sg])
                else:
                    kb.op("dve", lambda e, n=n, pA=pA, sg=sg: e.tensor_copy(out=sg.t[0:n, :], in_=pA.t[0:n, 0:TB]),
                          reads=[pA], writes=[sg])
                kb.dma("sp", pT[cc * 128:cc * 128 + n, cols], sg.t[0:n, :], reads=[sg])
            for tt in range(TB // 128 if ntm > 0 else 0):
                pV = P[2 + (tt % 2)]
                for k in range(8):
                    kb.op("pe", lambda e, k=k, tt=tt, pV=pV, ub=ub: e.matmul(
                        pV.t[:, 0:ntm], ub.t[:, k, tt * 128:(tt + 1) * 128], w_sb.t[:, k, nfm:nfm + ntm],
                        start=(k == 0), stop=(k == 7)), reads=[w_sb, ub], writes=[pV], sig=(k == 7))
                sv = stv[vi % 2]
                vi += 1
                kb.op("dve", lambda e, pV=pV, sv=sv: e.tensor_copy(out=sv.t[:], in_=pV.t[:, 0:ntm]), reads=[pV], writes=[sv])
                r0 = bi * TB + tt * 128
                kb.dma("sp", vtm[r0:r0 + 128, :], sv.t[:], reads=[sv])
        kb.barrier()


def emit_gla(kb, nc, pT, vtm, wdec, bdec, glag, masks_in, ident_in, ofs, obs, cat_out, P):
    INV_TAU = 1.0 / 16.0
    with ExitStack() as st:
        sbn = lambda n, sh, dt: kb.sb_in(st, "gl_" + n, sh, dt)
        wdec_sb = sbn("wdec", [16, 2, 128], F32)
        nbdec = sbn("nbdec", [64, 2, 2], F32)
        g_sb = sbn("g", [128, 1], F32)
        one = sbn("one", [128, 1], F32)
        eps = sbn("eps", [128, 1], F32)
        ones128 = sbn("ones128", [128, 128], BF16)
        cmask = sbn("cmask", [64, TB], F32)
        mk = sbn("mk", [128, 2, 128], F32)
        idb = sbn("idb", [64, 64], BF16)
        kb.dma("sp", wdec_sb.t[:], wdec, writes=[wdec_sb])
        kb.dma("sp", nbdec.t[:], bdec, writes=[nbdec])
        kb.dma("sp", g_sb.t[:], glag, writes=[g_sb])
        kb.dma("sp", mk.t[:], masks_in, writes=[mk])
        kb.dma("pool", idb.t[:], ident_in[0:64, 0:64], writes=[idb])
        kb.op("dve", lambda e: e.tensor_scalar(out=nbdec.t[:], in0=nbdec.t[:], scalar1=-1.0, scalar2=None, op0=ALU.mult),
              reads=[nbdec], writes=[nbdec])
        kb.op("pool", lambda e: e.memset(one.t[:], 1.0), writes=[one])
        kb.op("pool", lambda e: e.memset(eps.t[:], 1e-6), writes=[eps])
        kb.op("pool", lambda e: e.memset(ones128.t[:], 1.0 / 128.0), writes=[ones128])
        kb.op("pool", lambda e: e.memset(cmask.t[:], 1.0), writes=[cmask])
        for n in range(TB // 128):
            kb.op("pool", lambda e, n=n: e.memset(cmask.t[:, n * 128:n * 128 + 1], 0.0), writes=[cmask])
        S32 = {}
        Sbf = {}
        bufs = {}
        for hd in range(2):
            for dr in range(2):
                key = (hd, dr)
                S32[key] = sbn("S32_%d%d" % key, [64, 128], F32)
                Sbf[key] = sbn("Sbf_%d%d" % key, [64, 128], BF16)
                kb.op("pool", lambda e, key=key: e.memset(S32[key].t[:], 0.0), writes=[S32[key]])
                kb.op("pool", lambda e, key=key: e.memset(Sbf[key].t[:], 0.0), writes=[Sbf[key]])
        for dr in range(2):
            d = {}
            for n, sh, dt in (("q", [64, TB], F32), ("k", [64, TB], F32), ("lx", [16, TB], F32), ("v", [128, 3, 128], BF16),
                              ("sp", [64, TB], F32), ("X", [64, TB], F32), ("tmp", [64, TB], F32), ("D1", [64, TB], F32),
                              ("D4", [64, TB], F32), ("E", [64, TB], F32), ("dec", [64, 3], F32),
                              ("qt", [64, TB], BF16), ("qh", [64, TB], BF16), ("kt", [64, TB], BF16), ("kh", [64, TB], BF16),
                              ("attm", [128, 128], BF16), ("khs", [128, 64], BF16), ("ost", [128, TB], F32)):
                d[n] = sbn("%s_%d" % (n, dr), sh, dt)
            bufs[dr] = d
        PK = Buf(st.enter_context(nc.psum_tensor("ps_" + kb.pfx + "gl_pk", [128, 64], BF16)), "gl_pk")

        def gla_block(hd, dr, bi):
            B = bufs[dr]
            key = (hd, dr)
            segs = [(0, TB, bi * TB)] if dr == 0 else _bwd_segs(bi)
            for (lo, hi, src) in segs:
                n = hi - lo
                kb.dma("sp", B["q"].t[:, lo:hi], pT[hd * 64:(hd + 1) * 64, src:src + n], writes=[B["q"]])
                kb.dma("sp", B["k"].t[:, lo:hi], pT[128 + hd * 64:128 + (hd + 1) * 64, src:src + n], writes=[B["k"]])
                kb.dma("sp", B["lx"].t[:, lo:hi], pT[832 + dr * 16:848 + dr * 16, src:src + n], writes=[B["lx"]])
                kb.dma("sp", B["v"].t[:, lo // 128:hi // 128, :],
                       vtm[src:src + n, hd * 128:(hd + 1) * 128].rearrange("(n p) d -> p n d", p=128), writes=[B["v"]])
            pz = P[0]
            kb.op("pe", lambda e: e.matmul(pz.t[0:64, 0:TB], wdec_sb.t[:, dr, hd * 64:(hd + 1) * 64], B["lx"].t[:],
                                           start=True, stop=True), reads=[wdec_sb, B["lx"]], writes=[pz])
            kb.op("act", lambda e: e.activation(out=B["sp"].t[:], in_=pz.t[0:64, 0:TB], func=AF.Exp, scale=-1.0,
                                                bias=nbdec.t[:, dr, hd:hd + 1]), reads=[pz, nbdec], writes=[B["sp"]])
            kb.op("act", lambda e: e.activation(out=B["sp"].t[:], in_=B["sp"].t[:], func=AF.Ln, bias=one.t[0:64, 0:1]),
                  reads=[B["sp"], one], writes=[B["sp"]])
            kb.op("dve", lambda e: e.tensor_tensor_scan(out=B["X"].t[:], data0=cmask.t[:], data1=B["sp"].t[:], initial=0.0,
                                                        op0=ALU.mult, op1=ALU.add), reads=[cmask, B["sp"]], writes=[B["X"]])
            X3 = B["X"].t[:].rearrange("p (n l) -> p n l", l=128)
            tmp3 = B["tmp"].t[:].rearrange("p (n l) -> p n l", l=128)
            D13 = B["D1"].t[:].rearrange("p (n l) -> p n l", l=128)
            D43 = B["D4"].t[:].rearrange("p (n l) -> p n l", l=128)
            kb.op("act", lambda e: e.activation(out=B["dec"].t[:].unsqueeze(2), in_=X3[:, :, 127:128], func=AF.Exp,
                                                scale=-INV_TAU), reads=[B["X"]], writes=[B["dec"]])
            if dr == 0:
                kb.op("dve", lambda e: e.tensor_tensor(out=D43, in0=X3, in1=X3[:, :, 127:128].broadcast_to([64, 3, 128]),
                                                       op=ALU.subtract), reads=[B["X"]], writes=[B["D4"]])
                Xb = B["X"]
                Xb3 = X3
            else:
                kb.op("dve", lambda e: e.tensor_tensor(out=tmp3, in0=X3, in1=X3[:, :, 127:128].broadcast_to([64, 3, 128]),
                                                       op=ALU.subtract), reads=[B["X"]], writes=[B["tmp"]])
                kb.op("dve", lambda e: e.tensor_tensor(out=B["D1"].t[:], in0=B["sp"].t[:], in1=B["tmp"].t[:], op=ALU.subtract),
                      reads=[B["sp"], B["tmp"]], writes=[B["D1"]])
                kb.op("dve", lambda e: e.tensor_tensor(out=D43, in0=D13, in1=X3[:, :, 127:128].broadcast_to([64, 3, 128]),
                                                       op=ALU.subtract), reads=[B["D1"], B["X"]], writes=[B["D4"]])
                kb.op("dve", lambda e: e.tensor_copy(out=B["tmp"].t[:], in_=B["D1"].t[:]), reads=[B["D1"]], writes=[B["tmp"]])
                Xb = B["tmp"]
                Xb3 = tmp3
            kb.op("dve", lambda e: e.tensor_tensor(out=D13, in0=Xb3, in1=Xb3[:, :, 63:64].broadcast_to([64, 3, 128]),
                                                   op=ALU.subtract), reads=[Xb], writes=[B["D1"]])
            for (src, scale, dst, base, isq) in ((B["D1"], -INV_TAU, B["qt"], B["q"], True), (B["D1"], INV_TAU, B["kt"], B["k"], False),
                                                 (Xb, -INV_TAU, B["qh"], B["q"], True), (B["D4"], INV_TAU, B["kh"], B["k"], False)):
                kb.op("act", lambda e, src=src, scale=scale: e.activation(out=B["E"].t[:], in_=src.t[:], func=AF.Exp, scale=scale),
                      reads=[src], writes=[B["E"]])
                if isq:
                    kb.op("dve", lambda e, dst=dst, base=base: e.scalar_tensor_tensor(
                        out=dst.t[:], in0=base.t[:], scalar=0.125, in1=B["E"].t[:], op0=ALU.mult, op1=ALU.mult),
                        reads=[base, B["E"]], writes=[dst])
                else:
                    kb.op("dve", lambda e, dst=dst, base=base: e.tensor_tensor(out=dst.t[:], in0=base.t[:], in1=B["E"].t[:],
                                                                               op=ALU.mult), reads=[base, B["E"]], writes=[dst])
            order = range(3) if dr == 0 else range(2, -1, -1)
            for ci in order:
                cs = slice(ci * 128, (ci + 1) * 128)
                pA, pO, pD = P[1 + dr], P[3 + dr], P[5 + dr]
                kb.op("pe", lambda e: e.matmul(pA.t[:, 0:128], B["kt"].t[:, cs], B["qt"].t[:, cs], start=True, stop=True),
                      reads=[B["kt"], B["qt"]], writes=[pA])
                kb.op("dve", lambda e: e.tensor_tensor(out=B["attm"].t[:], in0=pA.t[:, 0:128], in1=mk.t[:, dr, :], op=ALU.mult),
                      reads=[pA, mk], writes=[B["attm"]])
                kb.op("pe", lambda e: e.matmul(pO.t[:, 0:128], B["v"].t[:, ci, :], B["attm"].t[:], start=True, stop=False),
                      reads=[B["v"], B["attm"]], writes=[pO], sig=False)
                kb.op("pe", lambda e: e.matmul(pO.t[:, 0:128], Sbf[key].t[:], B["qh"].t[:, cs], start=False, stop=True),
                      reads=[Sbf[key], B["qh"]], writes=[pO])
                kb.op("act", lambda e: e.activation(out=B["ost"].t[:, cs], in_=pO.t[:, 0:128], func=AF.Copy),
                      reads=[pO], writes=[B["ost"]])
                kb.op("pe", lambda e: e.transpose(PK.t[:, 0:64], B["kh"].t[:, cs], idb.t[:]), reads=[B["kh"], idb], writes=[PK])
                kb.op("act", lambda e: e.activation(out=B["khs"].t[:], in_=PK.t[:, 0:64], func=AF.Copy),
                      reads=[PK], writes=[B["khs"]])
                kb.op("pe", lambda e: e.matmul(pD.t[0:64, 0:128], B["khs"].t[:], B["v"].t[:, ci, :], start=True, stop=True),
                      reads=[B["khs"], B["v"]], writes=[pD])
                kb.op("dve", lambda e: e.scalar_tensor_tensor(out=S32[key].t[:], in0=S32[key].t[:], scalar=B["dec"].t[:, ci:ci + 1],
                                                              in1=pD.t[0:64, 0:128], op0=ALU.mult, op1=ALU.add),
                      reads=[S32[key], B["dec"], pD], writes=[S32[key]])
                kb.op("act", lambda e: e.activation(out=Sbf[key].t[:], in_=S32[key].t[:], func=AF.Copy),
                      reads=[S32[key]], writes=[Sbf[key]])
            dst = ofs if dr == 0 else obs
            for (lo, hi, src) in segs:
                kb.dma("sp", dst[hd * 128:(hd + 1) * 128, src:src + (hi - lo)], B["ost"].t[:, lo:hi], reads=[B["ost"]])

        for hd in range(2):
            for i in range(NBE):
                gla_block(hd, 0, i)
                gla_block(hd, 1, NBE - 1 - i)
        kb.barrier()
        fo = [sbn("fo%d" % i, [128, TB], F32) for i in range(2)]
        fb = [sbn("fb%d" % i, [128, TB], F32) for i in range(2)]
        fr = [sbn("fr%d" % i, [128, TB], F32) for i in range(2)]
        fsq = sbn("fsq", [128, TB], BF16)
        frs = sbn("frs", [128, TB], F32)
        fi = 0
        for hd in range(2):
            for bi in range(NBE):
                cols = slice(bi * TB, (bi + 1) * TB)
                o_, b_, r_ = fo[fi % 2], fb[fi % 2], fr[fi % 2]
                fi += 1
                kb.dma("sp", o_.t[:], ofs[hd * 128:(hd + 1) * 128, cols], writes=[o_])
                kb.dma("sp", b_.t[:], obs[hd * 128:(hd + 1) * 128, cols], writes=[b_])
                kb.dma("sp", r_.t[:], pT[256 + hd * 128:256 + (hd + 1) * 128, cols], writes=[r_])
                kb.op("dve", lambda e: e.tensor_tensor(out=o_.t[:], in0=o_.t[:], in1=b_.t[:], op=ALU.add), reads=[o_, b_], writes=[o_])
                kb.op("act", lambda e: e.activation(out=fsq.t[:], in_=o_.t[:], func=AF.Square), reads=[o_], writes=[fsq])
                kb.op("pe", lambda e: e.matmul(P[0].t[:, 0:TB], ones128.t[:], fsq.t[:], start=True, stop=True),
                      reads=[ones128, fsq], writes=[P[0]])
                kb.op("act", lambda e: e.activation(out=frs.t[:], in_=P[0].t[:, 0:TB], func=AF.Sqrt, bias=eps.t[:, 0:1]),
                      reads=[P[0], eps], writes=[frs])
                kb.op("dve", lambda e: e.reciprocal(out=frs.t[:], in_=frs.t[:]), reads=[frs], writes=[frs])
                kb.op("dve", lambda e: e.scalar_tensor_tensor(out=o_.t[:], in0=o_.t[:], scalar=g_sb.t[:, 0:1], in1=frs.t[:],
                                                              op0=ALU.mult, op1=ALU.mult), reads=[o_, g_sb, frs], writes=[o_])
                kb.op("act", lambda e: e.activation(out=r_.t[:], in_=r_.t[:], func=AF.Silu), reads=[r_], writes=[r_])
                kb.op("dve", lambda e: e.tensor_tensor(out=o_.t[:], in0=o_.t[:], in1=r_.t[:], op=ALU.mult), reads=[o_, r_], writes=[o_])
                kb.dma("sp", cat_out[hd * 128:(hd + 1) * 128, cols], o_.t[:], reads=[o_], is_out=True)
        kb.barrier()


def emit_gqa(kb, nc, pT, vtm, qkg, rope_R, cosT, sinT, cat_out, P):
    QB = 512
    with ExitStack() as st:
        sbn = lambda n, sh, dt: kb.sb_in(st, "gq_" + n, sh, dt)
        QT = [sbn("QT%d" % j, [128, SE], BF16) for j in range(4)]
        KT = sbn("KT", [128, SE], BF16)
        for t_ in QT + [KT]:
            kb.op("pool", lambda e, t_=t_: e.memset(t_.t[64:128, :], 0.0), writes=[t_])
        V1 = sbn("V1", [128, SE // 128, 65], BF16)
        g_sb = sbn("g", [64, 2], F32)
        R_sb = sbn("R", [64, 64], F32)
        ones64 = sbn("ones64", [64, 64], F32)
        onesr = sbn("onesr", [128, 64], F32)
        eps = sbn("eps", [64, 1], F32)
        kb.dma("sp", g_sb.t[:], qkg, writes=[g_sb])
        kb.dma("sp", R_sb.t[:], rope_R, writes=[R_sb])
        kb.op("pool", lambda e: e.memset(ones64.t[:], 1.0 / 64.0), writes=[ones64])
        kb.op("pool", lambda e: e.memset(onesr.t[:], 1.0), writes=[onesr])
        kb.op("pool", lambda e: e.memset(eps.t[:], 1e-6), writes=[eps])
        kb.op("pool", lambda e: e.memset(V1.t[:], 1.0), writes=[V1])
        kb.dma("sp", V1.t[:, :, 0:64], vtm[:, 256:320].rearrange("(n p) d -> p n d", p=128), writes=[V1])
        x = [sbn("x%d" % i, [64, TB], F32) for i in range(2)]
        sq = sbn("sq", [64, TB], F32)
        rs = sbn("rs", [64, TB], F32)
        xn = sbn("xn", [64, TB], F32)
        t1 = sbn("t1", [64, TB], F32)
        t2 = sbn("t2", [64, TB], F32)
        cs_sb = [sbn("cos%d" % i, [64, TB], F32) for i in range(2)]
        sn_sb = [sbn("sin%d" % i, [64, TB], F32) for i in range(2)]
        xi = 0
        for bi in range(NBE):
            cols = slice(bi * TB, (bi + 1) * TB)
            llo = 256 if bi == 0 else 0
            lat0 = bi * TB + llo - 256
            nl = TB - llo
            c_, s_ = cs_sb[bi % 2], sn_sb[bi % 2]
            kb.dma("sp", c_.t[:, llo:TB], cosT[:, lat0:lat0 + nl], writes=[c_])
            kb.dma("sp", s_.t[:, llo:TB], sinT[:, lat0:lat0 + nl], writes=[s_])
            for j in range(5):
                x_ = x[xi % 2]
                xi += 1
                row0 = 512 + j * 64
                dst = QT[j] if j < 4 else KT
                gcol = 0 if j < 4 else 1
                oscale = 0.125 if j < 4 else 1.0
                kb.dma("sp", x_.t[:], pT[row0:row0 + 64, cols], writes=[x_])
                kb.op("act", lambda e: e.activation(out=sq.t[:], in_=x_.t[:], func=AF.Square), reads=[x_], writes=[sq])
                kb.op("pe", lambda e: e.matmul(P[0].t[0:64, 0:TB], ones64.t[:], sq.t[:], start=True, stop=True),
                      reads=[ones64, sq], writes=[P[0]])
                kb.op("act", lambda e: e.activation(out=rs.t[:], in_=P[0].t[0:64, 0:TB], func=AF.Sqrt, bias=eps.t[:, 0:1]),
                      reads=[P[0], eps], writes=[rs])
                kb.op("dve", lambda e: e.reciprocal(out=rs.t[:], in_=rs.t[:]), reads=[rs], writes=[rs])
                kb.op("dve", lambda e: e.scalar_tensor_tensor(out=xn.t[:], in0=x_.t[:], scalar=g_sb.t[:, gcol:gcol + 1], in1=rs.t[:],
                                                              op0=ALU.mult, op1=ALU.mult), reads=[x_, g_sb, rs], writes=[xn])
                if llo > 0:
                    kb.op("act", lambda e: e.activation(out=dst.t[0:64, bi * TB:bi * TB + llo], in_=xn.t[:, 0:llo], func=AF.Copy,
                                                        scale=oscale), reads=[xn], writes=[dst])
                kb.op("pe", lambda e: e.matmul(P[1].t[0:64, 0:nl], R_sb.t[:], xn.t[:, llo:TB], start=True, stop=True),
                      reads=[R_sb, xn], writes=[P[1]])
                kb.op("dve", lambda e: e.tensor_tensor(out=t1.t[:, llo:TB], in0=xn.t[:, llo:TB], in1=c_.t[:, llo:TB], op=ALU.mult),
                      reads=[xn, c_], writes=[t1])
                kb.op("dve", lambda e: e.tensor_tensor(out=t2.t[:, llo:TB], in0=P[1].t[0:64, 0:nl], in1=s_.t[:, llo:TB], op=ALU.mult),
                      reads=[P[1], s_], writes=[t2])
                kb.op("dve", lambda e: e.tensor_tensor(out=t1.t[:, llo:TB], in0=t1.t[:, llo:TB], in1=t2.t[:, llo:TB], op=ALU.add),
                      reads=[t1, t2], writes=[t1])
                kb.op("act", lambda e: e.activation(out=dst.t[0:64, bi * TB + llo:(bi + 1) * TB], in_=t1.t[:, llo:TB], func=AF.Copy,
                                                    scale=oscale), reads=[t1], writes=[dst])
        pt = [sbn("pt%d" % i, [128, QB], BF16) for i in range(3)]
        osb = [sbn("osb%d" % i, [64, QB], F32) for i in range(2)]
        rsum = sbn("rsum", [128, QB], F32)
        pi = 0
        qi = 0
        jobs = []
        for j in range(4):
            jobs.append((j, 0, 256, 2))
            for qb in range(8192 // QB):
                jobs.append((j, 256 + qb * QB, QB, SE // 128))
        for (j, q0, nq, nkt) in jobs:
            pO = P[4 + (qi % 2)]
            o_ = osb[qi % 2]
            qi += 1
            pts = {}
            for kt in range(nkt + 1):
                if kt < nkt:
                    pS = P[2 + (kt % 2)]
                    p_ = pt[pi % 3]
                    pi += 1
                    pts[kt] = p_
                    kb.op("pe", lambda e: e.matmul(pS.t[:, 0:nq], KT.t[:, kt * 128:(kt + 1) * 128], QT[j].t[:, q0:q0 + nq],
                                                   start=True, stop=True), reads=[KT, QT[j]], writes=[pS])
                    kb.op("act", lambda e: e.activation(out=p_.t[:, 0:nq], in_=pS.t[:, 0:nq], func=AF.Exp), reads=[pS], writes=[p_])
                if kt >= 1:
                    kp = kt - 1
                    pp = pts.pop(kp)
                    kb.op("pe", lambda e: e.matmul(pO.t[0:65, 0:nq], V1.t[:, kp, :], pp.t[:, 0:nq], start=(kp == 0), stop=(kp == nkt - 1)),
                          reads=[V1, pp], writes=[pO], sig=(kp == nkt - 1))
            kb.op("dve", lambda e: e.reciprocal(out=rsum.t[64:65, 0:nq], in_=pO.t[64:65, 0:nq]), reads=[pO], writes=[rsum])
            kb.op("act", lambda e: e.activation(out=o_.t[:, 0:nq], in_=pO.t[0:64, 0:nq], func=AF.Copy), reads=[pO], writes=[o_])
            kb.op("pe", lambda e: e.matmul(P[6].t[0:64, 0:nq], onesr.t[64:65, :], rsum.t[64:65, 0:nq], start=True, stop=True),
                  reads=[onesr, rsum], writes=[P[6]])
            kb.op("dve", lambda e: e.tensor_tensor(out=o_.t[:, 0:nq], in0=o_.t[:, 0:nq], in1=P[6].t[0:64, 0:nq], op=ALU.mult),
                  reads=[o_, P[6]], writes=[o_])
            kb.dma("sp", cat_out[256 + j * 64:256 + (j + 1) * 64, q0:q0 + nq], o_.t[:, 0:nq], reads=[o_], is_out=True)
        kb.barrier()


def build_stageB_even(parts=("inproj", "gla", "gqa")):
    nc = bass.Bass("TRN2", target_bir_lowering=False)
    xT = _din(nc, "xT", [1024, SE]).rearrange("(c p) t -> p c t", p=128)
    w_in = _din(nc, "w_in", [1024, FM_E + TM_E]).rearrange("(c p) n -> p c n", p=128)
    mods = _din(nc, "mods", [128, 2, 6, 8])
    wdec = _din(nc, "wdec", [16, 2, 128])
    bdec = _din(nc, "bdec", [64, 2, 2])
    glag = _din(nc, "glag", [128, 1])
    masks = _din(nc, "masks", [128, 2, 128])
    ident = _din(nc, "ident", [128, 128])
    qkg = _din(nc, "qkg", [64, 2])
    ropeR = _din(nc, "ropeR", [64, 64])
    cosT = _din(nc, "cosT", [64, 8192])
    sinT = _din(nc, "sinT", [64, 8192])
    cat = _dout(nc, "cat", [512, SE])
    pT = nc.dram_tensor("pT", [FM_E, SE], F32, kind="Internal").ap()
    vtm = nc.dram_tensor("vtm", [SE, TM_E], BF16, kind="Internal").ap()
    ofs = nc.dram_tensor("ofs", [256, SE], F32, kind="Internal").ap()
    obs = nc.dram_tensor("obs", [256, SE], F32, kind="Internal").ap()
    with ExitStack() as st:
        kb = KB(nc, st)
        P = [kb.ps("P%d" % i, [128, 512]) for i in range(7)]
        mods_sb = kb.sb("modsb", [128, 2, 6, 8], F32)
        kb.dma("sp", mods_sb.t[:], mods, writes=[mods_sb])
        if "inproj" in parts:
            emit_inproj(kb, nc, xT, w_in, mods_sb, 0, pT, vtm, FM_E, TM_E, P)
        if "gla" in parts:
            emit_gla(kb, nc, pT, vtm, wdec, bdec, glag, masks, ident, ofs, obs, cat, P)
        if "gqa" in parts:
            emit_gqa(kb, nc, pT, vtm, qkg, ropeR, cosT, sinT, cat, P)
        kb.finish()
        print("stageB_even instructions:", kb.n_inst, dict(kb.cnt))
    return nc


def const_masks():
    m = np.arange(128)[:, None]
    l = np.arange(128)[None, :]
    return np.ascontiguousarray(np.stack([(l >= m), (l <= m)], 1).astype(np.float32))


def const_ropeR():
    R = np.zeros((64, 64), np.float32)
    for dp in range(64):
        blk = dp // 16
        if blk % 2 == 0:
            R[dp + 16, dp] = -1.0
        else:
            R[dp - 16, dp] = 1.0
    return R


def const_rope_tables():
    rows = 8192 // 64
    r = np.repeat(np.arange(rows, dtype=np.float32), 64)
    col = np.tile(np.arange(64, dtype=np.float32), rows)
    half = 32
    inv = (np.float32(10000.0) ** (-np.arange(0, half, 2, dtype=np.float32) / np.float32(half))).astype(np.float32)
    ar = r[:, None] * inv
    ac = col[:, None] * inv
    ang = np.concatenate([ar, ar, ac, ac], -1)
    return np.ascontiguousarray(np.cos(ang).T.astype(np.float32)), np.ascontiguousarray(np.sin(ang).T.astype(np.float32))


def even_core_cols(g):
    o_q, o_k, o_v, o_r, o_lf, o_lb, o_gq, o_gk, o_gv = np.cumsum([0, 256, 256, 512, 512, 16, 16, 512, 128])
    fm = []
    for hd in (2 * g, 2 * g + 1):
        fm += list(range(o_q + hd * 64, o_q + (hd + 1) * 64))
    for hd in (2 * g, 2 * g + 1):
        fm += list(range(o_k + hd * 64, o_k + (hd + 1) * 64))
    for hd in (2 * g, 2 * g + 1):
        fm += list(range(o_r + hd * 128, o_r + (hd + 1) * 128))
    for j in range(4 * g, 4 * g + 4):
        fm += list(range(o_gq + j * 64, o_gq + (j + 1) * 64))
    fm += list(range(o_gk + g * 64, o_gk + (g + 1) * 64))
    fm += list(range(o_lf, o_lf + 16)) + list(range(o_lb, o_lb + 16))
    tm = []
    for hd in (2 * g, 2 * g + 1):
        tm += list(range(o_v + hd * 128, o_v + (hd + 1) * 128))
    tm += list(range(o_gv + g * 64, o_gv + (g + 1) * 64))
    assert len(fm) == FM_E and len(tm) == TM_E
    return np.array(fm + tm)


def even_core_inputs(g, xT_b, mods_b, w_in, w_dec, b_dec, gla_g, qg, kg, consts):
    hd = (2 * g, 2 * g + 1)
    wdec = np.stack([np.concatenate([w_dec[d][:, h * 64:(h + 1) * 64] for h in hd], 1) for d in range(2)], 1)
    bdec = np.stack([np.stack([b_dec[d][h * 64:(h + 1) * 64] for h in hd], 1) for d in range(2)], 1)
    return dict(xT=xT_b, w_in=np.ascontiguousarray(w_in[:, even_core_cols(g)]), mods=mods_b,
                wdec=np.ascontiguousarray(wdec, np.float32), bdec=np.ascontiguousarray(bdec, np.float32),
                glag=np.ascontiguousarray(gla_g.reshape(128, 1)), masks=consts["masks"], ident=consts["ident"],
                qkg=np.ascontiguousarray(np.stack([qg, kg], 1)), ropeR=consts["ropeR"], cosT=consts["cosT"], sinT=consts["sinT"])


FM_O = 1164
NEGBIG = -30000.0


def emit_conv(kb, nc, pT, cT, convw, convb, P):
    with ExitStack() as st:
        sbn = lambda n, sh, dt: kb.sb_in(st, "cv_" + n, sh, dt)
        w_sb = sbn("w", [128, 5, 5], F32)
        b_sb = sbn("b", [128, 5], F32)
        kb.dma("sp", w_sb.t[:], convw, writes=[w_sb])
        kb.dma("sp", b_sb.t[:], convb, writes=[b_sb])
        xin = [sbn("xin%d" % i, [128, TB + 4], F32) for i in range(3)]
        acc = [sbn("acc%d" % i, [128, TB], F32) for i in range(3)]
        k = 0
        segs = [(0, 256, 0, 256)]
        segs += [(256 + i * TB, min(256 + (i + 1) * TB, SE), 256, SE) for i in range((8192 + TB - 1) // TB)]
        for (a, b, s0, s1) in segs:
            n = b - a
            lo, hi = max(a - 2, s0), min(b + 2, s1)
            for c in range(5):
                xi, ac = xin[k % 3], acc[k % 3]
                k += 1
                kb.op("pool", lambda e: e.memset(xi.t[:], 0.0), writes=[xi])
                kb.dma("sp", xi.t[:, lo - (a - 2):hi - (a - 2)], pT[384 + c * 128:384 + (c + 1) * 128, lo:hi], writes=[xi])
                kb.op("dve", lambda e: e.tensor_scalar(out=ac.t[:, 0:n], in0=xi.t[:, 0:n], scalar1=w_sb.t[:, c, 0:1],
                                                       scalar2=b_sb.t[:, c:c + 1], op0=ALU.mult, op1=ALU.add),
                      reads=[xi, w_sb, b_sb], writes=[ac])
                for j in range(1, 5):
                    kb.op("dve", lambda e, j=j: e.scalar_tensor_tensor(out=ac.t[:, 0:n], in0=xi.t[:, j:j + n], scalar=w_sb.t[:, c, j:j + 1],
                                                                       in1=ac.t[:, 0:n], op0=ALU.mult, op1=ALU.add),
                          reads=[xi, w_sb, ac], writes=[ac])
                kb.op("act", lambda e: e.activation(out=ac.t[:, 0:n], in_=ac.t[:, 0:n], func=AF.Silu), reads=[ac], writes=[ac])
                kb.dma("sp", cT[c * 128:(c + 1) * 128, a:b], ac.t[:, 0:n], reads=[ac])
        kb.barrier()


def emit_ssd(kb, nc, pT, cT, dtb_in, alog_in, dsk_in, ng_in, masks_in, ident_in, yfs, ybs, cat_out, P):
    with ExitStack() as st:
        sbn = lambda n, sh, dt: kb.sb_in(st, "sd_" + n, sh, dt)
        dtbias = sbn("dtbias", [128, 12], F32)
        negA = sbn("negA", [128, 12], F32)
        mkb = sbn("mkb", [128, 2, 128], F32)
        onesr = sbn("onesr", [1, 128], F32)
        one1 = sbn("one1", [128, 1], F32)
        cmask = sbn("cmask", [128, TB], F32)
        idf = sbn("idf", [128, 128], BF16)
        kb.dma("sp", dtbias.t[:], dtb_in, writes=[dtbias])
        kb.dma("sp", negA.t[:], alog_in, writes=[negA])
        kb.dma("sp", mkb.t[:], masks_in, writes=[mkb])
        kb.dma("pool", idf.t[:], ident_in, writes=[idf])
        kb.op("act", lambda e: e.activation(out=negA.t[:], in_=negA.t[:], func=AF.Exp), reads=[negA], writes=[negA])
        kb.op("dve", lambda e: e.tensor_scalar(out=negA.t[:], in0=negA.t[:], scalar1=-1.0, scalar2=None, op0=ALU.mult),
              reads=[negA], writes=[negA])
        kb.op("dve", lambda e: e.tensor_scalar(out=mkb.t[:], in0=mkb.t[:], scalar1=-1.0, scalar2=-NEGBIG, op0=ALU.add, op1=ALU.mult),
              reads=[mkb], writes=[mkb])
        kb.op("pool", lambda e: e.memset(onesr.t[:], 1.0), writes=[onesr])
        kb.op("pool", lambda e: e.memset(one1.t[:], 1.0), writes=[one1])
        kb.op("pool", lambda e: e.memset(cmask.t[:], 1.0), writes=[cmask])
        for n in range(TB // 128):
            kb.op("pool", lambda e, n=n: e.memset(cmask.t[:, n * 128:n * 128 + 1], 0.0), writes=[cmask])
        H32, Hbf = {}, {}
        for h in range(6):
            for dr in range(2):
                key = (h, dr)
                H32[key] = sbn("H32_%d%d" % key, [128, 64], F32)
                Hbf[key] = sbn("Hbf_%d%d" % key, [128, 64], BF16)
                kb.op("pool", lambda e, key=key: e.memset(H32[key].t[:], 0.0), writes=[H32[key]])
                kb.op("pool", lambda e, key=key: e.memset(Hbf[key].t[:], 0.0), writes=[Hbf[key]])
        sh = {}
        for dr in range(2):
            d = {}
            for n, shp, dt in (("BT", [128, TB], BF16), ("CT", [128, TB], F32), ("xsT", [128, 3, TB], BF16), ("CB", [128, 3, 128], F32),
                               ("Btm", [128, 3, 128], BF16), ("xstm", [128, 3, 384], F32)):
                d[n] = sbn("%s_%d" % (n, dr), shp, dt)
            sh[dr] = d
        pd = {}
        for n, shp, dt in (("raw", [128, TB], F32), ("dt", [128, TB], F32), ("P", [128, TB], F32), ("X", [128, TB], F32),
                           ("nX", [128, TB], F32), ("EL", [128, TB], F32), ("w", [128, TB], F32), ("ED", [128, 3], F32),
                           ("dec", [128, 128], F32), ("G", [128, 128], BF16), ("cols", [128, 2], F32), ("xd", [128, 64], BF16),
                           ("xdw", [128, 64], BF16), ("Ct", [128, 128], BF16), ("yst", [64, TB], F32)):
            pd[n] = [sbn("%s_%d" % (n, i), shp, dt) for i in range(2)]
        PT = Buf(st.enter_context(nc.psum_tensor("ps_" + kb.pfx + "sd_pt", [128, 512], BF16)), "sd_pt")
        cnt = [0]

        def load_shared(dr, bi):
            Sd = sh[dr]
            segs = [(0, TB, bi * TB)] if dr == 0 else _bwd_segs(bi)
            for (lo, hi, src) in segs:
                n = hi - lo
                kb.dma("pool", Sd["BT"].t[:, lo:hi], cT[384:512, src:src + n], writes=[Sd["BT"]])
                kb.dma("sp", Sd["CT"].t[:, lo:hi], cT[512:640, src:src + n], writes=[Sd["CT"]])
                kb.dma("pool", Sd["xsT"].t[:, :, lo:hi], cT[0:384, src:src + n].rearrange("(c p) t -> p c t", p=128), writes=[Sd["xsT"]])
            ctb = pd["Ct"][0]
            for ci in range(3):
                cs = slice(ci * 128, (ci + 1) * 128)
                kb.op("act", lambda e: e.activation(out=ctb.t[:], in_=Sd["CT"].t[:, cs], func=AF.Copy), reads=[Sd["CT"]], writes=[ctb])
                kb.op("pe", lambda e: e.matmul(P[0].t[:, 0:128], Sd["BT"].t[:, cs], ctb.t[:], start=True, stop=True),
                      reads=[Sd["BT"], ctb], writes=[P[0]])
                kb.op("act", lambda e: e.activation(out=Sd["CB"].t[:, ci, :], in_=P[0].t[:, 0:128], func=AF.Copy),
                      reads=[P[0]], writes=[Sd["CB"]])
                kb.op("pe", lambda e: e.transpose(PT.t[:, 0:128], Sd["BT"].t[:, cs], idf.t[:]), reads=[Sd["BT"], idf], writes=[PT])
                kb.op("act", lambda e: e.activation(out=Sd["Btm"].t[:, ci, :], in_=PT.t[:, 0:128], func=AF.Copy),
                      reads=[PT], writes=[Sd["Btm"]])
                for c in range(3):
                    kb.op("pe", lambda e: e.transpose(PT.t[:, 128 + c * 128:256 + c * 128], Sd["xsT"].t[:, c, cs], idf.t[:]),
                          reads=[Sd["xsT"], idf], writes=[PT], sig=(c == 2))
                kb.op("dve", lambda e: e.tensor_copy(out=Sd["xstm"].t[:, ci, :], in_=PT.t[:, 128:512]), reads=[PT], writes=[Sd["xstm"]])

        def ssd_block(h, dr, bi):
            Sd = sh[dr]
            i = cnt[0] % 2
            cnt[0] += 1
            B = {k_: v_[i] for k_, v_ in pd.items()}
            key = (h, dr)
            hd = dr * 6 + h
            segs = [(0, TB, bi * TB)] if dr == 0 else _bwd_segs(bi)
            for (lo, hi, src) in segs:
                n = hi - lo
                row = 1152 + dr * 6 + h
                kb.dma("sp", B["raw"].t[:, lo:hi].unsqueeze(1), pT[row:row + 1, src:src + n].partition_broadcast(128), writes=[B["raw"]])
            kb.op("act", lambda e: e.activation(out=B["dt"].t[:], in_=B["raw"].t[:], func=AF.Exp, bias=dtbias.t[:, hd:hd + 1]),
                  reads=[B["raw"], dtbias], writes=[B["dt"]])
            kb.op("act", lambda e: e.activation(out=B["dt"].t[:], in_=B["dt"].t[:], func=AF.Ln, bias=one1.t[:, 0:1]),
                  reads=[B["dt"], one1], writes=[B["dt"]])
            kb.op("dve", lambda e: e.tensor_scalar(out=B["raw"].t[:], in0=B["dt"].t[:], scalar1=negA.t[:, hd:hd + 1], scalar2=None,
                                                   op0=ALU.mult), reads=[B["dt"], negA], writes=[B["raw"]])
            kb.op("dve", lambda e: e.tensor_tensor_scan(out=B["P"].t[:], data0=cmask.t[:], data1=B["raw"].t[:], initial=0.0,
                                                        op0=ALU.mult, op1=ALU.add), reads=[cmask, B["raw"]], writes=[B["P"]])
            P3 = B["P"].t[:].rearrange("p (n l) -> p n l", l=128)
            tot_bc = P3[:, :, 127:128].broadcast_to([128, 3, 128])
            r3 = lambda b_: b_.t[:].rearrange("p (n l) -> p n l", l=128)
            kb.op("act", lambda e: e.activation(out=B["ED"].t[:].unsqueeze(2), in_=P3[:, :, 127:128], func=AF.Exp),
                  reads=[B["P"]], writes=[B["ED"]])
            if dr == 0:
                kb.op("dve", lambda e: e.tensor_copy(out=B["X"].t[:], in_=B["P"].t[:]), reads=[B["P"]], writes=[B["X"]])
                kb.op("act", lambda e: e.activation(out=B["EL"].t[:], in_=B["P"].t[:], func=AF.Exp), reads=[B["P"]], writes=[B["EL"]])
                kb.op("dve", lambda e: e.tensor_tensor(out=r3(B["w"]), in0=tot_bc, in1=P3, op=ALU.subtract), reads=[B["P"]], writes=[B["w"]])
            else:
                kb.op("dve", lambda e: e.tensor_tensor(out=B["X"].t[:], in0=B["P"].t[:], in1=B["raw"].t[:], op=ALU.subtract),
                      reads=[B["P"], B["raw"]], writes=[B["X"]])
                kb.op("dve", lambda e: e.tensor_tensor(out=r3(B["EL"]), in0=tot_bc, in1=r3(B["X"]), op=ALU.subtract),
                      reads=[B["P"], B["X"]], writes=[B["EL"]])
                kb.op("act", lambda e: e.activation(out=B["EL"].t[:], in_=B["EL"].t[:], func=AF.Exp), reads=[B["EL"]], writes=[B["EL"]])
                kb.op("dve", lambda e: e.tensor_copy(out=B["w"].t[:], in_=B["X"].t[:]), reads=[B["X"]], writes=[B["w"]])
            kb.op("act", lambda e: e.activation(out=B["w"].t[:], in_=B["w"].t[:], func=AF.Exp), reads=[B["w"]], writes=[B["w"]])
            kb.op("dve", lambda e: e.tensor_tensor(out=B["w"].t[:], in0=B["w"].t[:], in1=B["dt"].t[:], op=ALU.mult),
                  reads=[B["w"], B["dt"]], writes=[B["w"]])
            kb.op("dve", lambda e: e.tensor_scalar(out=B["nX"].t[:], in0=B["X"].t[:], scalar1=-1.0, scalar2=None, op0=ALU.mult),
                  reads=[B["X"]], writes=[B["nX"]])
            order = range(3) if dr == 0 else range(2, -1, -1)
            for ci in order:
                cs = slice(ci * 128, (ci + 1) * 128)
                pS, pC, pY, pH = P[1], P[2], P[3 + dr], P[5 + dr]
                if dr == 0:
                    kb.op("pe", lambda e: e.matmul(pS.t[:, 0:128], onesr.t[0:1, :], B["X"].t[0:1, cs], start=True, stop=False),
                          reads=[onesr, B["X"]], writes=[pS], sig=False)
                    kb.op("pe", lambda e: e.matmul(pS.t[:, 0:128], B["nX"].t[0:1, cs], onesr.t[0:1, :], start=False, stop=True),
                          reads=[onesr, B["nX"]], writes=[pS])
                else:
                    kb.op("pe", lambda e: e.matmul(pS.t[:, 0:128], onesr.t[0:1, :], B["nX"].t[0:1, cs], start=True, stop=False),
                          reads=[onesr, B["nX"]], writes=[pS], sig=False)
                    kb.op("pe", lambda e: e.matmul(pS.t[:, 0:128], B["X"].t[0:1, cs], onesr.t[0:1, :], start=False, stop=True),
                          reads=[onesr, B["X"]], writes=[pS])
                kb.op("dve", lambda e: e.tensor_tensor(out=B["dec"].t[:], in0=pS.t[:, 0:128], in1=mkb.t[:, dr, :], op=ALU.add),
                      reads=[pS, mkb], writes=[B["dec"]])
                kb.op("act", lambda e: e.activation(out=B["dec"].t[:], in_=B["dec"].t[:], func=AF.Exp), reads=[B["dec"]], writes=[B["dec"]])
                kb.op("dve", lambda e: e.tensor_tensor(out=B["G"].t[:], in0=B["dec"].t[:], in1=Sd["CB"].t[:, ci, :], op=ALU.mult),
                      reads=[B["dec"], Sd["CB"]], writes=[B["G"]])
                kb.op("pe", lambda e: e.matmul(pC.t[:, 0:1], B["dt"].t[0:1, cs], onesr.t[0:1, 0:1], start=True, stop=True),
                      reads=[B["dt"], onesr], writes=[pC], sig=False)
                kb.op("pe", lambda e: e.matmul(pC.t[:, 1:2], B["w"].t[0:1, cs], onesr.t[0:1, 0:1], start=True, stop=True),
                      reads=[B["w"], onesr], writes=[pC])
                kb.op("act", lambda e: e.activation(out=B["cols"].t[:], in_=pC.t[:, 0:2], func=AF.Copy), reads=[pC], writes=[B["cols"]])
                xs_h = Sd["xstm"].t[:, ci, h * 64:(h + 1) * 64]
                kb.op("dve", lambda e: e.tensor_scalar(out=B["xd"].t[:], in0=xs_h, scalar1=B["cols"].t[:, 0:1], scalar2=None, op0=ALU.mult),
                      reads=[Sd["xstm"], B["cols"]], writes=[B["xd"]])
                kb.op("dve", lambda e: e.tensor_scalar(out=B["xdw"].t[:], in0=xs_h, scalar1=B["cols"].t[:, 1:2], scalar2=None, op0=ALU.mult),
                      reads=[Sd["xstm"], B["cols"]], writes=[B["xdw"]])
                kb.op("dve", lambda e: e.tensor_tensor(out=B["Ct"].t[:], in0=Sd["CT"].t[:, cs], in1=B["EL"].t[:, cs], op=ALU.mult),
                      reads=[Sd["CT"], B["EL"]], writes=[B["Ct"]])
                kb.op("pe", lambda e: e.matmul(pY.t[0:64, 0:128], B["xd"].t[:], B["G"].t[:], start=True, stop=False),
                      reads=[B["xd"], B["G"]], writes=[pY], sig=False)
                kb.op("pe", lambda e: e.matmul(pY.t[0:64, 0:128], Hbf[key].t[:], B["Ct"].t[:], start=False, stop=True),
                      reads=[Hbf[key], B["Ct"]], writes=[pY])
                kb.op("act", lambda e: e.activation(out=B["yst"].t[:, cs], in_=pY.t[0:64, 0:128], func=AF.Copy), reads=[pY], writes=[B["yst"]])
                kb.op("pe", lambda e: e.matmul(pH.t[:, 0:64], Sd["Btm"].t[:, ci, :], B["xdw"].t[:], start=True, stop=True),
                      reads=[Sd["Btm"], B["xdw"]], writes=[pH])
                kb.op("dve", lambda e: e.scalar_tensor_tensor(out=H32[key].t[:], in0=H32[key].t[:], scalar=B["ED"].t[:, ci:ci + 1],
                                                              in1=pH.t[:, 0:64], op0=ALU.mult, op1=ALU.add),
                      reads=[H32[key], B["ED"], pH], writes=[H32[key]])
                kb.op("act", lambda e: e.activation(out=Hbf[key].t[:], in_=H32[key].t[:], func=AF.Copy), reads=[H32[key]], writes=[Hbf[key]])
            dst = yfs if dr == 0 else ybs
            for (lo, hi, src) in segs:
                kb.dma("sp", dst[h * 64:(h + 1) * 64, src:src + (hi - lo)], B["yst"].t[:, lo:hi], reads=[B["yst"]])

        for i in range(NBE):
            load_shared(0, i)
            load_shared(1, NBE - 1 - i)
            for h in range(6):
                ssd_block(h, 0, i)
                ssd_block(h, 1, NBE - 1 - i)
        kb.barrier()
        dsk = sbn("dsk", [128, 3], F32)
        ng = sbn("ng", [128, 3], F32)
        ones384 = sbn("ones384", [128, 128], BF16)
        epsf = sbn("epsf", [128, 1], F32)
        kb.dma("sp", dsk.t[:], dsk_in, writes=[dsk])
        kb.dma("sp", ng.t[:], ng_in, writes=[ng])
        kb.op("pool", lambda e: e.memset(ones384.t[:], 1.0 / 384.0), writes=[ones384])
        kb.op("pool", lambda e: e.memset(epsf.t[:], 1e-6), writes=[epsf])
        fy = sbn("fy", [128, 3, TB], F32)
        fb = sbn("fb", [128, 3, TB], F32)
        fx = sbn("fx", [128, 3, TB], F32)
        fz = sbn("fz", [128, 3, TB], F32)
        fsq = sbn("fsq", [128, 3, TB], BF16)
        frs = sbn("frs", [128, TB], F32)
        v3 = lambda ap: ap.rearrange("(c p) t -> p c t", p=128)
        for bi in range(NBE):
            cols = slice(bi * TB, (bi + 1) * TB)
            kb.dma("sp", fy.t[:], v3(yfs[:, cols]), writes=[fy])
            kb.dma("sp", fb.t[:], v3(ybs[:, cols]), writes=[fb])
            kb.dma("sp", fx.t[:], v3(cT[0:384, cols]), writes=[fx])
            kb.dma("sp", fz.t[:], v3(pT[0:384, cols]), writes=[fz])
            for c in range(3):
                kb.op("dve", lambda e: e.tensor_tensor(out=fy.t[:, c, :], in0=fy.t[:, c, :], in1=fb.t[:, c, :], op=ALU.add),
                      reads=[fy, fb], writes=[fy])
                kb.op("dve", lambda e: e.scalar_tensor_tensor(out=fy.t[:, c, :], in0=fx.t[:, c, :], scalar=dsk.t[:, c:c + 1], in1=fy.t[:, c, :],
                                                              op0=ALU.mult, op1=ALU.add), reads=[fx, dsk, fy], writes=[fy])
                kb.op("act", lambda e: e.activation(out=fz.t[:, c, :], in_=fz.t[:, c, :], func=AF.Silu), reads=[fz], writes=[fz])
                kb.op("dve", lambda e: e.tensor_tensor(out=fy.t[:, c, :], in0=fy.t[:, c, :], in1=fz.t[:, c, :], op=ALU.mult),
                      reads=[fy, fz], writes=[fy])
                kb.op("act", lambda e: e.activation(out=fsq.t[:, c, :], in_=fy.t[:, c, :], func=AF.Square), reads=[fy], writes=[fsq])
            for c in range(3):
                kb.op("pe", lambda e: e.matmul(P[0].t[:, 0:TB], ones384.t[:], fsq.t[:, c, :], start=(c == 0), stop=(c == 2)),
                      reads=[ones384, fsq], writes=[P[0]], sig=(c == 2))
            kb.op("act", lambda e: e.activation(out=frs.t[:], in_=P[0].t[:, 0:TB], func=AF.Sqrt, bias=epsf.t[:, 0:1]),
                  reads=[P[0], epsf], writes=[frs])
            kb.op("dve", lambda e: e.reciprocal(out=frs.t[:], in_=frs.t[:]), reads=[frs], writes=[frs])
            for c in range(3):
                kb.op("dve", lambda e: e.scalar_tensor_tensor(out=fy.t[:, c, :], in0=fy.t[:, c, :], scalar=ng.t[:, c:c + 1], in1=frs.t[:],
                                                              op0=ALU.mult, op1=ALU.mult), reads=[fy, ng, frs], writes=[fy])
            kb.dma("sp", v3(cat_out[0:384, cols]), fy.t[:], reads=[fy], is_out=True)
        kb.barrier()


def emit_fnet(kb, nc, pT, fg_in, fconst, cat_out, P):
    with ExitStack() as st:
        sbn = lambda n, sh, dt: kb.sb_in(st, "fn_" + n, sh, dt)
        unT = sbn("unT", [128, SE], BF16)
        g_sb = sbn("g", [128, 1], F32)
        bd = sbn("bd", [128, 128], BF16)
        ccsc = sbn("ccsc", [128, 256], BF16)
        f64 = sbn("f64", [64, 2, 128], BF16)
        tw = sbn("tw", [128, 2, 64], F32)
        f128 = sbn("f128", [128, 2, 128], BF16)
        c256 = sbn("c256", [128, 2, 2, 256], BF16)
        eps = sbn("eps", [128, 1], F32)
        kb.dma("sp", g_sb.t[:], fg_in, writes=[g_sb])
        kb.dma("pool", bd.t[:], fconst["bd"], writes=[bd])
        kb.dma("pool", ccsc.t[:], fconst["ccsc"], writes=[ccsc])
        kb.dma("pool", f64.t[:], fconst["f64"], writes=[f64])
        kb.dma("sp", tw.t[:], fconst["tw"], writes=[tw])
        kb.dma("pool", f128.t[:], fconst["f128"], writes=[f128])
        kb.dma("pool", c256.t[:], fconst["c256"], writes=[c256])
        kb.op("pool", lambda e: e.memset(eps.t[:], 1e-6), writes=[eps])
        fin = [sbn("fin%d" % i, [128, TB], F32) for i in range(2)]
        fsq = sbn("fsq", [128, TB], BF16)
        frs = sbn("frs", [128, TB], F32)
        for bi in range(NBE):
            cols = slice(bi * TB, (bi + 1) * TB)
            f_ = fin[bi % 2]
            kb.dma("sp", f_.t[:], pT[1024:1152, cols], writes=[f_])
            kb.op("act", lambda e: e.activation(out=fsq.t[:], in_=f_.t[:], func=AF.Square), reads=[f_], writes=[fsq])
            kb.op("pe", lambda e: e.matmul(P[0].t[:, 0:TB], bd.t[:], fsq.t[:], start=True, stop=True), reads=[bd, fsq], writes=[P[0]])
            kb.op("act", lambda e: e.activation(out=frs.t[:], in_=P[0].t[:, 0:TB], func=AF.Sqrt, bias=eps.t[:, 0:1]),
                  reads=[P[0], eps], writes=[frs])
            kb.op("dve", lambda e: e.reciprocal(out=frs.t[:], in_=frs.t[:]), reads=[frs], writes=[frs])
            kb.op("dve", lambda e: e.scalar_tensor_tensor(out=unT.t[:, cols], in0=f_.t[:], scalar=g_sb.t[:, 0:1], in1=frs.t[:],
                                                          op0=ALU.mult, op1=ALU.mult), reads=[f_, g_sb, frs], writes=[unT])
        Actx = sbn("Actx", [128, 2, 2, 128], BF16)
        octx = sbn("octx", [128, 256], F32)
        for tile in range(2):
            kb.op("pe", lambda e: e.matmul(P[1].t[:, 0:256], unT.t[:, tile * 128:(tile + 1) * 128], ccsc.t[:, :],
                                           start=True, stop=True), reads=[unT, ccsc], writes=[P[1]])
            kb.op("act", lambda e: e.activation(
                out=Actx.t[:, tile, :, :].rearrange("p a (g c) -> p g a c", g=2),
                in_=P[1].t[:, 0:256].rearrange("p (g a c) -> p g a c", g=2, a=2), func=AF.Copy), reads=[P[1]], writes=[Actx])
        k = 0
        for tile in range(2):
            for ab in range(2):
                kb.op("pe", lambda e: e.matmul(P[2].t[:, 0:256], Actx.t[:, tile, ab, :], c256.t[:, tile, ab, :], start=(k == 0), stop=(k == 3)),
                      reads=[Actx, c256], writes=[P[2]], sig=(k == 3))
                k += 1
        kb.op("act", lambda e: e.activation(out=octx.t[:], in_=P[2].t[:, 0:256], func=AF.Copy), reads=[P[2]], writes=[octx])
        kb.dma("sp", cat_out[384:512, 0:256], octx.t[:], reads=[octx], is_out=True)
        Y = sbn("Y", [64, 2, 2, 64, 128], BF16)
        Zp = sbn("Zp", [128, 2, 64, 128], BF16)
        zs = [sbn("zs%d" % i, [128, 4, 2, 64], F32) for i in range(2)]
        ta = [sbn("ta%d" % i, [128, 4, 64], F32) for i in range(4)]
        ost = sbn("ost", [128, 8192], F32)
        unL = unT.t[:, 256:SE].rearrange("p (t1 t2) -> p t2 t1", t2=128)
        for t2 in range(128):
            pz = P[3 + (t2 % 2)]
            kb.op("pe", lambda e: e.matmul(pz.t[0:64, 0:256], unL[:, t2, :], ccsc.t[:, :], start=True, stop=True),
                  reads=[unT, ccsc], writes=[pz])
            src = pz.t[0:64, 0:256].rearrange("p (g a c) -> p g a c", g=2, a=2)
            dst = Y.t[:, :, :, :, t2].rearrange("p a g c -> p g a c")
            if t2 % 2 == 0:
                kb.op("act", lambda e: e.activation(out=dst, in_=src, func=AF.Copy), reads=[pz], writes=[Y])
            else:
                kb.op("dve", lambda e: e.tensor_copy(out=dst, in_=src), reads=[pz], writes=[Y])
        tcb = tw.t[:, 0, :].unsqueeze(1).broadcast_to([128, 4, 64])
        tsb = tw.t[:, 1, :].unsqueeze(1).broadcast_to([128, 4, 64])
        for b4 in range(32):
            pz = P[3 + (b4 % 2)]
            z_ = zs[b4 % 2]
            for q in range(4):
                gc = b4 * 4 + q
                g, c = gc // 64, gc % 64
                kb.op("pe", lambda e: e.matmul(pz.t[:, q * 128:(q + 1) * 128], Y.t[:, 0, g, c, :], f64.t[:, 0, :], start=True, stop=False),
                      reads=[Y, f64], writes=[pz], sig=False)
                kb.op("pe", lambda e: e.matmul(pz.t[:, q * 128:(q + 1) * 128], Y.t[:, 1, g, c, :], f64.t[:, 1, :], start=False, stop=True),
                      reads=[Y, f64], writes=[pz], sig=(q == 3))
            kb.op("act", lambda e: e.activation(out=z_.t[:], in_=pz.t[:, 0:512].rearrange("p (q r t) -> p q r t", q=4, r=2), func=AF.Copy),
                  reads=[pz], writes=[z_])
            zr, zi = z_.t[:, :, 0, :], z_.t[:, :, 1, :]
            gsl = slice(b4 * 4, b4 * 4 + 4)
            kb.op("dve", lambda e: e.tensor_tensor(out=ta[0].t[:], in0=zr, in1=tcb, op=ALU.mult), reads=[z_, tw], writes=[ta[0]])
            kb.op("dve", lambda e: e.tensor_tensor(out=ta[1].t[:], in0=zi, in1=tsb, op=ALU.mult), reads=[z_, tw], writes=[ta[1]])
            kb.op("dve", lambda e: e.tensor_tensor(out=Zp.t[:, 0, :, gsl].rearrange("p t g -> p g t"), in0=ta[0].t[:], in1=ta[1].t[:], op=ALU.add),
                  reads=[ta[0], ta[1]], writes=[Zp])
            kb.op("pool", lambda e: e.tensor_tensor(out=ta[2].t[:], in0=zi, in1=tcb, op=ALU.mult), reads=[z_, tw], writes=[ta[2]])
            kb.op("pool", lambda e: e.tensor_tensor(out=ta[3].t[:], in0=zr, in1=tsb, op=ALU.mult), reads=[z_, tw], writes=[ta[3]])
            kb.op("pool", lambda e: e.tensor_tensor(out=Zp.t[:, 1, :, gsl].rearrange("p t g -> p g t"), in0=ta[2].t[:], in1=ta[3].t[:],
                                                    op=ALU.subtract), reads=[ta[2], ta[3]], writes=[Zp])
        ostv = ost.t[:].rearrange("p (t2 t1) -> p t1 t2", t1=64)
        for b4 in range(16):
            pz = P[3 + (b4 % 2)]
            for q in range(4):
                t1p = b4 * 4 + q
                kb.op("pe", lambda e: e.matmul(pz.t[:, q * 128:(q + 1) * 128], Zp.t[:, 0, t1p, :], f128.t[:, 0, :], start=True, stop=False),
                      reads=[Zp, f128], writes=[pz], sig=False)
                kb.op("pe", lambda e: e.matmul(pz.t[:, q * 128:(q + 1) * 128], Zp.t[:, 1, t1p, :], f128.t[:, 1, :], start=False, stop=True),
                      reads=[Zp, f128], writes=[pz], sig=(q == 3))
            kb.op("act", lambda e: e.activation(out=ostv[:, b4 * 4:b4 * 4 + 4, :], in_=pz.t[:, 0:512].rearrange("p (q t) -> p q t", q=4),
                                                func=AF.Copy), reads=[pz], writes=[ost])
        for i in range(4):
            kb.dma("sp", cat_out[384:512, 256 + i * 2048:256 + (i + 1) * 2048], ost.t[:, i * 2048:(i + 1) * 2048], reads=[ost], is_out=True)
        kb.barrier()


def build_stageB_odd(parts=("inproj", "conv", "ssd", "fnet")):
    nc = bass.Bass("TRN2", target_bir_lowering=False)
    xT = _din(nc, "xT", [1024, SE]).rearrange("(c p) t -> p c t", p=128)
    w_in = _din(nc, "w_in", [1024, FM_O]).rearrange("(c p) n -> p c n", p=128)
    mods = _din(nc, "mods", [128, 2, 6, 8])
    convw = _din(nc, "convw", [128, 5, 5])
    convb = _din(nc, "convb", [128, 5])
    dtb = _din(nc, "dtb", [128, 12])
    alog = _din(nc, "alog", [128, 12])
    dsk = _din(nc, "dsk", [128, 3])
    ng = _din(nc, "ng", [128, 3])
    masks = _din(nc, "masks", [128, 2, 128])
    ident = _din(nc, "ident", [128, 128])
    fg = _din(nc, "fg", [128, 1])
    fconst = dict(bd=_din(nc, "fc_bd", [128, 128]), ccsc=_din(nc, "fc_ccsc", [128, 256]), f64=_din(nc, "fc_f64", [64, 2, 128]),
                  tw=_din(nc, "fc_tw", [128, 2, 64]), f128=_din(nc, "fc_f128", [128, 2, 128]), c256=_din(nc, "fc_c256", [128, 2, 2, 256]))
    cat = _dout(nc, "cat", [512, SE])
    pT = nc.dram_tensor("pT", [FM_O, SE], F32, kind="Internal").ap()
    cT = nc.dram_tensor("cT", [640, SE], F32, kind="Internal").ap()
    yfs = nc.dram_tensor("yfs", [384, SE], F32, kind="Internal").ap()
    ybs = nc.dram_tensor("ybs", [384, SE], F32, kind="Internal").ap()
    with ExitStack() as st:
        kb = KB(nc, st)
        P = [kb.ps("P%d" % i, [128, 512]) for i in range(7)]
        mods_sb = kb.sb("modsb", [128, 2, 6, 8], F32)
        kb.dma("sp", mods_sb.t[:], mods, writes=[mods_sb])
        if "inproj" in parts:
            emit_inproj(kb, nc, xT, w_in, mods_sb, 0, pT, None, FM_O, 0, P)
        if "conv" in parts:
            emit_conv(kb, nc, pT, cT, convw, convb, P)
        if "ssd" in parts:
            emit_ssd(kb, nc, pT, cT, dtb, alog, dsk, ng, masks, ident, yfs, ybs, cat, P)
        if "fnet" in parts:
            emit_fnet(kb, nc, pT, fg, fconst, cat, P)
        kb.finish()
        print("stageB_odd instructions:", kb.n_inst, dict(kb.cnt))
    return nc


def const_fnet():
    c = np.arange(64)
    ang = 2 * np.pi * np.outer(c, c) / 64.0
    C64, S64 = np.cos(ang), np.sin(ang)
    ccsc = np.concatenate([C64, S64], 1) / 8.0
    z_ = np.zeros_like(ccsc)
    ccsc = np.concatenate([np.concatenate([ccsc, z_], 1), np.concatenate([z_, ccsc], 1)], 0)
    f64 = np.stack([np.concatenate([C64, -S64], 1), np.concatenate([-S64, -C64], 1)], 1) / 8.0
    t2 = np.arange(128)
    phi = 2 * np.pi * np.outer(t2, c) / 8192.0
    tw = np.stack([np.cos(phi), np.sin(phi)], 1)
    psi = 2 * np.pi * np.outer(t2, t2) / 128.0
    f128 = np.stack([np.cos(psi), np.sin(psi)], 1) / np.sqrt(128.0)
    t = np.arange(256)
    a256 = 2 * np.pi * np.outer(t, t) / 256.0
    c256 = np.stack([np.cos(a256), -np.sin(a256)], 1) / 16.0
    c256 = c256.reshape(2, 128, 2, 256).transpose(1, 0, 2, 3)
    bd = np.zeros((128, 128))
    bd[:64, :64] = 1.0 / 64.0
    bd[64:, 64:] = 1.0 / 64.0
    f32 = lambda a: np.ascontiguousarray(a, np.float32)
    return dict(fc_bd=f32(bd), fc_ccsc=f32(ccsc), fc_f64=f32(f64), fc_tw=f32(tw), fc_f128=f32(f128), fc_c256=f32(c256))


def odd_core_cols(g):
    o_z, o_x, o_b, o_c, o_dtf, o_dtb, o_f = np.cumsum([0, 768, 768, 256, 256, 12, 12])
    cols = list(range(o_z + g * 384, o_z + (g + 1) * 384)) + list(range(o_x + g * 384, o_x + (g + 1) * 384))
    cols += list(range(o_b + g * 128, o_b + (g + 1) * 128)) + list(range(o_c + g * 128, o_c + (g + 1) * 128))
    cols += list(range(o_f + g * 128, o_f + (g + 1) * 128))
    cols += list(range(o_dtf + g * 6, o_dtf + (g + 1) * 6)) + list(range(o_dtb + g * 6, o_dtb + (g + 1) * 6))
    assert len(cols) == FM_O
    return np.array(cols)


def odd_core_inputs(g, xT_b, mods_b, w_in, conv_w, conv_b, dt_bias, a_log, d_skip, ssd_g, fnet_g, consts):
    ch = np.concatenate([768 * 0 + np.arange(g * 384, (g + 1) * 384), 768 + np.arange(g * 128, (g + 1) * 128),
                         1024 + np.arange(g * 128, (g + 1) * 128)])
    cw = conv_w[:, ch].reshape(5, 5, 128).transpose(2, 1, 0)
    cb = conv_b[ch].reshape(5, 128).T
    rep = lambda v: np.ascontiguousarray(np.broadcast_to(np.asarray(v, np.float32)[None, :], (128, len(v))))
    dtb = rep(np.concatenate([dt_bias[0][g * 6:(g + 1) * 6], dt_bias[1][g * 6:(g + 1) * 6]]))
    alog = rep(np.concatenate([a_log[0][g * 6:(g + 1) * 6], a_log[1][g * 6:(g + 1) * 6]]))
    dsk = np.repeat(d_skip[g * 6:(g + 1) * 6], 64).reshape(3, 128).T
    ng = ssd_g[g * 384:(g + 1) * 384].reshape(3, 128).T
    fg = fnet_g[g * 128:(g + 1) * 128].reshape(128, 1)
    f32 = lambda a: np.ascontiguousarray(a, np.float32)
    im = dict(xT=xT_b, w_in=f32(w_in[:, odd_core_cols(g)]), mods=mods_b, convw=f32(cw), convb=f32(cb), dtb=dtb, alog=alog,
              dsk=f32(dsk), ng=f32(ng), masks=consts["masks"], ident=consts["ident"], fg=f32(fg))
    im.update(consts["fnet"])
    return im


def build_adaln():
    nc = bass.Bass("TRN2", target_bir_lowering=False)
    cin = _din(nc, "cin", [128, 8, 5])
    w = _din(nc, "w", [1024, 3072]).rearrange("(c p) n -> p c n", p=128)
    b = _din(nc, "b", [128, 24])
    out = _dout(nc, "out", [128, 24, 5])
    with ExitStack() as st:
        kb = KB(nc, st)
        P = [kb.ps("P%d" % i, [128, 512]) for i in range(4)]
        c_sb = kb.sb("c", [128, 8, 5], F32)
        b_sb = kb.sb("b", [128, 24], F32)
        o_sb = kb.sb("o", [128, 24, 5], F32)
        w_sb = [kb.sb("w%d" % i, [128, 8, 768], F32) for i in range(4)]
        kb.dma("sp", c_sb.t[:], cin, writes=[c_sb])
        kb.dma("sp", b_sb.t[:], b, writes=[b_sb])
        for i in range(4):
            kb.dma("sp", w_sb[i].t[:], w[:, :, i * 768:(i + 1) * 768], writes=[w_sb[i]])
        kb.op("act", lambda e: e.activation(out=c_sb.t[:], in_=c_sb.t[:], func=AF.Silu), reads=[c_sb], writes=[c_sb])
        for j in range(24):
            pj = P[j % 4]
            wi, wo = j // 6, (j % 6) * 128
            for k in range(8):
                kb.op("pe", lambda e, k=k: e.matmul(pj.t[:, 0:5], w_sb[wi].t[:, k, wo:wo + 128], c_sb.t[:, k, :], start=(k == 0), stop=(k == 7)),
                      reads=[w_sb[wi], c_sb], writes=[pj], sig=(k == 7))
            kb.op("dve", lambda e: e.tensor_scalar(out=o_sb.t[:, j, :], in0=pj.t[:, 0:5], scalar1=b_sb.t[:, j:j + 1], scalar2=None, op0=ALU.add),
                  reads=[pj, b_sb], writes=[o_sb])
        kb.dma("sp", out, o_sb.t[:], reads=[o_sb], is_out=True)
        kb.finish()
    return nc


_PROGS = {}


def _prog(name, fn):
    if name not in _PROGS:
        _PROGS[name] = fn()
    return _PROGS[name]


def kernel_unfused(x, c, ctx, c_ctx, ada_w, ada_b, ln_g, ln_b, ev_w_in, ev_w_o, gla_w_decay, gla_b_decay, gla_norm_g, gqa_q_norm_g,
           gqa_k_norm_g, od_w_in, od_w_o, ssd_conv_w, ssd_conv_b, ssd_dt_bias, ssd_a_log, ssd_d, ssd_norm_g, fnet_norm_g,
           router_w, router_b, exp_w_gate, exp_w_up, exp_w_down):
    f32 = lambda a: np.ascontiguousarray(np.asarray(a), np.float32)
    x, c, ctx, c_ctx = f32(x), f32(c), f32(ctx), f32(c_ctx)
    ada_w, ada_b = f32(ada_w), f32(ada_b)
    NB, DEPTH = 4, 4
    cores = list(range(8))
    call = np.concatenate([c, c_ctx[None, :]], 0)
    cin = np.ascontiguousarray(call.reshape(5, 8, 128).transpose(2, 1, 0))
    ims = []
    for core in cores:
        layer, half = core // 2, core % 2
        ims.append(dict(cin=cin, w=np.ascontiguousarray(ada_w[layer][:, half * 3072:(half + 1) * 3072]),
                        b=np.ascontiguousarray(ada_b[layer][half * 3072:(half + 1) * 3072].reshape(24, 128).T)))
    res = run_bass_kernel_spmd(_prog("adaln", build_adaln), ims, core_ids=cores)
    mods_all = np.zeros((DEPTH, 5, 6144), np.float32)
    for core in cores:
        layer, half = core // 2, core % 2
        o = np.asarray(res.results[core]["out"])
        mods_all[layer][:, half * 3072:(half + 1) * 3072] = o.transpose(2, 1, 0).reshape(5, 3072)
    XT = [np.ascontiguousarray(np.concatenate([ctx[b], x[b]], 0).T) for b in range(NB)]
    cosT, sinT = const_rope_tables()
    consts = dict(masks=const_masks(), ident=np.eye(128, dtype=np.float32), ropeR=const_ropeR(), cosT=cosT, sinT=sinT,
                  fnet=const_fnet())
    sel = const_sel()
    rbias = np.ascontiguousarray(np.broadcast_to(f32(router_b)[None, :], (128, 16)))
    for layer in range(DEPTH):
        i = layer // 2
        mods = [pack_mods(mods_all[layer][4], mods_all[layer][b]) for b in range(NB)]
        ims = []
        for core in cores:
            b, g = core // 2, core % 2
            if layer % 2 == 0:
                ims.append(even_core_inputs(g, XT[b], mods[b], f32(ev_w_in[i]), f32(gla_w_decay[i]), f32(gla_b_decay[i]),
                                            f32(gla_norm_g[i]), f32(gqa_q_norm_g[i]), f32(gqa_k_norm_g[i]), consts))
            else:
                ims.append(odd_core_inputs(g, XT[b], mods[b], f32(od_w_in[i]), f32(ssd_conv_w[i]), f32(ssd_conv_b[i]),
                                           f32(ssd_dt_bias[i]), f32(ssd_a_log[i]), f32(ssd_d[i]), f32(ssd_norm_g[i]),
                                           f32(fnet_norm_g[i]), consts))
        if layer % 2 == 0:
            res = run_bass_kernel_spmd(_prog("B_even", build_stageB_even), ims, core_ids=cores)
        else:
            res = run_bass_kernel_spmd(_prog("B_odd", build_stageB_odd), ims, core_ids=cores)
        CAT = []
        for b in range(NB):
            cat = np.empty((1024, SE), np.float32)
            for g in range(2):
                cg = np.asarray(res.results[2 * b + g]["cat"])
                if layer % 2 == 0:
                    cat[g * 256:(g + 1) * 256] = cg[0:256]
                    cat[512 + g * 256:512 + (g + 1) * 256] = cg[256:512]
                else:
                    cat[g * 384:(g + 1) * 384] = cg[0:384]
                    cat[768 + g * 128:768 + (g + 1) * 128] = cg[384:512]
            CAT.append(cat)
        w_o = f32(ev_w_o[i]) if layer % 2 == 0 else f32(od_w_o[i])
        lnp = np.ascontiguousarray(np.stack([pack_vec(f32(ln_g[layer])), pack_vec(f32(ln_b[layer]))], 1))
        wg, wu, wd = f32(exp_w_gate[layer]), f32(exp_w_up[layer]), f32(exp_w_down[layer])
        ims = []
        for core in cores:
            b, h = core // 2, core % 2
            colsel = np.concatenate([np.arange(h * 128, (h + 1) * 128), 256 + np.arange(h * 4096, (h + 1) * 4096)])
            ims.append(dict(catT=np.ascontiguousarray(CAT[b][:, colsel]), xT=np.ascontiguousarray(XT[b][:, colsel]), w_o=w_o,
                            mods=mods[b], lnp=lnp, rw=f32(router_w), rbias=rbias, sel=sel, ident=consts["ident"],
                            wg=wg, wu=wu, wd=wd))
        res = run_bass_kernel_spmd(_prog("C", build_stageC), ims, core_ids=cores)
        for core in cores:
            b, h = core // 2, core % 2
            colsel = np.concatenate([np.arange(h * 128, (h + 1) * 128), 256 + np.arange(h * 4096, (h + 1) * 4096)])
            XT[b][:, colsel] = np.asarray(res.results[core]["xo"])
    return np.ascontiguousarray(np.stack([XT[b][:, 256:].T for b in range(NB)], 0)).astype(np.float32)


EVEN_CHUNK_ROWS = [0, 128, 512, 640, 256, 384, 768, 896]
ODD_CHUNK_ROWS = [0, 128, 256, 512, 640, 768, 384, 896]
PAIRS = [[0, 1], [2, 3], [4, 5], [6, 7]]


def _gsegs(lo, hi):
    out = []
    for (a, b, j, t0) in ((0, 128, 0, 0), (128, 256, 1, 0), (256, 4352, 0, 128), (4352, 8448, 1, 128)):
        s, e = max(lo, a), min(hi, b)
        if s < e:
            out.append((s, e, j, t0 + s - a))
    return out


def _csegs(bi, j):
    if bi == 0:
        return [(0, 128, j * 128), (128, TB, 256 + j * 4096)]
    return [(0, TB, 256 + j * 4096 + bi * TB - 128)]


def emit_adaln(kb, nc, cin2, ada_w, ada_b48, mods_bufs, P):
    with ExitStack() as st:
        c_sb = kb.sb_in(st, "ad_c", [128, 8, 2], F32)
        b_sb = kb.sb_in(st, "ad_b", [128, 4, 48], F32)
        wt = [kb.sb_in(st, "ad_w%d" % i, [128, 8, 768], F32) for i in range(2)]
        kb.dma("sp", c_sb.t[:], cin2, writes=[c_sb])
        for L in range(4):
            kb.dma("sp", b_sb.t[:, L, :], ada_b48[L], writes=[b_sb])
        kb.op("act", lambda e: e.activation(out=c_sb.t[:], in_=c_sb.t[:], func=AF.Silu), reads=[c_sb], writes=[c_sb])
        n = 0
        for L in range(len(mods_bufs)):
            wv = ada_w[L].rearrange("(c p) n -> p c n", p=128)
            for piece in range(8):
                w_ = wt[n % 2]
                n += 1
                kb.dma("sp", w_.t[:], wv[:, :, piece * 768:(piece + 1) * 768], writes=[w_])
                for jj in range(6):
                    j = piece * 6 + jj
                    m, c = divmod(j, 8)
                    pj = P[jj % 2]
                    for k in range(8):
                        kb.op("pe", lambda e, k=k: e.matmul(pj.t[:, 0:2], w_.t[:, k, jj * 128:(jj + 1) * 128], c_sb.t[:, k, :],
                                                            start=(k == 0), stop=(k == 7)), reads=[w_, c_sb], writes=[pj], sig=(k == 7))
                    kb.op("dve", lambda e: e.tensor_scalar(out=mods_bufs[L].t[:, :, m, c], in0=pj.t[:, 0:2], scalar1=b_sb.t[:, L, j:j + 1],
                                                           scalar2=None, op0=ALU.add), reads=[pj, b_sb], writes=[mods_bufs[L]])
        kb.barrier()


def build_fused(depth=4):
    nc = bass.Bass("TRN2", target_bir_lowering=False)
    I = {}

    def din(name, shape):
        I[name] = _din(nc, name, shape)
        return I[name]

    cin2 = din("cin2", [128, 8, 2])
    ada_w = din("ada_w", [4, 1024, 6144])
    ada_b48 = din("ada_b48", [4, 128, 48])
    XTg0 = din("XTg0", [2048, 4224])
    xown0 = din("xown0", [1024, 4224])
    msel_in = din("msel", [128, 2])
    masks = din("masks", [128, 2, 128])
    ident = din("ident", [128, 128])
    ropeR = din("ropeR", [64, 64])
    cosT = din("cosT", [64, 8192])
    sinT = din("sinT", [64, 8192])
    fconst = dict(bd=din("fc_bd", [128, 128]), ccsc=din("fc_ccsc", [128, 256]), f64=din("fc_f64", [64, 2, 128]),
                  tw=din("fc_tw", [128, 2, 64]), f128=din("fc_f128", [128, 2, 128]), c256=din("fc_c256", [128, 2, 2, 256]))
    rw = din("rw", [1024, 16]).rearrange("(c p) n -> p c n", p=128)
    rbias = din("rbias", [128, 16])
    sel_in = din("sel", [16, 16 * 128])
    LW = []
    for L in range(depth):
        d = {}
        if L % 2 == 0:
            d["w_in"] = din("w_in%d" % L, [1024, FM_E + TM_E]).rearrange("(c p) n -> p c n", p=128)
            d["wdec"] = din("wdec%d" % L, [16, 2, 128])
            d["bdec"] = din("bdec%d" % L, [64, 2, 2])
            d["glag"] = din("glag%d" % L, [128, 1])
            d["qkg"] = din("qkg%d" % L, [64, 2])
        else:
            d["w_in"] = din("w_in%d" % L, [1024, FM_O]).rearrange("(c p) n -> p c n", p=128)
            for nme, shp in (("convw", [128, 5, 5]), ("convb", [128, 5]), ("dtb", [128, 12]), ("alog", [128, 12]), ("dsk", [128, 3]),
                             ("ng", [128, 3]), ("fg", [128, 1])):
                d[nme] = din("%s%d" % (nme, L), shp)
        d["w_o"] = din("w_o%d" % L, [1024, 1024]).rearrange("(c p) n -> p c n", p=128)
        d["lnp"] = din("lnp%d" % L, [128, 2, 2, 8])
        d["wg"] = din("wg%d" % L, [16, 1024, 768])
        d["wu"] = din("wu%d" % L, [16, 1024, 768])
        d["wd"] = din("wd%d" % L, [16, 768, 1024])
        LW.append(d)
    xout = _dout(nc, "xout", [1024, TC])
    dint = lambda name, shape, dt=F32: nc.dram_tensor(name, list(shape), dt, kind="Internal").ap()
    pT = dint("pT", [FM_O, SE])
    vtm = dint("vtm", [SE, TM_E], BF16)
    ofs, obs = dint("ofs", [256, SE]), dint("obs", [256, SE])
    cT = dint("cT", [640, SE])
    yfs, ybs = dint("yfs", [384, SE]), dint("ybs", [384, SE])
    xmid = dint("xmid", [1024, TC]).rearrange("(c p) t -> p c t", p=128)
    catp = dint("catp", [512, SE])
    catg = dint("catg", [1024, SE])
    xo_buf = [dint("xoA", [1024, TC]), dint("xoB", [1024, TC])]
    XTg = dint("XTg", [2048, TC])
    v3 = lambda ap: ap.rearrange("(c p) t -> p c t", p=128)
    with ExitStack() as st:
        kb = KB(nc, st)
        P = [kb.ps("P%d" % i, [128, 512]) for i in range(7)]
        mods_bufs = [kb.sb("mods%d" % L, [128, 2, 6, 8], F32) for L in range(depth)]
        msel = kb.sb("msel", [128, 2], F32)
        kb.dma("sp", msel.t[:], msel_in, writes=[msel])
        kb.pfx = "ad_"
        emit_adaln(kb, nc, cin2, ada_w, ada_b48, mods_bufs, P)
        catg_tok = Buf(None, "catg_tok")
        xtg_tok = Buf(None, "xtg_tok")
        for L in range(depth):
            W = LW[L]
            xsrc = XTg0 if L == 0 else XTg
            xv = xsrc.rearrange("(c ph s i) t -> ph s i c t", ph=2, s=2, i=64)

            def load_x(bi, xi, xv=xv):
                for (s, e, j, t0) in _gsegs(bi * TB, (bi + 1) * TB):
                    for ph in range(2):
                        kb.dma("sp", xi.t[ph * 64:(ph + 1) * 64, :, s - bi * TB:e - bi * TB], xv[ph, j][:, :, t0:t0 + (e - s)],
                               reads=[xtg_tok], writes=[xi])

            kb.pfx = "L%dB_" % L
            if L % 2 == 0:
                emit_inproj(kb, nc, None, W["w_in"], mods_bufs[L], 0, pT, vtm, FM_E, TM_E, P, load_x=load_x)
                emit_gla(kb, nc, pT, vtm, W["wdec"], W["bdec"], W["glag"], masks, ident, ofs, obs, catp, P)
                emit_gqa(kb, nc, pT, vtm, W["qkg"], ropeR, cosT, sinT, catp, P)
                rows = EVEN_CHUNK_ROWS
            else:
                emit_inproj(kb, nc, None, W["w_in"], mods_bufs[L], 0, pT, None, FM_O, 0, P, load_x=load_x)
                emit_conv(kb, nc, pT, cT, W["convw"], W["convb"], P)
                emit_ssd(kb, nc, pT, cT, W["dtb"], W["alog"], W["dsk"], W["ng"], masks, ident, yfs, ybs, catp, P)
                emit_fnet(kb, nc, pT, W["fg"], fconst, catp, P)
                rows = ODD_CHUNK_ROWS
            for k in range(16):
                kb.collective("AllGather", catp[k * 32:(k + 1) * 32, :], catg[k * 64:(k + 1) * 64, :], PAIRS, writes=[catg_tok])

            def load_cat(bi, catb, r, vf, rows=rows):
                for j, dst in ((0, r), (1, vf)):
                    for c in range(8):
                        srank, rho0 = rows[c] // 512, rows[c] % 512
                        for (lo, hi, gc) in _csegs(bi, j):
                            for q in range(4):
                                g0 = (rho0 // 32 + q) * 64 + srank * 32
                                kb.dma("sp", dst.t[q * 32:(q + 1) * 32, c, lo:hi], catg[g0:g0 + 32, gc:gc + (hi - lo)],
                                       reads=[catg_tok], writes=[dst])
                kb.op("dve", lambda e: e.tensor_scalar(out=r.t[:], in0=r.t[:], scalar1=msel.t[:, 0:1], scalar2=None, op0=ALU.mult),
                      reads=[r, msel], writes=[r])
                kb.op("dve", lambda e: e.scalar_tensor_tensor(out=catb.t[:], in0=vf.t[:], scalar=msel.t[:, 1:2], in1=r.t[:],
                                                              op0=ALU.mult, op1=ALU.add), reads=[vf, msel, r], writes=[catb])

            last = L == depth - 1
            xo_ap = xout if last else xo_buf[L % 2]
            xin_ap = xown0 if L == 0 else xo_buf[(L - 1) % 2]
            D = dict(xT=v3(xin_ap), xo=v3(xo_ap), w_o=W["w_o"], lnp=W["lnp"], rw=rw, rbias=rbias, sel=sel_in, ident=ident,
                     wg=W["wg"], wu=W["wu"], wd=W["wd"], xmid=xmid)
            kb.pfx = "L%dC_" % L
            with ExitStack() as stc:
                p7 = Buf(stc.enter_context(nc.psum_tensor("ps_L%dC_P7" % L, [128, 512], F32)), "P7")
                xo_tok = Buf(None, "xo_tok")
                emit_stageC(kb, nc, P + [p7], D, mods_buf=mods_bufs[L], load_cat=load_cat, xo_tok=xo_tok)
            if not last:
                for k in range(16):
                    kb.collective("AllGather", xo_ap[k * 64:(k + 1) * 64, :], XTg[k * 128:(k + 1) * 128, :], PAIRS,
                                  reads=[xo_tok], writes=[xtg_tok])
        kb.finish()
        print("fused instructions:", kb.n_inst, dict(kb.cnt))
    return nc


def kernel(x, c, ctx, c_ctx, ada_w, ada_b, ln_g, ln_b, ev_w_in, ev_w_o, gla_w_decay, gla_b_decay, gla_norm_g, gqa_q_norm_g,
                 gqa_k_norm_g, od_w_in, od_w_o, ssd_conv_w, ssd_conv_b, ssd_dt_bias, ssd_a_log, ssd_d, ssd_norm_g, fnet_norm_g,
                 router_w, router_b, exp_w_gate, exp_w_up, exp_w_down, _depth=4):
    f32 = lambda a: np.ascontiguousarray(np.asarray(a), np.float32)
    x, c, ctx, c_ctx = f32(x), f32(c), f32(ctx), f32(c_ctx)
    NB, DEPTH = 4, _depth
    cores = list(range(8))
    cosT, sinT = const_rope_tables()
    shared = dict(ada_w=f32(ada_w), ada_b48=np.ascontiguousarray(f32(ada_b).reshape(4, 48, 128).transpose(0, 2, 1)),
                  masks=const_masks(), ident=np.eye(128, dtype=np.float32), ropeR=const_ropeR(), cosT=cosT, sinT=sinT,
                  rw=f32(router_w), rbias=np.ascontiguousarray(np.broadcast_to(f32(router_b)[None, :], (128, 16))), sel=const_sel())
    shared.update(const_fnet())
    for L in range(DEPTH):
        i = L // 2
        shared["w_o%d" % L] = f32(ev_w_o[i]) if L % 2 == 0 else f32(od_w_o[i])
        shared["lnp%d" % L] = np.ascontiguousarray(np.stack([pack_vec(f32(ln_g[L])), pack_vec(f32(ln_b[L]))], 1))
        shared["wg%d" % L], shared["wu%d" % L], shared["wd%d" % L] = f32(exp_w_gate[L]), f32(exp_w_up[L]), f32(exp_w_down[L])
    dummy_c = dict(masks=None, ident=None, ropeR=None, cosT=None, sinT=None, fnet={})
    ims = []
    for core in cores:
        b, r = core // 2, core % 2
        im = dict(shared)
        cc = np.stack([c_ctx, c[b]], 0)
        im["cin2"] = np.ascontiguousarray(cc.reshape(2, 8, 128).transpose(2, 1, 0))
        halves = [np.concatenate([ctx[b][j * 128:(j + 1) * 128], x[b][j * 4096:(j + 1) * 4096]], 0).T for j in range(2)]
        im["XTg0"] = np.ascontiguousarray(np.stack([h_.reshape(16, 64, TC) for h_ in halves], 1).reshape(2048, TC))
        im["xown0"] = np.ascontiguousarray(halves[r])
        im["msel"] = np.ascontiguousarray(np.broadcast_to(np.array([1.0 - r, float(r)], np.float32)[None, :], (128, 2)))
        for L in range(DEPTH):
            i = L // 2
            if L % 2 == 0:
                e = even_core_inputs(r, None, None, f32(ev_w_in[i]), f32(gla_w_decay[i]), f32(gla_b_decay[i]), f32(gla_norm_g[i]),
                                     f32(gqa_q_norm_g[i]), f32(gqa_k_norm_g[i]), dummy_c)
                for k in ("w_in", "wdec", "bdec", "glag", "qkg"):
                    im["%s%d" % (k, L)] = e[k]
            else:
                o = odd_core_inputs(r, None, None, f32(od_w_in[i]), f32(ssd_conv_w[i]), f32(ssd_conv_b[i]), f32(ssd_dt_bias[i]),
                                    f32(ssd_a_log[i]), f32(ssd_d[i]), f32(ssd_norm_g[i]), f32(fnet_norm_g[i]), dummy_c)
                for k in ("w_in", "convw", "convb", "dtb", "alog", "dsk", "ng", "fg"):
                    im["%s%d" % (k, L)] = o[k]
        ims.append(im)
    res = run_bass_kernel_spmd(_prog("fused%d" % DEPTH, lambda: build_fused(DEPTH)), ims, core_ids=cores)
    out = np.empty((NB, 8192, 1024), np.float32)
    for core in cores:
        b, r = core // 2, core % 2
        out[b, r * 4096:(r + 1) * 4096] = np.asarray(res.results[core]["xout"])[:, 128:].T
    return out
```

```python
import numpy as np
from contextlib import ExitStack
import concourse.bass as bass
import concourse.mybir as mybir
from concourse.bass_utils import run_bass_kernel_spmd

F32 = mybir.dt.float32
BF16 = mybir.dt.bfloat16
I32 = mybir.dt.int32
AF = mybir.ActivationFunctionType
ALU = mybir.AluOpType
AX = mybir.AxisListType


SAME_ENG_SYNC = True


class Buf:
    __slots__ = ("t", "name", "lw", "rd")

    def __init__(self, t, name):
        self.t = t
        self.name = name
        self.lw = None
        self.rd = {}


class KB:
    ENG = ("pe", "dve", "act", "pool", "sp")

    def __init__(self, nc, st, ndma=12):
        self.nc = nc
        self.st = st
        self.st0 = st
        self.pfx = ""
        self.e = {"pe": nc.tensor, "dve": nc.vector, "act": nc.scalar, "pool": nc.gpsimd, "sp": nc.sync}
        self.sem = {}
        self.cnt = {}
        for k in ("pe", "dve", "act", "pool"):
            self.sem[k] = st.enter_context(nc.semaphore("s_" + k))
            self.cnt[k] = 0
        self.dsem = [st.enter_context(nc.semaphore("d%d" % i)) for i in range(ndma)]
        self.dcnt = [0] * ndma
        self.drr = 0
        self.waited = {k: {} for k in self.ENG}
        self.out_tickets = []
        self.n_inst = 0

    def sb(self, name, shape, dt):
        return Buf(self.st.enter_context(self.nc.sbuf_tensor("sb_" + self.pfx + name, list(shape), dt)), name)

    def ps(self, name, shape, dt=F32):
        return Buf(self.st.enter_context(self.nc.psum_tensor("ps_" + self.pfx + name, list(shape), dt)), name)

    def _semobj(self, key):
        return self.sem[key] if isinstance(key, str) else self.dsem[key]

    def _wait(self, eng, key, val):
        if key == eng and (eng == "pe" or not SAME_ENG_SYNC):
            return
        w = self.waited[eng]
        if w.get(key, 0) >= val:
            return
        self.e[eng].wait_ge(self._semobj(key), val)
        w[key] = val

    def _deps(self, eng, reads, writes):
        deps = {}
        for b in reads:
            if b.lw is not None and deps.get(b.lw[0], 0) < b.lw[1]:
                deps[b.lw[0]] = b.lw[1]
        for b in writes:
            if b.lw is not None and deps.get(b.lw[0], 0) < b.lw[1]:
                deps[b.lw[0]] = b.lw[1]
            for k, v in b.rd.items():
                if deps.get(k, 0) < v:
                    deps[k] = v
        for k, v in deps.items():
            self._wait(eng, k, v)

    def _mark(self, ticket, reads, writes):
        k, v = ticket
        for b in writes:
            b.lw = ticket
            b.rd = {}
        for b in reads:
            if b.rd.get(k, 0) < v:
                b.rd[k] = v

    def op(self, eng, fn, reads=(), writes=(), sig=True):
        self._deps(eng, reads, writes)
        inst = fn(self.e[eng])
        self.n_inst += 1
        if sig:
            self.cnt[eng] += 1
            inst.then_inc(self.sem[eng], 1)
            ticket = (eng, self.cnt[eng])
        else:
            ticket = (eng, self.cnt[eng] + 1)
        self._mark(ticket, reads, writes)
        return ticket

    def dma(self, q, out, in_, reads=(), writes=(), is_out=False):
        i = self.drr
        self.drr = (self.drr + 1) % len(self.dsem)
        if self.dcnt[i] > 0:
            self._wait(q, i, self.dcnt[i])
        self._deps(q, reads, writes)
        self.dcnt[i] += 16
        self.e[q].dma_start(out=out, in_=in_).then_inc(self.dsem[i], 16)
        self.n_inst += 1
        ticket = (i, self.dcnt[i])
        self._mark(ticket, reads, writes)
        if is_out:
            self.out_tickets.append(ticket)
        return ticket

    def collective(self, kind, ins_ap, outs_ap, groups, reads=(), writes=()):
        if "cc" not in self.sem:
            self.sem["cc"] = self.st0.enter_context(self.nc.semaphore("s_cc"))
            self.cnt["cc"] = 0
        self._deps("pool", reads, writes)
        self.cnt["cc"] += 1
        self.nc.gpsimd.collective_compute(kind, ALU.bypass, replica_groups=groups, ins=[ins_ap], outs=[outs_ap]).then_inc(self.sem["cc"], 1)
        self.n_inst += 1
        ticket = ("cc", self.cnt["cc"])
        self._mark(ticket, reads, writes)
        return ticket

    def barrier(self):
        for eng in self.ENG:
            for i, c in enumerate(self.dcnt):
                if c > 0:
                    self._wait(eng, i, c)
            for k in self.cnt:
                if self.cnt[k] > 0 and k != eng:
                    self._wait(eng, k, self.cnt[k])

    def sb_in(self, st, name, shape, dt):
        return Buf(st.enter_context(self.nc.sbuf_tensor("sb_" + self.pfx + name, list(shape), dt)), name)

    def finish(self):
        for i, c in enumerate(self.dcnt):
            if c > 0:
                self._wait("sp", i, c)
        for k in self.cnt:
            if self.cnt[k] > 0:
                self._wait("sp", k, self.cnt[k])


ALPHA = (2.0 * 4) ** 0.25
LN_EPS = 1e-6
TB = 384
TC = 4224


def _din(nc, name, shape, dt=F32):
    return nc.dram_tensor(name, list(shape), dt, kind="ExternalInput").ap()


def _dout(nc, name, shape, dt=F32):
    return nc.dram_tensor(name, list(shape), dt, kind="ExternalOutput").ap()


def _segs(bi):
    return [(0, 128, 0), (128, TB, 1)] if bi == 0 else [(0, TB, 1)]


class LNCtx:
    def __init__(self, kb, pS1, pS2):
        self.kb = kb
        self.pS1, self.pS2 = pS1, pS2
        self.rb = kb.sb("ln_rb", [128, 8, TB], BF16)
        self.rsq = kb.sb("ln_rsq", [128, 8, TB], BF16)
        self.mean = kb.sb("ln_mean", [128, TB], F32)
        self.m2 = kb.sb("ln_m2", [128, TB], F32)
        self.rstd = kb.sb("ln_rstd", [128, TB], F32)
        self.nmr = kb.sb("ln_nmr", [128, TB], F32)
        self.ones = kb.sb("ln_ones", [128, 128], BF16)
        self.eps = kb.sb("ln_eps", [128, 1], F32)
        kb.op("pool", lambda e: e.memset(self.ones.t[:], 1.0 / 1024.0), writes=[self.ones])
        kb.op("pool", lambda e: e.memset(self.eps.t[:], LN_EPS / (ALPHA * ALPHA)), writes=[self.eps])

    def normalize(self, r):
        kb = self.kb
        for c in range(8):
            kb.op("act", lambda e, c=c: e.activation(out=self.rb.t[:, c, :], in_=r.t[:, c, :], func=AF.Copy),
                  reads=[r], writes=[self.rb])
            kb.op("act", lambda e, c=c: e.activation(out=self.rsq.t[:, c, :], in_=r.t[:, c, :], func=AF.Square),
                  reads=[r], writes=[self.rsq])
        for c in range(8):
            kb.op("pe", lambda e, c=c: e.matmul(self.pS1.t[:, 0:TB], self.ones.t[:], self.rb.t[:, c, :],
                                                start=(c == 0), stop=(c == 7)),
                  reads=[self.ones, self.rb], writes=[self.pS1], sig=(c == 7))
        for c in range(8):
            kb.op("pe", lambda e, c=c: e.matmul(self.pS2.t[:, 0:TB], self.ones.t[:], self.rsq.t[:, c, :],
                                                start=(c == 0), stop=(c == 7)),
                  reads=[self.ones, self.rsq], writes=[self.pS2], sig=(c == 7))
        kb.op("act", lambda e: e.activation(out=self.mean.t[:], in_=self.pS1.t[:, 0:TB], func=AF.Copy),
              reads=[self.pS1], writes=[self.mean])
        kb.op("dve", lambda e: e.tensor_tensor(out=self.m2.t[:], in0=self.mean.t[:], in1=self.mean.t[:], op=ALU.mult),
              reads=[self.mean], writes=[self.m2])
        kb.op("dve", lambda e: e.tensor_tensor(out=self.m2.t[:], in0=self.pS2.t[:, 0:TB], in1=self.m2.t[:],
                                               op=ALU.subtract),
              reads=[self.pS2, self.m2], writes=[self.m2])
        kb.op("act", lambda e: e.activation(out=self.m2.t[:], in_=self.m2.t[:], func=AF.Sqrt, bias=self.eps.t[:, 0:1]),
              reads=[self.m2, self.eps], writes=[self.m2])
        kb.op("dve", lambda e: e.reciprocal(out=self.rstd.t[:], in_=self.m2.t[:]), reads=[self.m2], writes=[self.rstd])
        kb.op("dve", lambda e: e.scalar_tensor_tensor(out=self.nmr.t[:], in0=self.mean.t[:], scalar=-1.0,
                                                      in1=self.rstd.t[:], op0=ALU.mult, op1=ALU.mult),
              reads=[self.mean, self.rstd], writes=[self.nmr])
        for c in range(8):
            kb.op("dve", lambda e, c=c: e.tensor_tensor(out=r.t[:, c, :], in0=r.t[:, c, :], in1=self.rstd.t[:],
                                                        op=ALU.mult), reads=[r, self.rstd], writes=[r])
            kb.op("dve", lambda e, c=c: e.tensor_tensor(out=r.t[:, c, :], in0=r.t[:, c, :], in1=self.nmr.t[:],
                                                        op=ALU.add), reads=[r, self.nmr], writes=[r])


def emit_stageC(kb, nc, P, D, nblk=11, sbs=(3, 3, 3, 2), nexp=16, mods_buf=None, load_cat=None, xo_tok=None):
    catT, xT, xo, w_o, mods, lnp, rw, rbias, sel_in, ident_in, wg, wu, wd, xmid_d = (
        D.get(k) for k in ("catT", "xT", "xo", "w_o", "mods", "lnp", "rw", "rbias", "sel", "ident", "wg", "wu", "wd", "xmid"))
    if xo_tok is None:
        xo_tok = Buf(None, "xo_tok")
    with ExitStack() as st:
        old_st = kb.st
        kb.st = st
        ln = LNCtx(kb, P[2], P[3])
        SBT = max(sbs) * TB
        wo_sb = kb.sb("wo", [128, 8, 1024], BF16)
        mods_sb = mods_buf if mods_buf is not None else kb.sb("mods", [128, 2, 6, 8], F32)
        lnp_sb = kb.sb("lnp", [128, 2, 2, 8], F32)
        rw_sb = kb.sb("rw", [128, 8, 16], F32)
        rb_sb = kb.sb("rbias", [128, 16], F32)
        sel_sb = kb.sb("sel", [128, 16 * 128], F32)
        kb.op("pool", lambda e: e.memset(sel_sb.t[:], 0.0), writes=[sel_sb])
        id_sb = kb.sb("ident", [128, 128], F32)
        ga = kb.sb("ga", [128, 2, 2, 8], F32)
        gp = kb.sb("gp", [128, 2, 8], F32)
        bp = kb.sb("bp", [128, 2, 8], F32)
        kb.dma("pool", wo_sb.t[:], w_o, writes=[wo_sb])
        if mods_buf is None:
            kb.dma("sp", mods_sb.t[:], mods, writes=[mods_sb])
        kb.dma("sp", lnp_sb.t[:], lnp, writes=[lnp_sb])
        kb.dma("sp", rw_sb.t[:], rw, writes=[rw_sb])
        kb.dma("sp", rb_sb.t[:], rbias, writes=[rb_sb])
        kb.dma("sp", sel_sb.t[0:16, :], sel_in, writes=[sel_sb])
        kb.dma("sp", id_sb.t[:], ident_in, writes=[id_sb])
        for w in range(2):
            kb.op("dve", lambda e, w=w: e.tensor_scalar(out=ga.t[:, 0, w, :], in0=mods_sb.t[:, w, 2, :], scalar1=1.0 / ALPHA,
                                                        scalar2=None, op0=ALU.mult), reads=[mods_sb], writes=[ga])
            kb.op("dve", lambda e, w=w: e.tensor_scalar(out=ga.t[:, 1, w, :], in0=mods_sb.t[:, w, 5, :], scalar1=1.0 / ALPHA,
                                                        scalar2=None, op0=ALU.mult), reads=[mods_sb], writes=[ga])
            kb.op("dve", lambda e, w=w: e.scalar_tensor_tensor(out=gp.t[:, w, :], in0=mods_sb.t[:, w, 4, :], scalar=1.0,
                                                               in1=lnp_sb.t[:, 0, 0, :], op0=ALU.add, op1=ALU.mult),
                  reads=[mods_sb, lnp_sb], writes=[gp])
            kb.op("dve", lambda e, w=w: e.scalar_tensor_tensor(out=bp.t[:, w, :], in0=mods_sb.t[:, w, 4, :], scalar=1.0,
                                                               in1=lnp_sb.t[:, 1, 0, :], op0=ALU.add, op1=ALU.mult),
                  reads=[mods_sb, lnp_sb], writes=[bp])
            kb.op("dve", lambda e, w=w: e.tensor_tensor(out=bp.t[:, w, :], in0=bp.t[:, w, :], in1=mods_sb.t[:, w, 3, :],
                                                        op=ALU.add), reads=[bp, mods_sb], writes=[bp])
        xin = kb.sb("xin", [128, 8, TB], F32)
        catb = kb.sb("catb", [128, 8, TB], BF16)
        r = kb.sb("r", [128, 8, TB], F32)
        vf = kb.sb("vf", [128, 8, TB], F32)
        vb = kb.sb("vb", [128, 8, SBT], BF16)
        yacc = kb.sb("yacc", [128, 8, SBT], F32)
        gT = kb.sb("gT", [128, SBT], F32)
        kb.op("pool", lambda e: e.memset(gT.t[:], 0.0), writes=[gT])
        NW = 3
        wbuf = [kb.sb("wbuf%d" % i, [128, 8 * 768], BF16) for i in range(NW)]
        wrr = [0]
        rt = {n: kb.sb("rt_" + n, [128, 16], F32) for n in ("s", "sel", "c1", "c2", "c3", "min", "selm", "mask", "sg", "gates")}
        rgs = kb.sb("rt_gs", [128, 4], F32)
        rgm = kb.sb("rt_gm", [128, 4], F32)
        rmx = kb.sb("rt_mx", [128, 1], F32)
        rden = kb.sb("rt_den", [128, 1], F32)
        gb = [kb.sb("gb%d" % i, [128, TB], F32) for i in range(2)]
        sg_t = [kb.sb("sgt%d" % i, [128, TB], F32) for i in range(2)]
        aj = [kb.sb("aj%d" % i, [128, TB], BF16) for i in range(6)]
        xmid_tok = [Buf(None, "xmid%d" % i) for i in range(nblk)]

        def v4(b):
            return b.t[:].rearrange("p (g e) -> p g e", e=4)

        def route_tile(vcols, gcol0):
            pR = P[4]
            for k in range(8):
                kb.op("pe", lambda e, k=k: e.matmul(pR.t[:, 0:16], vf.t[:, k, vcols], rw_sb.t[:, k, :],
                                                    start=(k == 0), stop=(k == 7)),
                      reads=[vf, rw_sb], writes=[pR], sig=(k == 7))
            kb.op("act", lambda e: e.activation(out=rt["s"].t[:], in_=pR.t[:, 0:16], func=AF.Sigmoid),
                  reads=[pR], writes=[rt["s"]])
            kb.op("dve", lambda e: e.tensor_tensor(out=rt["sel"].t[:], in0=rt["s"].t[:], in1=rb_sb.t[:], op=ALU.add),
                  reads=[rt["s"], rb_sb], writes=[rt["sel"]])
            sel4 = v4(rt["sel"])
            for k, cn in ((1, "c1"), (2, "c2"), (3, "c3")):
                c4 = v4(rt[cn])
                kb.op("dve", lambda e, k=k, c4=c4: e.tensor_tensor(out=c4[:, :, 0:4 - k], in0=sel4[:, :, k:4],
                                                                   in1=sel4[:, :, 0:4 - k], op=ALU.is_gt),
                      reads=[rt["sel"]], writes=[rt[cn]])
                kb.op("dve", lambda e, k=k, c4=c4: e.tensor_tensor(out=c4[:, :, 4 - k:4], in0=sel4[:, :, 0:k],
                                                                   in1=sel4[:, :, 4 - k:4], op=ALU.is_gt),
                      reads=[rt["sel"]], writes=[rt[cn]])
            kb.op("dve", lambda e: e.tensor_tensor(out=rt["c1"].t[:], in0=rt["c1"].t[:], in1=rt["c2"].t[:], op=ALU.add),
                  reads=[rt["c1"], rt["c2"]], writes=[rt["c1"]])
            kb.op("dve", lambda e: e.tensor_tensor(out=rt["c1"].t[:], in0=rt["c1"].t[:], in1=rt["c3"].t[:], op=ALU.add),
                  reads=[rt["c1"], rt["c3"]], writes=[rt["c1"]])
            kb.op("dve", lambda e: e.tensor_single_scalar(out=rt["min"].t[:], in_=rt["c1"].t[:], scalar=1.5, op=ALU.is_lt),
                  reads=[rt["c1"]], writes=[rt["min"]])
            kb.op("dve", lambda e: e.tensor_tensor(out=rt["selm"].t[:], in0=rt["sel"].t[:], in1=rt["min"].t[:], op=ALU.mult),
                  reads=[rt["sel"], rt["min"]], writes=[rt["selm"]])
            kb.op("dve", lambda e: e.tensor_reduce(out=rgs.t[:], in_=v4(rt["selm"]), axis=AX.X, op=ALU.add),
                  reads=[rt["selm"]], writes=[rgs])
            kb.op("dve", lambda e: e.tensor_reduce(out=rmx.t[:], in_=rgs.t[:], axis=AX.X, op=ALU.max),
                  reads=[rgs], writes=[rmx])
            kb.op("dve", lambda e: e.tensor_scalar(out=rgm.t[:], in0=rgs.t[:], scalar1=rmx.t[:, 0:1], scalar2=None,
                                                   op0=ALU.is_ge), reads=[rgs, rmx], writes=[rgm])
            m4 = v4(rt["mask"])
            min4 = v4(rt["min"])
            for g in range(4):
                kb.op("dve", lambda e, g=g: e.tensor_scalar(out=m4[:, g, :], in0=min4[:, g, :], scalar1=rgm.t[:, g:g + 1],
                                                            scalar2=None, op0=ALU.mult),
                      reads=[rt["min"], rgm], writes=[rt["mask"]])
            kb.op("dve", lambda e: e.tensor_tensor(out=rt["sg"].t[:], in0=rt["s"].t[:], in1=rt["mask"].t[:], op=ALU.mult),
                  reads=[rt["s"], rt["mask"]], writes=[rt["sg"]])
            kb.op("dve", lambda e: e.tensor_reduce(out=rden.t[:], in_=rt["sg"].t[:], axis=AX.X, op=ALU.add),
                  reads=[rt["sg"]], writes=[rden])
            kb.op("dve", lambda e: e.reciprocal(out=rden.t[:], in_=rden.t[:]), reads=[rden], writes=[rden])
            kb.op("dve", lambda e: e.tensor_scalar(out=rt["gates"].t[:], in0=rt["sg"].t[:], scalar1=rden.t[:, 0:1],
                                                   scalar2=None, op0=ALU.mult), reads=[rt["sg"], rden], writes=[rt["gates"]])
            kb.op("pe", lambda e: e.transpose(pR.t[0:16, 128:256], rt["gates"].t[:], id_sb.t[:]),
                  reads=[rt["gates"], id_sb], writes=[pR])
            kb.op("act", lambda e: e.activation(out=gT.t[0:16, gcol0:gcol0 + 128], in_=pR.t[0:16, 128:256], func=AF.Copy),
                  reads=[pR], writes=[gT])

        def phase1(bi, lb):
            cols = slice(bi * TB, (bi + 1) * TB)
            kb.dma("sp", xin.t[:], xT[:, :, cols], writes=[xin])
            if load_cat is None:
                kb.dma("pool", catb.t[:], catT[:, :, cols], writes=[catb])
            else:
                load_cat(bi, catb, r, vf)
            for d in range(8):
                pA = P[d % 2]
                for k in range(8):
                    kb.op("pe", lambda e, k=k, d=d, pA=pA: e.matmul(pA.t[:, 0:TB], wo_sb.t[:, k, d * 128:(d + 1) * 128],
                                                                    catb.t[:, k, :], start=(k == 0), stop=(k == 7)),
                          reads=[wo_sb, catb], writes=[pA], sig=(k == 7))
                for (lo, hi, w) in _segs(bi):
                    kb.op("dve", lambda e, d=d, lo=lo, hi=hi, w=w, pA=pA: e.scalar_tensor_tensor(
                        out=r.t[:, d, lo:hi], in0=pA.t[:, lo:hi], scalar=ga.t[:, 0, w, d:d + 1], in1=xin.t[:, d, lo:hi],
                        op0=ALU.mult, op1=ALU.add), reads=[pA, ga, xin], writes=[r])
            ln.normalize(r)
            for c in range(8):
                for (lo, hi, w) in _segs(bi):
                    kb.op("act", lambda e, c=c, lo=lo, hi=hi, w=w: e.activation(
                        out=vf.t[:, c, lo:hi], in_=r.t[:, c, lo:hi], func=AF.Identity,
                        scale=gp.t[:, w, c:c + 1], bias=bp.t[:, w, c:c + 1]), reads=[r, gp, bp], writes=[vf])
                kb.op("pool", lambda e, c=c: e.tensor_copy(out=vb.t[:, c, lb * TB:(lb + 1) * TB], in_=vf.t[:, c, :]),
                      reads=[vf], writes=[vb])
                kb.op("act", lambda e, c=c: e.activation(out=r.t[:, c, :], in_=r.t[:, c, :], func=AF.Identity,
                                                         scale=lnp_sb.t[:, 0, 0, c:c + 1], bias=lnp_sb.t[:, 1, 0, c:c + 1]),
                      reads=[r, lnp_sb], writes=[r])
            kb.dma("sp", xmid_d[:, :, cols], r.t[:], reads=[r], writes=[xmid_tok[bi]])
            for tt in range(TB // 128):
                route_tile(slice(tt * 128, (tt + 1) * 128), lb * TB + tt * 128)

        wscr = D.get("wscr")
        scr_tok = {}

        def load_w(src, nchunk, ncol, ex, m, first):
            wb = wbuf[wrr[0] % NW]
            wrr[0] += 1
            view = wb.t[:, 0:nchunk * ncol].rearrange("p (c n) -> p c n", n=ncol)
            if wscr is None:
                kb.dma("pool", view, src.rearrange("(c p) n -> p c n", p=128), writes=[wb])
            elif first:
                tok = scr_tok[(ex, m)] = Buf(None, "wscr_tok")
                kb.dma("pool", view, src.rearrange("(c p) n -> p c n", p=128), writes=[wb])
                kb.dma("sp", wscr[ex, m], wb.t[:], reads=[wb], writes=[tok])
            else:
                kb.dma("sp", wb.t[:], wscr[ex, m], reads=[scr_tok[(ex, m)]], writes=[wb])
            return wb, view

        def moe(nb_, first=True):
            for ex in range(nexp):
                wgb, wgv = load_w(wg[ex], 8, 768, ex, 0, first)
                wub, wuv = load_w(wu[ex], 8, 768, ex, 1, first)
                wdb, wdv = load_w(wd[ex], 6, 1024, ex, 2, first)
                for lb in range(nb_):
                    cs = slice(lb * TB, (lb + 1) * TB)
                    g_ = gb[lb % 2]
                    kb.op("pe", lambda e, ex=ex, cs=cs: e.matmul(P[5].t[:, 0:TB], sel_sb.t[:, ex * 128:(ex + 1) * 128],
                                                                 gT.t[:, cs], start=True, stop=True),
                          reads=[sel_sb, gT], writes=[P[5]])
                    kb.op("act", lambda e, g_=g_: e.activation(out=g_.t[:], in_=P[5].t[:, 0:TB], func=AF.Copy),
                          reads=[P[5]], writes=[g_])
                    ajs = []
                    for j in range(6):
                        pG, pU = P[0 + (j % 2)], P[2 + (j % 2)]
                        for k in range(8):
                            kb.op("pe", lambda e, k=k, j=j, pG=pG: e.matmul(pG.t[:, 0:TB], wgv[:, k, j * 128:(j + 1) * 128],
                                                                            vb.t[:, k, cs], start=(k == 0), stop=(k == 7)),
                                  reads=[wgb, vb], writes=[pG], sig=(k == 7))
                        for k in range(8):
                            kb.op("pe", lambda e, k=k, j=j, pU=pU: e.matmul(pU.t[:, 0:TB], wuv[:, k, j * 128:(j + 1) * 128],
                                                                            vb.t[:, k, cs], start=(k == 0), stop=(k == 7)),
                                  reads=[wub, vb], writes=[pU], sig=(k == 7))
                        s_ = sg_t[j % 2]
                        a_ = aj[j]
                        kb.op("act", lambda e, s_=s_, pG=pG: e.activation(out=s_.t[:], in_=pG.t[:, 0:TB], func=AF.Silu),
                              reads=[pG], writes=[s_])
                        kb.op("dve", lambda e, s_=s_, g_=g_: e.tensor_tensor(out=s_.t[:], in0=s_.t[:], in1=g_.t[:], op=ALU.mult),
                              reads=[s_, g_], writes=[s_])
                        kb.op("dve", lambda e, s_=s_, a_=a_, pU=pU: e.tensor_tensor(out=a_.t[:], in0=pU.t[:, 0:TB], in1=s_.t[:],
                                                                                    op=ALU.mult),
                              reads=[pU, s_], writes=[a_])
                        ajs.append(a_)
                    for d in range(8):
                        pY = P[6 + (d % 2)]
                        for j in range(6):
                            kb.op("pe", lambda e, j=j, d=d, pY=pY: e.matmul(pY.t[:, 0:TB], wdv[:, j, d * 128:(d + 1) * 128],
                                                                            ajs[j].t[:], start=(j == 0), stop=(j == 5)),
                                  reads=[wdb, ajs[j]], writes=[pY], sig=(j == 5))
                        if ex == 0:
                            kb.op("act", lambda e, d=d, pY=pY: e.activation(out=yacc.t[:, d, cs], in_=pY.t[:, 0:TB], func=AF.Copy),
                                  reads=[pY], writes=[yacc])
                        else:
                            kb.op("dve", lambda e, d=d, pY=pY: e.tensor_tensor(out=yacc.t[:, d, cs], in0=pY.t[:, 0:TB],
                                                                               in1=yacc.t[:, d, cs], op=ALU.add),
                                  reads=[pY, yacc], writes=[yacc])

        def phase3(bi, lb):
            cols = slice(bi * TB, (bi + 1) * TB)
            cs = slice(lb * TB, (lb + 1) * TB)
            kb.dma("sp", xin.t[:], xmid_d[:, :, cols], reads=[xmid_tok[bi]], writes=[xin])
            for d in range(8):
                for (lo, hi, w) in _segs(bi):
                    kb.op("dve", lambda e, d=d, lo=lo, hi=hi, w=w: e.scalar_tensor_tensor(
                        out=r.t[:, d, lo:hi], in0=yacc.t[:, d, lb * TB + lo:lb * TB + hi], scalar=ga.t[:, 1, w, d:d + 1],
                        in1=xin.t[:, d, lo:hi], op0=ALU.mult, op1=ALU.add), reads=[yacc, ga, xin], writes=[r])
            ln.normalize(r)
            for c in range(8):
                kb.op("act", lambda e, c=c: e.activation(out=r.t[:, c, :], in_=r.t[:, c, :], func=AF.Identity,
                                                         scale=lnp_sb.t[:, 0, 1, c:c + 1], bias=lnp_sb.t[:, 1, 1, c:c + 1]),
                      reads=[r, lnp_sb], writes=[r])
            kb.dma("sp", xo[:, :, cols], r.t[:], reads=[r], writes=[xo_tok], is_out=True)

        b0 = 0
        for nb_ in sbs:
            for lb in range(nb_):
                phase1(b0 + lb, lb)
            moe(nb_, first=(b0 == 0))
            for lb in range(nb_):
                phase3(b0 + lb, lb)
            b0 += nb_
        assert b0 == nblk


        kb.barrier()
        kb.st = old_st


def build_stageC(nblk=11, sbs=(3, 3, 3, 2), nexp=16):
    nc = bass.Bass("TRN2", target_bir_lowering=False)
    T = nblk * TB
    catT = _din(nc, "catT", [1024, T]).rearrange("(c p) t -> p c t", p=128)
    xT = _din(nc, "xT", [1024, T]).rearrange("(c p) t -> p c t", p=128)
    w_o = _din(nc, "w_o", [1024, 1024]).rearrange("(c p) n -> p c n", p=128)
    mods = _din(nc, "mods", [128, 2, 6, 8])
    lnp = _din(nc, "lnp", [128, 2, 2, 8])
    rw = _din(nc, "rw", [1024, 16]).rearrange("(c p) n -> p c n", p=128)
    rbias = _din(nc, "rbias", [128, 16])
    sel_in = _din(nc, "sel", [16, 16 * 128])
    ident_in = _din(nc, "ident", [128, 128])
    wg = _din(nc, "wg", [nexp, 1024, 768])
    wu = _din(nc, "wu", [nexp, 1024, 768])
    wd = _din(nc, "wd", [nexp, 768, 1024])
    xo = _dout(nc, "xo", [1024, T]).rearrange("(c p) t -> p c t", p=128)
    xmid_d = nc.dram_tensor("xmid", [1024, T], F32, kind="Internal").ap().rearrange("(c p) t -> p c t", p=128)
    D = dict(catT=catT, xT=xT, xo=xo, w_o=w_o, mods=mods, lnp=lnp, rw=rw, rbias=rbias, sel=sel_in, ident=ident_in, wg=wg, wu=wu, wd=wd,
             xmid=xmid_d, wscr=nc.dram_tensor("wscr", [nexp, 3, 128, 8 * 768], BF16, kind="Internal").ap())
    with ExitStack() as st:
        kb = KB(nc, st)
        P = [kb.ps("P%d" % i, [128, 512]) for i in range(8)]
        emit_stageC(kb, nc, P, D, nblk, sbs, nexp)
        kb.finish()
        print("stageC instructions:", kb.n_inst, dict(kb.cnt))
    return nc


def pack_vec(v):
    v = np.asarray(v, np.float32)
    n = v.shape[-1] // 128
    return np.ascontiguousarray(np.moveaxis(v.reshape(v.shape[:-1] + (n, 128)), -1, 0))


def pack_mods(m_ctx, m_lat):
    a = np.stack([np.asarray(m_ctx, np.float32).reshape(6, 8, 128), np.asarray(m_lat, np.float32).reshape(6, 8, 128)], 0)
    return np.ascontiguousarray(a.transpose(3, 0, 1, 2))


def const_sel():
    s = np.zeros((16, 16, 128), np.float32)
    for e in range(16):
        s[e, e, :] = 1.0
    return s.reshape(16, 16 * 128)


SE = 8448
NBE = SE // TB
NCTX = 256
FM_E = 864
TM_E = 320


def _segs_h(bi):
    return [(0, 256, 0), (256, TB, 1)] if bi == 0 else [(0, TB, 1)]


def _bwd_segs(bi):
    s0 = bi * TB
    if s0 + TB <= 8192:
        return [(0, TB, 256 + s0)]
    nlat = 8192 - s0
    return [(0, nlat, 256 + s0), (nlat, TB, 0)]


def emit_inproj(kb, nc, xT, w_in, mods_sb, midx, pT, vtm, nfm, ntm, P, nblk=NBE, load_x=None):
    with ExitStack() as st:
        ncol = nfm + ntm
        w_sb = kb.sb_in(st, "ip_w", [128, 8, ncol], BF16)
        kb.dma("pool", w_sb.t[:], w_in, writes=[w_sb])
        sc1 = kb.sb_in(st, "ip_sc1", [128, 2, 8], F32)
        for w in range(2):
            kb.op("dve", lambda e, w=w: e.tensor_scalar(out=sc1.t[:, w, :], in0=mods_sb.t[:, w, midx + 1, :], scalar1=1.0,
                                                        scalar2=None, op0=ALU.add), reads=[mods_sb], writes=[sc1])
        xin = [kb.sb_in(st, "ip_xin%d" % i, [128, 8, TB], F32) for i in range(2)]
        u = [kb.sb_in(st, "ip_u%d" % i, [128, 8, TB], BF16) for i in range(2)]
        stg = [kb.sb_in(st, "ip_stg%d" % i, [128, TB], F32) for i in range(3)]
        stv = [kb.sb_in(st, "ip_stv%d" % i, [128, max(ntm, 1)], BF16) for i in range(2)]
        nch = (nfm + 127) // 128
        si = 0
        vi = 0
        for bi in range(nblk):
            cols = slice(bi * TB, (bi + 1) * TB)
            xi, ub = xin[bi % 2], u[bi % 2]
            if load_x is None:
                kb.dma("sp", xi.t[:], xT[:, :, cols], writes=[xi])
            else:
                load_x(bi, xi)
            for c in range(8):
                for (lo, hi, w) in _segs_h(bi):
                    kb.op("act", lambda e, c=c, lo=lo, hi=hi, w=w, xi=xi, ub=ub: e.activation(
                        out=ub.t[:, c, lo:hi], in_=xi.t[:, c, lo:hi], func=AF.Identity,
                        scale=sc1.t[:, w, c:c + 1], bias=mods_sb.t[:, w, midx, c:c + 1]), reads=[xi, sc1, mods_sb], writes=[ub])
            for cc in range(nch):
                n = min(128, nfm - cc * 128)
                pA = P[cc % 2]
                for k in range(8):
                    kb.op("pe", lambda e, k=k, cc=cc, n=n, pA=pA, ub=ub: e.matmul(
                        pA.t[0:n, 0:TB], w_sb.t[:, k, cc * 128:cc * 128 + n], ub.t[:, k, :], start=(k == 0), stop=(k == 7)),
                        reads=[w_sb, ub], writes=[pA], sig=(k == 7))
                sg = stg[si % 3]
                si += 1
                eng = "act" if cc % 2 == 0 else "dve"
                if eng == "act":
                    kb.op("act", lambda e, n=n, pA=pA, sg=sg: e.activation(out=sg.t[0:n, :], in_=pA.t[0:n, 0:TB], func=AF.Copy),
                          reads=[pA], writes=[sg])
                else:
                    kb.op("dve", lambda e, n=n, pA=pA, sg=sg: e.tensor_copy(out=sg.t[0:n, :], in_=pA.t[0:n, 0:TB]),
                          reads=[pA], writes=[sg])
                kb.dma("sp", pT[cc * 128:cc * 128 + n, cols], sg.t[0:n, :], reads=[sg])
            for tt in range(TB // 128 if ntm > 0 else 0):
                pV = P[2 + (tt % 2)]
                for k in range(8):
                    kb.op("pe", lambda e, k=k, tt=tt, pV=pV, ub=ub: e.matmul(
                        pV.t[:, 0:ntm], ub.t[:, k, tt * 128:(tt + 1) * 128], w_sb.t[:, k, nfm:nfm + ntm],
                        start=(k == 0), stop=(k == 7)), reads=[w_sb, ub], writes=[pV], sig=(k == 7))
                sv = stv[vi % 2]
                vi += 1
                kb.op("dve", lambda e, pV=pV, sv=sv: e.tensor_copy(out=sv.t[:], in_=pV.t[:, 0:ntm]), reads=[pV], writes=[sv])
                r0 = bi * TB + tt * 128
                kb.dma("sp", vtm[r0:r0 + 128, :], sv.t[:], reads=[sv])
        kb.barrier()


def emit_gla(kb, nc, pT, vtm, wdec, bdec, glag, masks_in, ident_in, ofs, obs, cat_out, P):
    INV_TAU = 1.0 / 16.0
    with ExitStack() as st:
        sbn = lambda n, sh, dt: kb.sb_in(st, "gl_" + n, sh, dt)
        wdec_sb = sbn("wdec", [16, 2, 128], F32)
        nbdec = sbn("nbdec", [64, 2, 2], F32)
        g_sb = sbn("g", [128, 1], F32)
        one = sbn("one", [128, 1], F32)
        eps = sbn("eps", [128, 1], F32)
        ones128 = sbn("ones128", [128, 128], BF16)
        cmask = sbn("cmask", [64, TB], F32)
        mk = sbn("mk", [128, 2, 128], F32)
        idb = sbn("idb", [64, 64], BF16)
        kb.dma("sp", wdec_sb.t[:], wdec, writes=[wdec_sb])
        kb.dma("sp", nbdec.t[:], bdec, writes=[nbdec])
        kb.dma("sp", g_sb.t[:], glag, writes=[g_sb])
        kb.dma("sp", mk.t[:], masks_in, writes=[mk])
        kb.dma("pool", idb.t[:], ident_in[0:64, 0:64], writes=[idb])
        kb.op("dve", lambda e: e.tensor_scalar(out=nbdec.t[:], in0=nbdec.t[:], scalar1=-1.0, scalar2=None, op0=ALU.mult),
              reads=[nbdec], writes=[nbdec])
        kb.op("pool", lambda e: e.memset(one.t[:], 1.0), writes=[one])
        kb.op("pool", lambda e: e.memset(eps.t[:], 1e-6), writes=[eps])
        kb.op("pool", lambda e: e.memset(ones128.t[:], 1.0 / 128.0), writes=[ones128])
        kb.op("pool", lambda e: e.memset(cmask.t[:], 1.0), writes=[cmask])
        for n in range(TB // 128):
            kb.op("pool", lambda e, n=n: e.memset(cmask.t[:, n * 128:n * 128 + 1], 0.0), writes=[cmask])
        S32 = {}
        Sbf = {}
        bufs = {}
        for hd in range(2):
            for dr in range(2):
                key = (hd, dr)
                S32[key] = sbn("S32_%d%d" % key, [64, 128], F32)
                Sbf[key] = sbn("Sbf_%d%d" % key, [64, 128], BF16)
                kb.op("pool", lambda e, key=key: e.memset(S32[key].t[:], 0.0), writes=[S32[key]])
                kb.op("pool", lambda e, key=key: e.memset(Sbf[key].t[:], 0.0), writes=[Sbf[key]])
        for dr in range(2):
            d = {}
            for n, sh, dt in (("q", [64, TB], F32), ("k", [64, TB], F32), ("lx", [16, TB], F32), ("v", [128, 3, 128], BF16),
                              ("sp", [64, TB], F32), ("X", [64, TB], F32), ("tmp", [64, TB], F32), ("D1", [64, TB], F32),
                              ("D4", [64, TB], F32), ("E", [64, TB], F32), ("dec", [64, 3], F32),
                              ("qt", [64, TB], BF16), ("qh", [64, TB], BF16), ("kt", [64, TB], BF16), ("kh", [64, TB], BF16),
                              ("attm", [128, 128], BF16), ("khs", [128, 64], BF16), ("ost", [128, TB], F32)):
                d[n] = sbn("%s_%d" % (n, dr), sh, dt)
            bufs[dr] = d
        PK = Buf(st.enter_context(nc.psum_tensor("ps_" + kb.pfx + "gl_pk", [128, 64], BF16)), "gl_pk")

        def gla_block(hd, dr, bi):
            B = bufs[dr]
            key = (hd, dr)
            segs = [(0, TB, bi * TB)] if dr == 0 else _bwd_segs(bi)
            for (lo, hi, src) in segs:
                n = hi - lo
                kb.dma("sp", B["q"].t[:, lo:hi], pT[hd * 64:(hd + 1) * 64, src:src + n], writes=[B["q"]])
                kb.dma("sp", B["k"].t[:, lo:hi], pT[128 + hd * 64:128 + (hd + 1) * 64, src:src + n], writes=[B["k"]])
                kb.dma("sp", B["lx"].t[:, lo:hi], pT[832 + dr * 16:848 + dr * 16, src:src + n], writes=[B["lx"]])
                kb.dma("sp", B["v"].t[:, lo // 128:hi // 128, :],
                       vtm[src:src + n, hd * 128:(hd + 1) * 128].rearrange("(n p) d -> p n d", p=128), writes=[B["v"]])
            pz = P[0]
            kb.op("pe", lambda e: e.matmul(pz.t[0:64, 0:TB], wdec_sb.t[:, dr, hd * 64:(hd + 1) * 64], B["lx"].t[:],
                                           start=True, stop=True), reads=[wdec_sb, B["lx"]], writes=[pz])
            kb.op("act", lambda e: e.activation(out=B["sp"].t[:], in_=pz.t[0:64, 0:TB], func=AF.Exp, scale=-1.0,
                                                bias=nbdec.t[:, dr, hd:hd + 1]), reads=[pz, nbdec], writes=[B["sp"]])
            kb.op("act", lambda e: e.activation(out=B["sp"].t[:], in_=B["sp"].t[:], func=AF.Ln, bias=one.t[0:64, 0:1]),
                  reads=[B["sp"], one], writes=[B["sp"]])
            kb.op("dve", lambda e: e.tensor_tensor_scan(out=B["X"].t[:], data0=cmask.t[:], data1=B["sp"].t[:], initial=0.0,
                                                        op0=ALU.mult, op1=ALU.add), reads=[cmask, B["sp"]], writes=[B["X"]])
            X3 = B["X"].t[:].rearrange("p (n l) -> p n l", l=128)
            tmp3 = B["tmp"].t[:].rearrange("p (n l) -> p n l", l=128)
            D13 = B["D1"].t[:].rearrange("p (n l) -> p n l", l=128)
            D43 = B["D4"].t[:].rearrange("p (n l) -> p n l", l=128)
            kb.op("act", lambda e: e.activation(out=B["dec"].t[:].unsqueeze(2), in_=X3[:, :, 127:128], func=AF.Exp,
                                                scale=-INV_TAU), reads=[B["X"]], writes=[B["dec"]])
            if dr == 0:
                kb.op("dve", lambda e: e.tensor_tensor(out=D43, in0=X3, in1=X3[:, :, 127:128].broadcast_to([64, 3, 128]),
                                                       op=ALU.subtract), reads=[B["X"]], writes=[B["D4"]])
                Xb = B["X"]
                Xb3 = X3
            else:
                kb.op("dve", lambda e: e.tensor_tensor(out=tmp3, in0=X3, in1=X3[:, :, 127:128].broadcast_to([64, 3, 128]),
                                                       op=ALU.subtract), reads=[B["X"]], writes=[B["tmp"]])
                kb.op("dve", lambda e: e.tensor_tensor(out=B["D1"].t[:], in0=B["sp"].t[:], in1=B["tmp"].t[:], op=ALU.subtract),
                      reads=[B["sp"], B["tmp"]], writes=[B["D1"]])
                kb.op("dve", lambda e: e.tensor_tensor(out=D43, in0=D13, in1=X3[:, :, 127:128].broadcast_to([64, 3, 128]),
                                                       op=ALU.subtract), reads=[B["D1"], B["X"]], writes=[B["D4"]])
                kb.op("dve", lambda e: e.tensor_copy(out=B["tmp"].t[:], in_=B["D1"].t[:]), reads=[B["D1"]], writes=[B["tmp"]])
                Xb = B["tmp"]
                Xb3 = tmp3
            kb.op("dve", lambda e: e.tensor_tensor(out=D13, in0=Xb3, in1=Xb3[:, :, 63:64].broadcast_to([64, 3, 128]),
                                                   op=ALU.subtract), reads=[Xb], writes=[B["D1"]])
            for (src, scale, dst, base, isq) in ((B["D1"], -INV_TAU, B["qt"], B["q"], True), (B["D1"], INV_TAU, B["kt"], B["k"], False),
                                                 (Xb, -INV_TAU, B["qh"], B["q"], True), (B["D4"], INV_TAU, B["kh"], B["k"], False)):
                kb.op("act", lambda e, src=src, scale=scale: e.activation(out=B["E"].t[:], in_=src.t[:], func=AF.Exp, scale=scale),
                      reads=[src], writes=[B["E"]])
                if isq:
                    kb.op("dve", lambda e, dst=dst, base=base: e.scalar_tensor_tensor(
                        out=dst.t[:], in0=base.t[:], scalar=0.125, in1=B["E"].t[:], op0=ALU.mult, op1=ALU.mult),
                        reads=[base, B["E"]], writes=[dst])
                else:
                    kb.op("dve", lambda e, dst=dst, base=base: e.tensor_tensor(out=dst.t[:], in0=base.t[:], in1=B["E"].t[:],
                                                                               op=ALU.mult), reads=[base, B["E"]], writes=[dst])
            order = range(3) if dr == 0 else range(2, -1, -1)
            for ci in order:
                cs = slice(ci * 128, (ci + 1) * 128)
                pA, pO, pD = P[1 + dr], P[3 + dr], P[5 + dr]
                kb.op("pe", lambda e: e.matmul(pA.t[:, 0:128], B["kt"].t[:, cs], B["qt"].t[:, cs], start=True, stop=True),
                      reads=[B["kt"], B["qt"]], writes=[pA])
                kb.op("dve", lambda e: e.tensor_tensor(out=B["attm"].t[:], in0=pA.t[:, 0:128], in1=mk.t[:, dr, :], op=ALU.mult),
                      reads=[pA, mk], writes=[B["attm"]])
                kb.op("pe", lambda e: e.matmul(pO.t[:, 0:128], B["v"].t[:, ci, :], B["attm"].t[:], start=True, stop=False),
                      reads=[B["v"], B["attm"]], writes=[pO], sig=False)
                kb.op("pe", lambda e: e.matmul(pO.t[:, 0:128], Sbf[key].t[:], B["qh"].t[:, cs], start=False, stop=True),
                      reads=[Sbf[key], B["qh"]], writes=[pO])
                kb.op("act", lambda e: e.activation(out=B["ost"].t[:, cs], in_=pO.t[:, 0:128], func=AF.Copy),
                      reads=[pO], writes=[B["ost"]])
                kb.op("pe", lambda e: e.transpose(PK.t[:, 0:64], B["kh"].t[:, cs], idb.t[:]), reads=[B["kh"], idb], writes=[PK])
                kb.op("act", lambda e: e.activation(out=B["khs"].t[:], in_=PK.t[:, 0:64], func=AF.Copy),
                      reads=[PK], writes=[B["khs"]])
                kb.op("pe", lambda e: e.matmul(pD.t[0:64, 0:128], B["khs"].t[:], B["v"].t[:, ci, :], start=True, stop=True),
                      reads=[B["khs"], B["v"]], writes=[pD])
                kb.op("dve", lambda e: e.scalar_tensor_tensor(out=S32[key].t[:], in0=S32[key].t[:], scalar=B["dec"].t[:, ci:ci + 1],
                                                              in1=pD.t[0:64, 0:128], op0=ALU.mult, op1=ALU.add),
                      reads=[S32[key], B["dec"], pD], writes=[S32[key]])
                kb.op("act", lambda e: e.activation(out=Sbf[key].t[:], in_=S32[key].t[:], func=AF.Copy),
                      reads=[S32[key]], writes=[Sbf[key]])
            dst = ofs if dr == 0 else obs
            for (lo, hi, src) in segs:
                kb.dma("sp", dst[hd * 128:(hd + 1) * 128, src:src + (hi - lo)], B["ost"].t[:, lo:hi], reads=[B["ost"]])

        for hd in range(2):
            for i in range(NBE):
                gla_block(hd, 0, i)
                gla_block(hd, 1, NBE - 1 - i)
        kb.barrier()
        fo = [sbn("fo%d" % i, [128, TB], F32) for i in range(2)]
        fb = [sbn("fb%d" % i, [128, TB], F32) for i in range(2)]
        fr = [sbn("fr%d" % i, [128, TB], F32) for i in range(2)]
        fsq = sbn("fsq", [128, TB], BF16)
        frs = sbn("frs", [128, TB], F32)
        fi = 0
        for hd in range(2):
            for bi in range(NBE):
                cols = slice(bi * TB, (bi + 1) * TB)
                o_, b_, r_ = fo[fi % 2], fb[fi % 2], fr[fi % 2]
                fi += 1
                kb.dma("sp", o_.t[:], ofs[hd * 128:(hd + 1) * 128, cols], writes=[o_])
                kb.dma("sp", b_.t[:], obs[hd * 128:(hd + 1) * 128, cols], writes=[b_])
                kb.dma("sp", r_.t[:], pT[256 + hd * 128:256 + (hd + 1) * 128, cols], writes=[r_])
                kb.op("dve", lambda e: e.tensor_tensor(out=o_.t[:], in0=o_.t[:], in1=b_.t[:], op=ALU.add), reads=[o_, b_], writes=[o_])
                kb.op("act", lambda e: e.activation(out=fsq.t[:], in_=o_.t[:], func=AF.Square), reads=[o_], writes=[fsq])
                kb.op("pe", lambda e: e.matmul(P[0].t[:, 0:TB], ones128.t[:], fsq.t[:], start=True, stop=True),
                      reads=[ones128, fsq], writes=[P[0]])
                kb.op("act", lambda e: e.activation(out=frs.t[:], in_=P[0].t[:, 0:TB], func=AF.Sqrt, bias=eps.t[:, 0:1]),
                      reads=[P[0], eps], writes=[frs])
                kb.op("dve", lambda e: e.reciprocal(out=frs.t[:], in_=frs.t[:]), reads=[frs], writes=[frs])
                kb.op("dve", lambda e: e.scalar_tensor_tensor(out=o_.t[:], in0=o_.t[:], scalar=g_sb.t[:, 0:1], in1=frs.t[:],
                                                              op0=ALU.mult, op1=ALU.mult), reads=[o_, g_sb, frs], writes=[o_])
                kb.op("act", lambda e: e.activation(out=r_.t[:], in_=r_.t[:], func=AF.Silu), reads=[r_], writes=[r_])
                kb.op("dve", lambda e: e.tensor_tensor(out=o_.t[:], in0=o_.t[:], in1=r_.t[:], op=ALU.mult), reads=[o_, r_], writes=[o_])
                kb.dma("sp", cat_out[hd * 128:(hd + 1) * 128, cols], o_.t[:], reads=[o_], is_out=True)
        kb.barrier()


def emit_gqa(kb, nc, pT, vtm, qkg, rope_R, cosT, sinT, cat_out, P):
    QB = 512
    with ExitStack() as st:
        sbn = lambda n, sh, dt: kb.sb_in(st, "gq_" + n, sh, dt)
        QT = [sbn("QT%d" % j, [128, SE], BF16) for j in range(4)]
        KT = sbn("KT", [128, SE], BF16)
        for t_ in QT + [KT]:
            kb.op("pool", lambda e, t_=t_: e.memset(t_.t[64:128, :], 0.0), writes=[t_])
        V1 = sbn("V1", [128, SE // 128, 65], BF16)
        g_sb = sbn("g", [64, 2], F32)
        R_sb = sbn("R", [64, 64], F32)
        ones64 = sbn("ones64", [64, 64], F32)
        onesr = sbn("onesr", [128, 64], F32)
        eps = sbn("eps", [64, 1], F32)
        kb.dma("sp", g_sb.t[:], qkg, writes=[g_sb])
        kb.dma("sp", R_sb.t[:], rope_R, writes=[R_sb])
        kb.op("pool", lambda e: e.memset(ones64.t[:], 1.0 / 64.0), writes=[ones64])
        kb.op("pool", lambda e: e.memset(onesr.t[:], 1.0), writes=[onesr])
        kb.op("pool", lambda e: e.memset(eps.t[:], 1e-6), writes=[eps])
        kb.op("pool", lambda e: e.memset(V1.t[:], 1.0), writes=[V1])
        kb.dma("sp", V1.t[:, :, 0:64], vtm[:, 256:320].rearrange("(n p) d -> p n d", p=128), writes=[V1])
        x = [sbn("x%d" % i, [64, TB], F32) for i in range(2)]
        sq = sbn("sq", [64, TB], F32)
        rs = sbn("rs", [64, TB], F32)
        xn = sbn("xn", [64, TB], F32)
        t1 = sbn("t1", [64, TB], F32)
        t2 = sbn("t2", [64, TB], F32)
        cs_sb = [sbn("cos%d" % i, [64, TB], F32) for i in range(2)]
        sn_sb = [sbn("sin%d" % i, [64, TB], F32) for i in range(2)]
        xi = 0
        for bi in range(NBE):
            cols = slice(bi * TB, (bi + 1) * TB)
            llo = 256 if bi == 0 else 0
            lat0 = bi * TB + llo - 256
            nl = TB - llo
            c_, s_ = cs_sb[bi % 2], sn_sb[bi % 2]
            kb.dma("sp", c_.t[:, llo:TB], cosT[:, lat0:lat0 + nl], writes=[c_])
            kb.dma("sp", s_.t[:, llo:TB], sinT[:, lat0:lat0 + nl], writes=[s_])
            for j in range(5):
                x_ = x[xi % 2]
                xi += 1
                row0 = 512 + j * 64
                dst = QT[j] if j < 4 else KT
                gcol = 0 if j < 4 else 1
                oscale = 0.125 if j < 4 else 1.0
                kb.dma("sp", x_.t[:], pT[row0:row0 + 64, cols], writes=[x_])
                kb.op("act", lambda e: e.activation(out=sq.t[:], in_=x_.t[:], func=AF.Square), reads=[x_], writes=[sq])
                kb.op("pe", lambda e: e.matmul(P[0].t[0:64, 0:TB], ones64.t[:], sq.t[:], start=True, stop=True),
                      reads=[ones64, sq], writes=[P[0]])
                kb.op("act", lambda e: e.activation(out=rs.t[:], in_=P[0].t[0:64, 0:TB], func=AF.Sqrt, bias=eps.t[:, 0:1]),
                      reads=[P[0], eps], writes=[rs])
                kb.op("dve", lambda e: e.reciprocal(out=rs.t[:], in_=rs.t[:]), reads=[rs], writes=[rs])
                kb.op("dve", lambda e: e.scalar_tensor_tensor(out=xn.t[:], in0=x_.t[:], scalar=g_sb.t[:, gcol:gcol + 1], in1=rs.t[:],
                                                              op0=ALU.mult, op1=ALU.mult), reads=[x_, g_sb, rs], writes=[xn])
                if llo > 0:
                    kb.op("act", lambda e: e.activation(out=dst.t[0:64, bi * TB:bi * TB + llo], in_=xn.t[:, 0:llo], func=AF.Copy,
                                                        scale=oscale), reads=[xn], writes=[dst])
                kb.op("pe", lambda e: e.matmul(P[1].t[0:64, 0:nl], R_sb.t[:], xn.t[:, llo:TB], start=True, stop=True),
                      reads=[R_sb, xn], writes=[P[1]])
                kb.op("dve", lambda e: e.tensor_tensor(out=t1.t[:, llo:TB], in0=xn.t[:, llo:TB], in1=c_.t[:, llo:TB], op=ALU.mult),
                      reads=[xn, c_], writes=[t1])
                kb.op("dve", lambda e: e.tensor_tensor(out=t2.t[:, llo:TB], in0=P[1].t[0:64, 0:nl], in1=s_.t[:, llo:TB], op=ALU.mult),
                      reads=[P[1], s_], writes=[t2])
                kb.op("dve", lambda e: e.tensor_tensor(out=t1.t[:, llo:TB], in0=t1.t[:, llo:TB], in1=t2.t[:, llo:TB], op=ALU.add),
                      reads=[t1, t2], writes=[t1])
                kb.op("act", lambda e: e.activation(out=dst.t[0:64, bi * TB + llo:(bi + 1) * TB], in_=t1.t[:, llo:TB], func=AF.Copy,
                                                    scale=oscale), reads=[t1], writes=[dst])
        pt = [sbn("pt%d" % i, [128, QB], BF16) for i in range(4)]
        osb = [sbn("osb%d" % i, [64, QB], F32) for i in range(2)]
        rsum = sbn("rsum", [128, QB], F32)
        pi = 0
        qi = 0
        jobs = []
        for j in range(4):
            jobs.append((j, 0, 256, 2))
            for qb in range(8192 // QB):
                jobs.append((j, 256 + qb * QB, QB, SE // 128))
        for (j, q0, nq, nkt) in jobs:
            pO = P[4 + (qi % 2)]
            o_ = osb[qi % 2]
            qi += 1
            pts = {}
            LAG = 2
            for kt in range(nkt + LAG):
                if kt < nkt:
                    pS = (P[2], P[3], P[0])[kt % 3]
                    p_ = pt[pi % 4]
                    pi += 1
                    pts[kt] = p_
                    kb.op("pe", lambda e: e.matmul(pS.t[:, 0:nq], KT.t[:, kt * 128:(kt + 1) * 128], QT[j].t[:, q0:q0 + nq],
                                                   start=True, stop=True), reads=[KT, QT[j]], writes=[pS])
                    kb.op("act", lambda e: e.activation(out=p_.t[:, 0:nq], in_=pS.t[:, 0:nq], func=AF.Exp), reads=[pS], writes=[p_])
                if kt >= LAG:
                    kp = kt - LAG
                    pp = pts.pop(kp)
                    kb.op("pe", lambda e: e.matmul(pO.t[0:65, 0:nq], V1.t[:, kp, :], pp.t[:, 0:nq], start=(kp == 0), stop=(kp == nkt - 1)),
                          reads=[V1, pp], writes=[pO], sig=(kp == nkt - 1))
            kb.op("dve", lambda e: e.reciprocal(out=rsum.t[64:65, 0:nq], in_=pO.t[64:65, 0:nq]), reads=[pO], writes=[rsum])
            kb.op("act", lambda e: e.activation(out=o_.t[:, 0:nq], in_=pO.t[0:64, 0:nq], func=AF.Copy), reads=[pO], writes=[o_])
            kb.op("pe", lambda e: e.matmul(P[6].t[0:64, 0:nq], onesr.t[64:65, :], rsum.t[64:65, 0:nq], start=True, stop=True),
                  reads=[onesr, rsum], writes=[P[6]])
            kb.op("dve", lambda e: e.tensor_tensor(out=o_.t[:, 0:nq], in0=o_.t[:, 0:nq], in1=P[6].t[0:64, 0:nq], op=ALU.mult),
                  reads=[o_, P[6]], writes=[o_])
            kb.dma("sp", cat_out[256 + j * 64:256 + (j + 1) * 64, q0:q0 + nq], o_.t[:, 0:nq], reads=[o_], is_out=True)
        kb.barrier()


def build_stageB_even(parts=("inproj", "gla", "gqa")):
    nc = bass.Bass("TRN2", target_bir_lowering=False)
    xT = _din(nc, "xT", [1024, SE]).rearrange("(c p) t -> p c t", p=128)
    w_in = _din(nc, "w_in", [1024, FM_E + TM_E]).rearrange("(c p) n -> p c n", p=128)
    mods = _din(nc, "mods", [128, 2, 6, 8])
    wdec = _din(nc, "wdec", [16, 2, 128])
    bdec = _din(nc, "bdec", [64, 2, 2])
    glag = _din(nc, "glag", [128, 1])
    masks = _din(nc, "masks", [128, 2, 128])
    ident = _din(nc, "ident", [128, 128])
    qkg = _din(nc, "qkg", [64, 2])
    ropeR = _din(nc, "ropeR", [64, 64])
    cosT = _din(nc, "cosT", [64, 8192])
    sinT = _din(nc, "sinT", [64, 8192])
    cat = _dout(nc, "cat", [512, SE])
    pT = nc.dram_tensor("pT", [FM_E, SE], F32, kind="Internal").ap()
    vtm = nc.dram_tensor("vtm", [SE, TM_E], BF16, kind="Internal").ap()
    ofs = nc.dram_tensor("ofs", [256, SE], F32, kind="Internal").ap()
    obs = nc.dram_tensor("obs", [256, SE], F32, kind="Internal").ap()
    with ExitStack() as st:
        kb = KB(nc, st)
        P = [kb.ps("P%d" % i, [128, 512]) for i in range(7)]
        mods_sb = kb.sb("modsb", [128, 2, 6, 8], F32)
        kb.dma("sp", mods_sb.t[:], mods, writes=[mods_sb])
        if "inproj" in parts:
            emit_inproj(kb, nc, xT, w_in, mods_sb, 0, pT, vtm, FM_E, TM_E, P)
        if "gla" in parts:
            emit_gla(kb, nc, pT, vtm, wdec, bdec, glag, masks, ident, ofs, obs, cat, P)
        if "gqa" in parts:
            emit_gqa(kb, nc, pT, vtm, qkg, ropeR, cosT, sinT, cat, P)
        kb.finish()
        print("stageB_even instructions:", kb.n_inst, dict(kb.cnt))
    return nc


def const_masks():
    m = np.arange(128)[:, None]
    l = np.arange(128)[None, :]
    return np.ascontiguousarray(np.stack([(l >= m), (l <= m)], 1).astype(np.float32))


def const_ropeR():
    R = np.zeros((64, 64), np.float32)
    for dp in range(64):
        blk = dp // 16
        if blk % 2 == 0:
            R[dp + 16, dp] = -1.0
        else:
            R[dp - 16, dp] = 1.0
    return R


def const_rope_tables():
    rows = 8192 // 64
    r = np.repeat(np.arange(rows, dtype=np.float32), 64)
    col = np.tile(np.arange(64, dtype=np.float32), rows)
    half = 32
    inv = (np.float32(10000.0) ** (-np.arange(0, half, 2, dtype=np.float32) / np.float32(half))).astype(np.float32)
    ar = r[:, None] * inv
    ac = col[:, None] * inv
    ang = np.concatenate([ar, ar, ac, ac], -1)
    return np.ascontiguousarray(np.cos(ang).T.astype(np.float32)), np.ascontiguousarray(np.sin(ang).T.astype(np.float32))


def even_core_cols(g):
    o_q, o_k, o_v, o_r, o_lf, o_lb, o_gq, o_gk, o_gv = np.cumsum([0, 256, 256, 512, 512, 16, 16, 512, 128])
    fm = []
    for hd in (2 * g, 2 * g + 1):
        fm += list(range(o_q + hd * 64, o_q + (hd + 1) * 64))
    for hd in (2 * g, 2 * g + 1):
        fm += list(range(o_k + hd * 64, o_k + (hd + 1) * 64))
    for hd in (2 * g, 2 * g + 1):
        fm += list(range(o_r + hd * 128, o_r + (hd + 1) * 128))
    for j in range(4 * g, 4 * g + 4):
        fm += list(range(o_gq + j * 64, o_gq + (j + 1) * 64))
    fm += list(range(o_gk + g * 64, o_gk + (g + 1) * 64))
    fm += list(range(o_lf, o_lf + 16)) + list(range(o_lb, o_lb + 16))
    tm = []
    for hd in (2 * g, 2 * g + 1):
        tm += list(range(o_v + hd * 128, o_v + (hd + 1) * 128))
    tm += list(range(o_gv + g * 64, o_gv + (g + 1) * 64))
    assert len(fm) == FM_E and len(tm) == TM_E
    return np.array(fm + tm)


def even_core_inputs(g, xT_b, mods_b, w_in, w_dec, b_dec, gla_g, qg, kg, consts):
    hd = (2 * g, 2 * g + 1)
    wdec = np.stack([np.concatenate([w_dec[d][:, h * 64:(h + 1) * 64] for h in hd], 1) for d in range(2)], 1)
    bdec = np.stack([np.stack([b_dec[d][h * 64:(h + 1) * 64] for h in hd], 1) for d in range(2)], 1)
    return dict(xT=xT_b, w_in=np.ascontiguousarray(w_in[:, even_core_cols(g)]), mods=mods_b,
                wdec=np.ascontiguousarray(wdec, np.float32), bdec=np.ascontiguousarray(bdec, np.float32),
                glag=np.ascontiguousarray(gla_g.reshape(128, 1)), masks=consts["masks"], ident=consts["ident"],
                qkg=np.ascontiguousarray(np.stack([qg, kg], 1)), ropeR=consts["ropeR"], cosT=consts["cosT"], sinT=consts["sinT"])


FM_O = 1164
NEGBIG = -30000.0


def emit_conv(kb, nc, pT, cT, convw, convb, P):
    with ExitStack() as st:
        sbn = lambda n, sh, dt: kb.sb_in(st, "cv_" + n, sh, dt)
        w_sb = sbn("w", [128, 5, 5], F32)
        b_sb = sbn("b", [128, 5], F32)
        kb.dma("sp", w_sb.t[:], convw, writes=[w_sb])
        kb.dma("sp", b_sb.t[:], convb, writes=[b_sb])
        xin = [sbn("xin%d" % i, [128, TB + 4], F32) for i in range(3)]
        acc = [sbn("acc%d" % i, [128, TB], F32) for i in range(3)]
        k = 0
        segs = [(0, 256, 0, 256)]
        segs += [(256 + i * TB, min(256 + (i + 1) * TB, SE), 256, SE) for i in range((8192 + TB - 1) // TB)]
        for (a, b, s0, s1) in segs:
            n = b - a
            lo, hi = max(a - 2, s0), min(b + 2, s1)
            for c in range(5):
                xi, ac = xin[k % 3], acc[k % 3]
                k += 1
                kb.op("pool", lambda e: e.memset(xi.t[:], 0.0), writes=[xi])
                kb.dma("sp", xi.t[:, lo - (a - 2):hi - (a - 2)], pT[384 + c * 128:384 + (c + 1) * 128, lo:hi], writes=[xi])
                kb.op("dve", lambda e: e.tensor_scalar(out=ac.t[:, 0:n], in0=xi.t[:, 0:n], scalar1=w_sb.t[:, c, 0:1],
                                                       scalar2=b_sb.t[:, c:c + 1], op0=ALU.mult, op1=ALU.add),
                      reads=[xi, w_sb, b_sb], writes=[ac])
                for j in range(1, 5):
                    kb.op("dve", lambda e, j=j: e.scalar_tensor_tensor(out=ac.t[:, 0:n], in0=xi.t[:, j:j + n], scalar=w_sb.t[:, c, j:j + 1],
                                                                       in1=ac.t[:, 0:n], op0=ALU.mult, op1=ALU.add),
                          reads=[xi, w_sb, ac], writes=[ac])
                kb.op("act", lambda e: e.activation(out=ac.t[:, 0:n], in_=ac.t[:, 0:n], func=AF.Silu), reads=[ac], writes=[ac])
                kb.dma("sp", cT[c * 128:(c + 1) * 128, a:b], ac.t[:, 0:n], reads=[ac])
        kb.barrier()


def emit_ssd(kb, nc, pT, cT, dtb_in, alog_in, dsk_in, ng_in, masks_in, ident_in, yfs, ybs, cat_out, P):
    with ExitStack() as st:
        sbn = lambda n, sh, dt: kb.sb_in(st, "sd_" + n, sh, dt)
        dtbias = sbn("dtbias", [128, 12], F32)
        negA = sbn("negA", [128, 12], F32)
        mkb = sbn("mkb", [128, 2, 128], F32)
        onesr = sbn("onesr", [1, 128], F32)
        one1 = sbn("one1", [128, 1], F32)
        cmask = sbn("cmask", [128, TB], F32)
        idf = sbn("idf", [128, 128], BF16)
        kb.dma("sp", dtbias.t[:], dtb_in, writes=[dtbias])
        kb.dma("sp", negA.t[:], alog_in, writes=[negA])
        kb.dma("sp", mkb.t[:], masks_in, writes=[mkb])
        kb.dma("pool", idf.t[:], ident_in, writes=[idf])
        kb.op("act", lambda e: e.activation(out=negA.t[:], in_=negA.t[:], func=AF.Exp), reads=[negA], writes=[negA])
        kb.op("dve", lambda e: e.tensor_scalar(out=negA.t[:], in0=negA.t[:], scalar1=-1.0, scalar2=None, op0=ALU.mult),
              reads=[negA], writes=[negA])
        kb.op("dve", lambda e: e.tensor_scalar(out=mkb.t[:], in0=mkb.t[:], scalar1=-1.0, scalar2=-NEGBIG, op0=ALU.add, op1=ALU.mult),
              reads=[mkb], writes=[mkb])
        kb.op("pool", lambda e: e.memset(onesr.t[:], 1.0), writes=[onesr])
        kb.op("pool", lambda e: e.memset(one1.t[:], 1.0), writes=[one1])
        kb.op("pool", lambda e: e.memset(cmask.t[:], 1.0), writes=[cmask])
        for n in range(TB // 128):
            kb.op("pool", lambda e, n=n: e.memset(cmask.t[:, n * 128:n * 128 + 1], 0.0), writes=[cmask])
        H32, Hbf = {}, {}
        for h in range(6):
            for dr in range(2):
                key = (h, dr)
                H32[key] = sbn("H32_%d%d" % key, [128, 64], F32)
                Hbf[key] = sbn("Hbf_%d%d" % key, [128, 64], BF16)
                kb.op("pool", lambda e, key=key: e.memset(H32[key].t[:], 0.0), writes=[H32[key]])
                kb.op("pool", lambda e, key=key: e.memset(Hbf[key].t[:], 0.0), writes=[Hbf[key]])
        sh = {}
        for dr in range(2):
            d = {}
            for n, shp, dt in (("BT", [128, TB], BF16), ("CT", [128, TB], F32), ("xsT", [128, 3, TB], BF16), ("CB", [128, 3, 128], F32),
                               ("Btm", [128, 3, 128], BF16), ("xstm", [128, 3, 384], F32)):
                d[n] = sbn("%s_%d" % (n, dr), shp, dt)
            sh[dr] = d
        pd = {}
        for n, shp, dt in (("raw", [128, TB], F32), ("dt", [128, TB], F32), ("P", [128, TB], F32), ("X", [128, TB], F32),
                           ("nX", [128, TB], F32), ("EL", [128, TB], F32), ("w", [128, TB], F32), ("ED", [128, 3], F32),
                           ("dec", [128, 128], F32), ("G", [128, 128], BF16), ("cols", [128, 2], F32), ("xd", [128, 64], BF16),
                           ("xdw", [128, 64], BF16), ("Ct", [128, 128], BF16), ("yst", [64, TB], F32)):
            pd[n] = [sbn("%s_%d" % (n, i), shp, dt) for i in range(2)]
        PT = Buf(st.enter_context(nc.psum_tensor("ps_" + kb.pfx + "sd_pt", [128, 512], BF16)), "sd_pt")
        cnt = [0]

        def load_shared(dr, bi):
            Sd = sh[dr]
            segs = [(0, TB, bi * TB)] if dr == 0 else _bwd_segs(bi)
            for (lo, hi, src) in segs:
                n = hi - lo
                kb.dma("pool", Sd["BT"].t[:, lo:hi], cT[384:512, src:src + n], writes=[Sd["BT"]])
                kb.dma("sp", Sd["CT"].t[:, lo:hi], cT[512:640, src:src + n], writes=[Sd["CT"]])
                kb.dma("pool", Sd["xsT"].t[:, :, lo:hi], cT[0:384, src:src + n].rearrange("(c p) t -> p c t", p=128), writes=[Sd["xsT"]])
            ctb = pd["Ct"][0]
            for ci in range(3):
                cs = slice(ci * 128, (ci + 1) * 128)
                kb.op("act", lambda e: e.activation(out=ctb.t[:], in_=Sd["CT"].t[:, cs], func=AF.Copy), reads=[Sd["CT"]], writes=[ctb])
                kb.op("pe", lambda e: e.matmul(P[0].t[:, 0:128], Sd["BT"].t[:, cs], ctb.t[:], start=True, stop=True),
                      reads=[Sd["BT"], ctb], writes=[P[0]])
                kb.op("act", lambda e: e.activation(out=Sd["CB"].t[:, ci, :], in_=P[0].t[:, 0:128], func=AF.Copy),
                      reads=[P[0]], writes=[Sd["CB"]])
                kb.op("pe", lambda e: e.transpose(PT.t[:, 0:128], Sd["BT"].t[:, cs], idf.t[:]), reads=[Sd["BT"], idf], writes=[PT])
                kb.op("act", lambda e: e.activation(out=Sd["Btm"].t[:, ci, :], in_=PT.t[:, 0:128], func=AF.Copy),
                      reads=[PT], writes=[Sd["Btm"]])
                for c in range(3):
                    kb.op("pe", lambda e: e.transpose(PT.t[:, 128 + c * 128:256 + c * 128], Sd["xsT"].t[:, c, cs], idf.t[:]),
                          reads=[Sd["xsT"], idf], writes=[PT], sig=(c == 2))
                kb.op("dve", lambda e: e.tensor_copy(out=Sd["xstm"].t[:, ci, :], in_=PT.t[:, 128:512]), reads=[PT], writes=[Sd["xstm"]])

        def ssd_block(h, dr, bi):
            Sd = sh[dr]
            i = cnt[0] % 2
            cnt[0] += 1
            B = {k_: v_[i] for k_, v_ in pd.items()}
            key = (h, dr)
            hd = dr * 6 + h
            segs = [(0, TB, bi * TB)] if dr == 0 else _bwd_segs(bi)
            for (lo, hi, src) in segs:
                n = hi - lo
                row = 1152 + dr * 6 + h
                kb.dma("sp", B["raw"].t[:, lo:hi].unsqueeze(1), pT[row:row + 1, src:src + n].partition_broadcast(128), writes=[B["raw"]])
            kb.op("act", lambda e: e.activation(out=B["dt"].t[:], in_=B["raw"].t[:], func=AF.Exp, bias=dtbias.t[:, hd:hd + 1]),
                  reads=[B["raw"], dtbias], writes=[B["dt"]])
            kb.op("act", lambda e: e.activation(out=B["dt"].t[:], in_=B["dt"].t[:], func=AF.Ln, bias=one1.t[:, 0:1]),
                  reads=[B["dt"], one1], writes=[B["dt"]])
            kb.op("dve", lambda e: e.tensor_scalar(out=B["raw"].t[:], in0=B["dt"].t[:], scalar1=negA.t[:, hd:hd + 1], scalar2=None,
                                                   op0=ALU.mult), reads=[B["dt"], negA], writes=[B["raw"]])
            kb.op("dve", lambda e: e.tensor_tensor_scan(out=B["P"].t[:], data0=cmask.t[:], data1=B["raw"].t[:], initial=0.0,
                                                        op0=ALU.mult, op1=ALU.add), reads=[cmask, B["raw"]], writes=[B["P"]])
            P3 = B["P"].t[:].rearrange("p (n l) -> p n l", l=128)
            tot_bc = P3[:, :, 127:128].broadcast_to([128, 3, 128])
            r3 = lambda b_: b_.t[:].rearrange("p (n l) -> p n l", l=128)
            kb.op("act", lambda e: e.activation(out=B["ED"].t[:].unsqueeze(2), in_=P3[:, :, 127:128], func=AF.Exp),
                  reads=[B["P"]], writes=[B["ED"]])
            if dr == 0:
                kb.op("dve", lambda e: e.tensor_copy(out=B["X"].t[:], in_=B["P"].t[:]), reads=[B["P"]], writes=[B["X"]])
                kb.op("act", lambda e: e.activation(out=B["EL"].t[:], in_=B["P"].t[:], func=AF.Exp), reads=[B["P"]], writes=[B["EL"]])
                kb.op("dve", lambda e: e.tensor_tensor(out=r3(B["w"]), in0=tot_bc, in1=P3, op=ALU.subtract), reads=[B["P"]], writes=[B["w"]])
            else:
                kb.op("dve", lambda e: e.tensor_tensor(out=B["X"].t[:], in0=B["P"].t[:], in1=B["raw"].t[:], op=ALU.subtract),
                      reads=[B["P"], B["raw"]], writes=[B["X"]])
                kb.op("dve", lambda e: e.tensor_tensor(out=r3(B["EL"]), in0=tot_bc, in1=r3(B["X"]), op=ALU.subtract),
                      reads=[B["P"], B["X"]], writes=[B["EL"]])
                kb.op("act", lambda e: e.activation(out=B["EL"].t[:], in_=B["EL"].t[:], func=AF.Exp), reads=[B["EL"]], writes=[B["EL"]])
                kb.op("dve", lambda e: e.tensor_copy(out=B["w"].t[:], in_=B["X"].t[:]), reads=[B["X"]], writes=[B["w"]])
            kb.op("act", lambda e: e.activation(out=B["w"].t[:], in_=B["w"].t[:], func=AF.Exp), reads=[B["w"]], writes=[B["w"]])
            kb.op("dve", lambda e: e.tensor_tensor(out=B["w"].t[:], in0=B["w"].t[:], in1=B["dt"].t[:], op=ALU.mult),
                  reads=[B["w"], B["dt"]], writes=[B["w"]])
            kb.op("dve", lambda e: e.tensor_scalar(out=B["nX"].t[:], in0=B["X"].t[:], scalar1=-1.0, scalar2=None, op0=ALU.mult),
                  reads=[B["X"]], writes=[B["nX"]])
            order = range(3) if dr == 0 else range(2, -1, -1)
            for ci in order:
                cs = slice(ci * 128, (ci + 1) * 128)
                pS, pC, pY, pH = P[1], P[2], P[3 + dr], P[5 + dr]
                if dr == 0:
                    kb.op("pe", lambda e: e.matmul(pS.t[:, 0:128], onesr.t[0:1, :], B["X"].t[0:1, cs], start=True, stop=False),
                          reads=[onesr, B["X"]], writes=[pS], sig=False)
                    kb.op("pe", lambda e: e.matmul(pS.t[:, 0:128], B["nX"].t[0:1, cs], onesr.t[0:1, :], start=False, stop=True),
                          reads=[onesr, B["nX"]], writes=[pS])
                else:
                    kb.op("pe", lambda e: e.matmul(pS.t[:, 0:128], onesr.t[0:1, :], B["nX"].t[0:1, cs], start=True, stop=False),
                          reads=[onesr, B["nX"]], writes=[pS], sig=False)
                    kb.op("pe", lambda e: e.matmul(pS.t[:, 0:128], B["X"].t[0:1, cs], onesr.t[0:1, :], start=False, stop=True),
                          reads=[onesr, B["X"]], writes=[pS])
                kb.op("dve", lambda e: e.tensor_tensor(out=B["dec"].t[:], in0=pS.t[:, 0:128], in1=mkb.t[:, dr, :], op=ALU.add),
                      reads=[pS, mkb], writes=[B["dec"]])
                kb.op("act", lambda e: e.activation(out=B["dec"].t[:], in_=B["dec"].t[:], func=AF.Exp), reads=[B["dec"]], writes=[B["dec"]])
                kb.op("dve", lambda e: e.tensor_tensor(out=B["G"].t[:], in0=B["dec"].t[:], in1=Sd["CB"].t[:, ci, :], op=ALU.mult),
                      reads=[B["dec"], Sd["CB"]], writes=[B["G"]])
                kb.op("pe", lambda e: e.matmul(pC.t[:, 0:1], B["dt"].t[0:1, cs], onesr.t[0:1, 0:1], start=True, stop=True),
                      reads=[B["dt"], onesr], writes=[pC], sig=False)
                kb.op("pe", lambda e: e.matmul(pC.t[:, 1:2], B["w"].t[0:1, cs], onesr.t[0:1, 0:1], start=True, stop=True),
                      reads=[B["w"], onesr], writes=[pC])
                kb.op("act", lambda e: e.activation(out=B["cols"].t[:], in_=pC.t[:, 0:2], func=AF.Copy), reads=[pC], writes=[B["cols"]])
                xs_h = Sd["xstm"].t[:, ci, h * 64:(h + 1) * 64]
                kb.op("dve", lambda e: e.tensor_scalar(out=B["xd"].t[:], in0=xs_h, scalar1=B["cols"].t[:, 0:1], scalar2=None, op0=ALU.mult),
                      reads=[Sd["xstm"], B["cols"]], writes=[B["xd"]])
                kb.op("dve", lambda e: e.tensor_scalar(out=B["xdw"].t[:], in0=xs_h, scalar1=B["cols"].t[:, 1:2], scalar2=None, op0=ALU.mult),
                      reads=[Sd["xstm"], B["cols"]], writes=[B["xdw"]])
                kb.op("dve", lambda e: e.tensor_tensor(out=B["Ct"].t[:], in0=Sd["CT"].t[:, cs], in1=B["EL"].t[:, cs], op=ALU.mult),
                      reads=[Sd["CT"], B["EL"]], writes=[B["Ct"]])
                kb.op("pe", lambda e: e.matmul(pY.t[0:64, 0:128], B["xd"].t[:], B["G"].t[:], start=True, stop=False),
                      reads=[B["xd"], B["G"]], writes=[pY], sig=False)
                kb.op("pe", lambda e: e.matmul(pY.t[0:64, 0:128], Hbf[key].t[:], B["Ct"].t[:], start=False, stop=True),
                      reads=[Hbf[key], B["Ct"]], writes=[pY])
                kb.op("act", lambda e: e.activation(out=B["yst"].t[:, cs], in_=pY.t[0:64, 0:128], func=AF.Copy), reads=[pY], writes=[B["yst"]])
                kb.op("pe", lambda e: e.matmul(pH.t[:, 0:64], Sd["Btm"].t[:, ci, :], B["xdw"].t[:], start=True, stop=True),
                      reads=[Sd["Btm"], B["xdw"]], writes=[pH])
                kb.op("dve", lambda e: e.scalar_tensor_tensor(out=H32[key].t[:], in0=H32[key].t[:], scalar=B["ED"].t[:, ci:ci + 1],
                                                              in1=pH.t[:, 0:64], op0=ALU.mult, op1=ALU.add),
                      reads=[H32[key], B["ED"], pH], writes=[H32[key]])
                kb.op("act", lambda e: e.activation(out=Hbf[key].t[:], in_=H32[key].t[:], func=AF.Copy), reads=[H32[key]], writes=[Hbf[key]])
            dst = yfs if dr == 0 else ybs
            for (lo, hi, src) in segs:
                kb.dma("sp", dst[h * 64:(h + 1) * 64, src:src + (hi - lo)], B["yst"].t[:, lo:hi], reads=[B["yst"]])

        for i in range(NBE):
            load_shared(0, i)
            load_shared(1, NBE - 1 - i)
            for h in range(6):
                ssd_block(h, 0, i)
                ssd_block(h, 1, NBE - 1 - i)
        kb.barrier()
        dsk = sbn("dsk", [128, 3], F32)
        ng = sbn("ng", [128, 3], F32)
        ones384 = sbn("ones384", [128, 128], BF16)
        epsf = sbn("epsf", [128, 1], F32)
        kb.dma("sp", dsk.t[:], dsk_in, writes=[dsk])
        kb.dma("sp", ng.t[:], ng_in, writes=[ng])
        kb.op("pool", lambda e: e.memset(ones384.t[:], 1.0 / 384.0), writes=[ones384])
        kb.op("pool", lambda e: e.memset(epsf.t[:], 1e-6), writes=[epsf])
        fy = sbn("fy", [128, 3, TB], F32)
        fb = sbn("fb", [128, 3, TB], F32)
        fx = sbn("fx", [128, 3, TB], F32)
        fz = sbn("fz", [128, 3, TB], F32)
        fsq = sbn("fsq", [128, 3, TB], BF16)
        frs = sbn("frs", [128, TB], F32)
        v3 = lambda ap: ap.rearrange("(c p) t -> p c t", p=128)
        for bi in range(NBE):
            cols = slice(bi * TB, (bi + 1) * TB)
            kb.dma("sp", fy.t[:], v3(yfs[:, cols]), writes=[fy])
            kb.dma("sp", fb.t[:], v3(ybs[:, cols]), writes=[fb])
            kb.dma("sp", fx.t[:], v3(cT[0:384, cols]), writes=[fx])
            kb.dma("sp", fz.t[:], v3(pT[0:384, cols]), writes=[fz])
            for c in range(3):
                kb.op("dve", lambda e: e.tensor_tensor(out=fy.t[:, c, :], in0=fy.t[:, c, :], in1=fb.t[:, c, :], op=ALU.add),
                      reads=[fy, fb], writes=[fy])
                kb.op("dve", lambda e: e.scalar_tensor_tensor(out=fy.t[:, c, :], in0=fx.t[:, c, :], scalar=dsk.t[:, c:c + 1], in1=fy.t[:, c, :],
                                                              op0=ALU.mult, op1=ALU.add), reads=[fx, dsk, fy], writes=[fy])
                kb.op("act", lambda e: e.activation(out=fz.t[:, c, :], in_=fz.t[:, c, :], func=AF.Silu), reads=[fz], writes=[fz])
                kb.op("dve", lambda e: e.tensor_tensor(out=fy.t[:, c, :], in0=fy.t[:, c, :], in1=fz.t[:, c, :], op=ALU.mult),
                      reads=[fy, fz], writes=[fy])
                kb.op("act", lambda e: e.activation(out=fsq.t[:, c, :], in_=fy.t[:, c, :], func=AF.Square), reads=[fy], writes=[fsq])
            for c in range(3):
                kb.op("pe", lambda e: e.matmul(P[0].t[:, 0:TB], ones384.t[:], fsq.t[:, c, :], start=(c == 0), stop=(c == 2)),
                      reads=[ones384, fsq], writes=[P[0]], sig=(c == 2))
            kb.op("act", lambda e: e.activation(out=frs.t[:], in_=P[0].t[:, 0:TB], func=AF.Sqrt, bias=epsf.t[:, 0:1]),
                  reads=[P[0], epsf], writes=[frs])
            kb.op("dve", lambda e: e.reciprocal(out=frs.t[:], in_=frs.t[:]), reads=[frs], writes=[frs])
            for c in range(3):
                kb.op("dve", lambda e: e.scalar_tensor_tensor(out=fy.t[:, c, :], in0=fy.t[:, c, :], scalar=ng.t[:, c:c + 1], in1=frs.t[:],
                                                              op0=ALU.mult, op1=ALU.mult), reads=[fy, ng, frs], writes=[fy])
            kb.dma("sp", v3(cat_out[0:384, cols]), fy.t[:], reads=[fy], is_out=True)
        kb.barrier()


def emit_fnet(kb, nc, pT, fg_in, fconst, cat_out, P):
    with ExitStack() as st:
        sbn = lambda n, sh, dt: kb.sb_in(st, "fn_" + n, sh, dt)
        unT = sbn("unT", [128, SE], BF16)
        g_sb = sbn("g", [128, 1], F32)
        bd = sbn("bd", [128, 128], BF16)
        ccsc = sbn("ccsc", [128, 256], BF16)
        f64 = sbn("f64", [64, 2, 128], BF16)
        tw = sbn("tw", [128, 2, 64], F32)
        f128 = sbn("f128", [128, 2, 128], BF16)
        c256 = sbn("c256", [128, 2, 2, 256], BF16)
        eps = sbn("eps", [128, 1], F32)
        kb.dma("sp", g_sb.t[:], fg_in, writes=[g_sb])
        kb.dma("pool", bd.t[:], fconst["bd"], writes=[bd])
        kb.dma("pool", ccsc.t[:], fconst["ccsc"], writes=[ccsc])
        kb.dma("pool", f64.t[:], fconst["f64"], writes=[f64])
        kb.dma("sp", tw.t[:], fconst["tw"], writes=[tw])
        kb.dma("pool", f128.t[:], fconst["f128"], writes=[f128])
        kb.dma("pool", c256.t[:], fconst["c256"], writes=[c256])
        kb.op("pool", lambda e: e.memset(eps.t[:], 1e-6), writes=[eps])
        fin = [sbn("fin%d" % i, [128, TB], F32) for i in range(2)]
        fsq = sbn("fsq", [128, TB], BF16)
        frs = sbn("frs", [128, TB], F32)
        for bi in range(NBE):
            cols = slice(bi * TB, (bi + 1) * TB)
            f_ = fin[bi % 2]
            kb.dma("sp", f_.t[:], pT[1024:1152, cols], writes=[f_])
            kb.op("act", lambda e: e.activation(out=fsq.t[:], in_=f_.t[:], func=AF.Square), reads=[f_], writes=[fsq])
            kb.op("pe", lambda e: e.matmul(P[0].t[:, 0:TB], bd.t[:], fsq.t[:], start=True, stop=True), reads=[bd, fsq], writes=[P[0]])
            kb.op("act", lambda e: e.activation(out=frs.t[:], in_=P[0].t[:, 0:TB], func=AF.Sqrt, bias=eps.t[:, 0:1]),
                  reads=[P[0], eps], writes=[frs])
            kb.op("dve", lambda e: e.reciprocal(out=frs.t[:], in_=frs.t[:]), reads=[frs], writes=[frs])
            kb.op("dve", lambda e: e.scalar_tensor_tensor(out=unT.t[:, cols], in0=f_.t[:], scalar=g_sb.t[:, 0:1], in1=frs.t[:],
                                                          op0=ALU.mult, op1=ALU.mult), reads=[f_, g_sb, frs], writes=[unT])
        Actx = sbn("Actx", [128, 2, 2, 128], BF16)
        octx = sbn("octx", [128, 256], F32)
        for tile in range(2):
            kb.op("pe", lambda e: e.matmul(P[1].t[:, 0:256], unT.t[:, tile * 128:(tile + 1) * 128], ccsc.t[:, :],
                                           start=True, stop=True), reads=[unT, ccsc], writes=[P[1]])
            kb.op("act", lambda e: e.activation(
                out=Actx.t[:, tile, :, :].rearrange("p a (g c) -> p g a c", g=2),
                in_=P[1].t[:, 0:256].rearrange("p (g a c) -> p g a c", g=2, a=2), func=AF.Copy), reads=[P[1]], writes=[Actx])
        k = 0
        for tile in range(2):
            for ab in range(2):
                kb.op("pe", lambda e: e.matmul(P[2].t[:, 0:256], Actx.t[:, tile, ab, :], c256.t[:, tile, ab, :], start=(k == 0), stop=(k == 3)),
                      reads=[Actx, c256], writes=[P[2]], sig=(k == 3))
                k += 1
        kb.op("act", lambda e: e.activation(out=octx.t[:], in_=P[2].t[:, 0:256], func=AF.Copy), reads=[P[2]], writes=[octx])
        kb.dma("sp", cat_out[384:512, 0:256], octx.t[:], reads=[octx], is_out=True)
        Y = sbn("Y", [64, 2, 2, 64, 128], BF16)
        Zp = sbn("Zp", [128, 2, 64, 128], BF16)
        zs = [sbn("zs%d" % i, [128, 4, 2, 64], F32) for i in range(2)]
        ta = [sbn("ta%d" % i, [128, 4, 64], F32) for i in range(4)]
        ost = sbn("ost", [128, 8192], F32)
        unL = unT.t[:, 256:SE].rearrange("p (t1 t2) -> p t2 t1", t2=128)
        for t2 in range(128):
            pz = P[3 + (t2 % 2)]
            kb.op("pe", lambda e: e.matmul(pz.t[0:64, 0:256], unL[:, t2, :], ccsc.t[:, :], start=True, stop=True),
                  reads=[unT, ccsc], writes=[pz])
            src = pz.t[0:64, 0:256].rearrange("p (g a c) -> p g a c", g=2, a=2)
            dst = Y.t[:, :, :, :, t2].rearrange("p a g c -> p g a c")
            if t2 % 2 == 0:
                kb.op("act", lambda e: e.activation(out=dst, in_=src, func=AF.Copy), reads=[pz], writes=[Y])
            else:
                kb.op("dve", lambda e: e.tensor_copy(out=dst, in_=src), reads=[pz], writes=[Y])
        tcb = tw.t[:, 0, :].unsqueeze(1).broadcast_to([128, 4, 64])
        tsb = tw.t[:, 1, :].unsqueeze(1).broadcast_to([128, 4, 64])
        for b4 in range(32):
            pz = P[3 + (b4 % 2)]
            z_ = zs[b4 % 2]
            for q in range(4):
                gc = b4 * 4 + q
                g, c = gc // 64, gc % 64
                kb.op("pe", lambda e: e.matmul(pz.t[:, q * 128:(q + 1) * 128], Y.t[:, 0, g, c, :], f64.t[:, 0, :], start=True, stop=False),
                      reads=[Y, f64], writes=[pz], sig=False)
                kb.op("pe", lambda e: e.matmul(pz.t[:, q * 128:(q + 1) * 128], Y.t[:, 1, g, c, :], f64.t[:, 1, :], start=False, stop=True),
                      reads=[Y, f64], writes=[pz], sig=(q == 3))
            kb.op("act", lambda e: e.activation(out=z_.t[:], in_=pz.t[:, 0:512].rearrange("p (q r t) -> p q r t", q=4, r=2), func=AF.Copy),
                  reads=[pz], writes=[z_])
            zr, zi = z_.t[:, :, 0, :], z_.t[:, :, 1, :]
            gsl = slice(b4 * 4, b4 * 4 + 4)
            kb.op("dve", lambda e: e.tensor_tensor(out=ta[0].t[:], in0=zr, in1=tcb, op=ALU.mult), reads=[z_, tw], writes=[ta[0]])
            kb.op("dve", lambda e: e.tensor_tensor(out=ta[1].t[:], in0=zi, in1=tsb, op=ALU.mult), reads=[z_, tw], writes=[ta[1]])
            kb.op("dve", lambda e: e.tensor_tensor(out=Zp.t[:, 0, :, gsl].rearrange("p t g -> p g t"), in0=ta[0].t[:], in1=ta[1].t[:], op=ALU.add),
                  reads=[ta[0], ta[1]], writes=[Zp])
            kb.op("pool", lambda e: e.tensor_tensor(out=ta[2].t[:], in0=zi, in1=tcb, op=ALU.mult), reads=[z_, tw], writes=[ta[2]])
            kb.op("pool", lambda e: e.tensor_tensor(out=ta[3].t[:], in0=zr, in1=tsb, op=ALU.mult), reads=[z_, tw], writes=[ta[3]])
            kb.op("pool", lambda e: e.tensor_tensor(out=Zp.t[:, 1, :, gsl].rearrange("p t g -> p g t"), in0=ta[2].t[:], in1=ta[3].t[:],
                                                    op=ALU.subtract), reads=[ta[2], ta[3]], writes=[Zp])
        ostv = ost.t[:].rearrange("p (t2 t1) -> p t1 t2", t1=64)
        for b4 in range(16):
            pz = P[3 + (b4 % 2)]
            for q in range(4):
                t1p = b4 * 4 + q
                kb.op("pe", lambda e: e.matmul(pz.t[:, q * 128:(q + 1) * 128], Zp.t[:, 0, t1p, :], f128.t[:, 0, :], start=True, stop=False),
                      reads=[Zp, f128], writes=[pz], sig=False)
                kb.op("pe", lambda e: e.matmul(pz.t[:, q * 128:(q + 1) * 128], Zp.t[:, 1, t1p, :], f128.t[:, 1, :], start=False, stop=True),
                      reads=[Zp, f128], writes=[pz], sig=(q == 3))
            kb.op("act", lambda e: e.activation(out=ostv[:, b4 * 4:b4 * 4 + 4, :], in_=pz.t[:, 0:512].rearrange("p (q t) -> p q t", q=4),
                                                func=AF.Copy), reads=[pz], writes=[ost])
        for i in range(4):
            kb.dma("sp", cat_out[384:512, 256 + i * 2048:256 + (i + 1) * 2048], ost.t[:, i * 2048:(i + 1) * 2048], reads=[ost], is_out=True)
        kb.barrier()


def build_stageB_odd(parts=("inproj", "conv", "ssd", "fnet")):
    nc = bass.Bass("TRN2", target_bir_lowering=False)
    xT = _din(nc, "xT", [1024, SE]).rearrange("(c p) t -> p c t", p=128)
    w_in = _din(nc, "w_in", [1024, FM_O]).rearrange("(c p) n -> p c n", p=128)
    mods = _din(nc, "mods", [128, 2, 6, 8])
    convw = _din(nc, "convw", [128, 5, 5])
    convb = _din(nc, "convb", [128, 5])
    dtb = _din(nc, "dtb", [128, 12])
    alog = _din(nc, "alog", [128, 12])
    dsk = _din(nc, "dsk", [128, 3])
    ng = _din(nc, "ng", [128, 3])
    masks = _din(nc, "masks", [128, 2, 128])
    ident = _din(nc, "ident", [128, 128])
    fg = _din(nc, "fg", [128, 1])
    fconst = dict(bd=_din(nc, "fc_bd", [128, 128]), ccsc=_din(nc, "fc_ccsc", [128, 256]), f64=_din(nc, "fc_f64", [64, 2, 128]),
                  tw=_din(nc, "fc_tw", [128, 2, 64]), f128=_din(nc, "fc_f128", [128, 2, 128]), c256=_din(nc, "fc_c256", [128, 2, 2, 256]))
    cat = _dout(nc, "cat", [512, SE])
    pT = nc.dram_tensor("pT", [FM_O, SE], F32, kind="Internal").ap()
    cT = nc.dram_tensor("cT", [640, SE], F32, kind="Internal").ap()
    yfs = nc.dram_tensor("yfs", [384, SE], F32, kind="Internal").ap()
    ybs = nc.dram_tensor("ybs", [384, SE], F32, kind="Internal").ap()
    with ExitStack() as st:
        kb = KB(nc, st)
        P = [kb.ps("P%d" % i, [128, 512]) for i in range(7)]
        mods_sb = kb.sb("modsb", [128, 2, 6, 8], F32)
        kb.dma("sp", mods_sb.t[:], mods, writes=[mods_sb])
        if "inproj" in parts:
            emit_inproj(kb, nc, xT, w_in, mods_sb, 0, pT, None, FM_O, 0, P)
        if "conv" in parts:
            emit_conv(kb, nc, pT, cT, convw, convb, P)
        if "ssd" in parts:
            emit_ssd(kb, nc, pT, cT, dtb, alog, dsk, ng, masks, ident, yfs, ybs, cat, P)
        if "fnet" in parts:
            emit_fnet(kb, nc, pT, fg, fconst, cat, P)
        kb.finish()
        print("stageB_odd instructions:", kb.n_inst, dict(kb.cnt))
    return nc


def const_fnet():
    c = np.arange(64)
    ang = 2 * np.pi * np.outer(c, c) / 64.0
    C64, S64 = np.cos(ang), np.sin(ang)
    ccsc = np.concatenate([C64, S64], 1) / 8.0
    z_ = np.zeros_like(ccsc)
    ccsc = np.concatenate([np.concatenate([ccsc, z_], 1), np.concatenate([z_, ccsc], 1)], 0)
    f64 = np.stack([np.concatenate([C64, -S64], 1), np.concatenate([-S64, -C64], 1)], 1) / 8.0
    t2 = np.arange(128)
    phi = 2 * np.pi * np.outer(t2, c) / 8192.0
    tw = np.stack([np.cos(phi), np.sin(phi)], 1)
    psi = 2 * np.pi * np.outer(t2, t2) / 128.0
    f128 = np.stack([np.cos(psi), np.sin(psi)], 1) / np.sqrt(128.0)
    t = np.arange(256)
    a256 = 2 * np.pi * np.outer(t, t) / 256.0
    c256 = np.stack([np.cos(a256), -np.sin(a256)], 1) / 16.0
    c256 = c256.reshape(2, 128, 2, 256).transpose(1, 0, 2, 3)
    bd = np.zeros((128, 128))
    bd[:64, :64] = 1.0 / 64.0
    bd[64:, 64:] = 1.0 / 64.0
    f32 = lambda a: np.ascontiguousarray(a, np.float32)
    return dict(fc_bd=f32(bd), fc_ccsc=f32(ccsc), fc_f64=f32(f64), fc_tw=f32(tw), fc_f128=f32(f128), fc_c256=f32(c256))


def odd_core_cols(g):
    o_z, o_x, o_b, o_c, o_dtf, o_dtb, o_f = np.cumsum([0, 768, 768, 256, 256, 12, 12])
    cols = list(range(o_z + g * 384, o_z + (g + 1) * 384)) + list(range(o_x + g * 384, o_x + (g + 1) * 384))
    cols += list(range(o_b + g * 128, o_b + (g + 1) * 128)) + list(range(o_c + g * 128, o_c + (g + 1) * 128))
    cols += list(range(o_f + g * 128, o_f + (g + 1) * 128))
    cols += list(range(o_dtf + g * 6, o_dtf + (g + 1) * 6)) + list(range(o_dtb + g * 6, o_dtb + (g + 1) * 6))
    assert len(cols) == FM_O
    return np.array(cols)


def odd_core_inputs(g, xT_b, mods_b, w_in, conv_w, conv_b, dt_bias, a_log, d_skip, ssd_g, fnet_g, consts):
    ch = np.concatenate([768 * 0 + np.arange(g * 384, (g + 1) * 384), 768 + np.arange(g * 128, (g + 1) * 128),
                         1024 + np.arange(g * 128, (g + 1) * 128)])
    cw = conv_w[:, ch].reshape(5, 5, 128).transpose(2, 1, 0)
    cb = conv_b[ch].reshape(5, 128).T
    rep = lambda v: np.ascontiguousarray(np.broadcast_to(np.asarray(v, np.float32)[None, :], (128, len(v))))
    dtb = rep(np.concatenate([dt_bias[0][g * 6:(g + 1) * 6], dt_bias[1][g * 6:(g + 1) * 6]]))
    alog = rep(np.concatenate([a_log[0][g * 6:(g + 1) * 6], a_log[1][g * 6:(g + 1) * 6]]))
    dsk = np.repeat(d_skip[g * 6:(g + 1) * 6], 64).reshape(3, 128).T
    ng = ssd_g[g * 384:(g + 1) * 384].reshape(3, 128).T
    fg = fnet_g[g * 128:(g + 1) * 128].reshape(128, 1)
    f32 = lambda a: np.ascontiguousarray(a, np.float32)
    im = dict(xT=xT_b, w_in=f32(w_in[:, odd_core_cols(g)]), mods=mods_b, convw=f32(cw), convb=f32(cb), dtb=dtb, alog=alog,
              dsk=f32(dsk), ng=f32(ng), masks=consts["masks"], ident=consts["ident"], fg=f32(fg))
    im.update(consts["fnet"])
    return im


def build_adaln():
    nc = bass.Bass("TRN2", target_bir_lowering=False)
    cin = _din(nc, "cin", [128, 8, 5])
    w = _din(nc, "w", [1024, 3072]).rearrange("(c p) n -> p c n", p=128)
    b = _din(nc, "b", [128, 24])
    out = _dout(nc, "out", [128, 24, 5])
    with ExitStack() as st:
        kb = KB(nc, st)
        P = [kb.ps("P%d" % i, [128, 512]) for i in range(4)]
        c_sb = kb.sb("c", [128, 8, 5], F32)
        b_sb = kb.sb("b", [128, 24], F32)
        o_sb = kb.sb("o", [128, 24, 5], F32)
        w_sb = [kb.sb("w%d" % i, [128, 8, 768], F32) for i in range(4)]
        kb.dma("sp", c_sb.t[:], cin, writes=[c_sb])
        kb.dma("sp", b_sb.t[:], b, writes=[b_sb])
        for i in range(4):
            kb.dma("sp", w_sb[i].t[:], w[:, :, i * 768:(i + 1) * 768], writes=[w_sb[i]])
        kb.op("act", lambda e: e.activation(out=c_sb.t[:], in_=c_sb.t[:], func=AF.Silu), reads=[c_sb], writes=[c_sb])
        for j in range(24):
            pj = P[j % 4]
            wi, wo = j // 6, (j % 6) * 128
            for k in range(8):
                kb.op("pe", lambda e, k=k: e.matmul(pj.t[:, 0:5], w_sb[wi].t[:, k, wo:wo + 128], c_sb.t[:, k, :], start=(k == 0), stop=(k == 7)),
                      reads=[w_sb[wi], c_sb], writes=[pj], sig=(k == 7))
            kb.op("dve", lambda e: e.tensor_scalar(out=o_sb.t[:, j, :], in0=pj.t[:, 0:5], scalar1=b_sb.t[:, j:j + 1], scalar2=None, op0=ALU.add),
                  reads=[pj, b_sb], writes=[o_sb])
        kb.dma("sp", out, o_sb.t[:], reads=[o_sb], is_out=True)
        kb.finish()
    return nc


_PROGS = {}


def _prog(name, fn):
    if name not in _PROGS:
        _PROGS[name] = fn()
    return _PROGS[name]


def kernel_unfused(x, c, ctx, c_ctx, ada_w, ada_b, ln_g, ln_b, ev_w_in, ev_w_o, gla_w_decay, gla_b_decay, gla_norm_g, gqa_q_norm_g,
           gqa_k_norm_g, od_w_in, od_w_o, ssd_conv_w, ssd_conv_b, ssd_dt_bias, ssd_a_log, ssd_d, ssd_norm_g, fnet_norm_g,
           router_w, router_b, exp_w_gate, exp_w_up, exp_w_down):
    f32 = lambda a: np.ascontiguousarray(np.asarray(a), np.float32)
    x, c, ctx, c_ctx = f32(x), f32(c), f32(ctx), f32(c_ctx)
    ada_w, ada_b = f32(ada_w), f32(ada_b)
    NB, DEPTH = 4, 4
    cores = list(range(8))
    call = np.concatenate([c, c_ctx[None, :]], 0)
    cin = np.ascontiguousarray(call.reshape(5, 8, 128).transpose(2, 1, 0))
    ims = []
    for core in cores:
        layer, half = core // 2, core % 2
        ims.append(dict(cin=cin, w=np.ascontiguousarray(ada_w[layer][:, half * 3072:(half + 1) * 3072]),
                        b=np.ascontiguousarray(ada_b[layer][half * 3072:(half + 1) * 3072].reshape(24, 128).T)))
    res = run_bass_kernel_spmd(_prog("adaln", build_adaln), ims, core_ids=cores)
    mods_all = np.zeros((DEPTH, 5, 6144), np.float32)
    for core in cores:
        layer, half = core // 2, core % 2
        o = np.asarray(res.results[core]["out"])
        mods_all[layer][:, half * 3072:(half + 1) * 3072] = o.transpose(2, 1, 0).reshape(5, 3072)
    XT = [np.ascontiguousarray(np.concatenate([ctx[b], x[b]], 0).T) for b in range(NB)]
    cosT, sinT = const_rope_tables()
    consts = dict(masks=const_masks(), ident=np.eye(128, dtype=np.float32), ropeR=const_ropeR(), cosT=cosT, sinT=sinT,
                  fnet=const_fnet())
    sel = const_sel()
    rbias = np.ascontiguousarray(np.broadcast_to(f32(router_b)[None, :], (128, 16)))
    for layer in range(DEPTH):
        i = layer // 2
        mods = [pack_mods(mods_all[layer][4], mods_all[layer][b]) for b in range(NB)]
        ims = []
        for core in cores:
            b, g = core // 2, core % 2
            if layer % 2 == 0:
                ims.append(even_core_inputs(g, XT[b], mods[b], f32(ev_w_in[i]), f32(gla_w_decay[i]), f32(gla_b_decay[i]),
                                            f32(gla_norm_g[i]), f32(gqa_q_norm_g[i]), f32(gqa_k_norm_g[i]), consts))
            else:
                ims.append(odd_core_inputs(g, XT[b], mods[b], f32(od_w_in[i]), f32(ssd_conv_w[i]), f32(ssd_conv_b[i]),
                                           f32(ssd_dt_bias[i]), f32(ssd_a_log[i]), f32(ssd_d[i]), f32(ssd_norm_g[i]),
                                           f32(fnet_norm_g[i]), consts))
        if layer % 2 == 0:
            res = run_bass_kernel_spmd(_prog("B_even", build_stageB_even), ims, core_ids=cores)
        else:
            res = run_bass_kernel_spmd(_prog("B_odd", build_stageB_odd), ims, core_ids=cores)
        CAT = []
        for b in range(NB):
            cat = np.empty((1024, SE), np.float32)
            for g in range(2):
                cg = np.asarray(res.results[2 * b + g]["cat"])
                if layer % 2 == 0:
                    cat[g * 256:(g + 1) * 256] = cg[0:256]
                    cat[512 + g * 256:512 + (g + 1) * 256] = cg[256:512]
                else:
                    cat[g * 384:(g + 1) * 384] = cg[0:384]
                    cat[768 + g * 128:768 + (g + 1) * 128] = cg[384:512]
            CAT.append(cat)
        w_o = f32(ev_w_o[i]) if layer % 2 == 0 else f32(od_w_o[i])
        lnp = np.ascontiguousarray(np.stack([pack_vec(f32(ln_g[layer])), pack_vec(f32(ln_b[layer]))], 1))
        wg, wu, wd = f32(exp_w_gate[layer]), f32(exp_w_up[layer]), f32(exp_w_down[layer])
        ims = []
        for core in cores:
            b, h = core // 2, core % 2
            colsel = np.concatenate([np.arange(h * 128, (h + 1) * 128), 256 + np.arange(h * 4096, (h + 1) * 4096)])
            ims.append(dict(catT=np.ascontiguousarray(CAT[b][:, colsel]), xT=np.ascontiguousarray(XT[b][:, colsel]), w_o=w_o,
                            mods=mods[b], lnp=lnp, rw=f32(router_w), rbias=rbias, sel=sel, ident=consts["ident"],
                            wg=wg, wu=wu, wd=wd))
        res = run_bass_kernel_spmd(_prog("C", build_stageC), ims, core_ids=cores)
        for core in cores:
            b, h = core // 2, core % 2
            colsel = np.concatenate([np.arange(h * 128, (h + 1) * 128), 256 + np.arange(h * 4096, (h + 1) * 4096)])
            XT[b][:, colsel] = np.asarray(res.results[core]["xo"])
    return np.ascontiguousarray(np.stack([XT[b][:, 256:].T for b in range(NB)], 0)).astype(np.float32)


EVEN_CHUNK_ROWS = [0, 128, 512, 640, 256, 384, 768, 896]
ODD_CHUNK_ROWS = [0, 128, 256, 512, 640, 768, 384, 896]
PAIRS = [[0, 1], [2, 3], [4, 5], [6, 7]]


def _gsegs(lo, hi):
    out = []
    for (a, b, j, t0) in ((0, 128, 0, 0), (128, 256, 1, 0), (256, 4352, 0, 128), (4352, 8448, 1, 128)):
        s, e = max(lo, a), min(hi, b)
        if s < e:
            out.append((s, e, j, t0 + s - a))
    return out


def _csegs(bi, j):
    if bi == 0:
        return [(0, 128, j * 128), (128, TB, 256 + j * 4096)]
    return [(0, TB, 256 + j * 4096 + bi * TB - 128)]


def emit_adaln(kb, nc, cin2, ada_w, ada_b48, mods_bufs, P):
    with ExitStack() as st:
        c_sb = kb.sb_in(st, "ad_c", [128, 8, 2], F32)
        b_sb = kb.sb_in(st, "ad_b", [128, 4, 48], F32)
        wt = [kb.sb_in(st, "ad_w%d" % i, [128, 8, 768], F32) for i in range(2)]
        kb.dma("sp", c_sb.t[:], cin2, writes=[c_sb])
        for L in range(4):
            kb.dma("sp", b_sb.t[:, L, :], ada_b48[L], writes=[b_sb])
        kb.op("act", lambda e: e.activation(out=c_sb.t[:], in_=c_sb.t[:], func=AF.Silu), reads=[c_sb], writes=[c_sb])
        n = 0
        for L in range(len(mods_bufs)):
            wv = ada_w[L].rearrange("(c p) n -> p c n", p=128)
            for piece in range(8):
                w_ = wt[n % 2]
                n += 1
                kb.dma("sp", w_.t[:], wv[:, :, piece * 768:(piece + 1) * 768], writes=[w_])
                for jj in range(6):
                    j = piece * 6 + jj
                    m, c = divmod(j, 8)
                    pj = P[jj % 2]
                    for k in range(8):
                        kb.op("pe", lambda e, k=k: e.matmul(pj.t[:, 0:2], w_.t[:, k, jj * 128:(jj + 1) * 128], c_sb.t[:, k, :],
                                                            start=(k == 0), stop=(k == 7)), reads=[w_, c_sb], writes=[pj], sig=(k == 7))
                    kb.op("dve", lambda e: e.tensor_scalar(out=mods_bufs[L].t[:, :, m, c], in0=pj.t[:, 0:2], scalar1=b_sb.t[:, L, j:j + 1],
                                                           scalar2=None, op0=ALU.add), reads=[pj, b_sb], writes=[mods_bufs[L]])
        kb.barrier()


def build_fused(depth=4):
    nc = bass.Bass("TRN2", target_bir_lowering=False)
    I = {}

    def din(name, shape):
        I[name] = _din(nc, name, shape)
        return I[name]

    cin2 = din("cin2", [128, 8, 2])
    ada_w = din("ada_w", [4, 1024, 6144])
    ada_b48 = din("ada_b48", [4, 128, 48])
    XTg0 = din("XTg0", [2048, 4224])
    xown0 = din("xown0", [1024, 4224])
    msel_in = din("msel", [128, 2])
    masks = din("masks", [128, 2, 128])
    ident = din("ident", [128, 128])
    ropeR = din("ropeR", [64, 64])
    cosT = din("cosT", [64, 8192])
    sinT = din("sinT", [64, 8192])
    fconst = dict(bd=din("fc_bd", [128, 128]), ccsc=din("fc_ccsc", [128, 256]), f64=din("fc_f64", [64, 2, 128]),
                  tw=din("fc_tw", [128, 2, 64]), f128=din("fc_f128", [128, 2, 128]), c256=din("fc_c256", [128, 2, 2, 256]))
    rw = din("rw", [1024, 16]).rearrange("(c p) n -> p c n", p=128)
    rbias = din("rbias", [128, 16])
    sel_in = din("sel", [16, 16 * 128])
    LW = []
    for L in range(depth):
        d = {}
        if L % 2 == 0:
            d["w_in"] = din("w_in%d" % L, [1024, FM_E + TM_E]).rearrange("(c p) n -> p c n", p=128)
            d["wdec"] = din("wdec%d" % L, [16, 2, 128])
            d["bdec"] = din("bdec%d" % L, [64, 2, 2])
            d["glag"] = din("glag%d" % L, [128, 1])
            d["qkg"] = din("qkg%d" % L, [64, 2])
        else:
            d["w_in"] = din("w_in%d" % L, [1024, FM_O]).rearrange("(c p) n -> p c n", p=128)
            for nme, shp in (("convw", [128, 5, 5]), ("convb", [128, 5]), ("dtb", [128, 12]), ("alog", [128, 12]), ("dsk", [128, 3]),
                             ("ng", [128, 3]), ("fg", [128, 1])):
                d[nme] = din("%s%d" % (nme, L), shp)
        d["w_o"] = din("w_o%d" % L, [1024, 1024]).rearrange("(c p) n -> p c n", p=128)
        d["lnp"] = din("lnp%d" % L, [128, 2, 2, 8])
        d["wg"] = din("wg%d" % L, [16, 1024, 768])
        d["wu"] = din("wu%d" % L, [16, 1024, 768])
        d["wd"] = din("wd%d" % L, [16, 768, 1024])
        LW.append(d)
    xout = _dout(nc, "xout", [1024, TC])
    dint = lambda name, shape, dt=F32: nc.dram_tensor(name, list(shape), dt, kind="Internal").ap()
    pT = dint("pT", [FM_O, SE])
    vtm = dint("vtm", [SE, TM_E], BF16)
    ofs, obs = dint("ofs", [256, SE]), dint("obs", [256, SE])
    cT = dint("cT", [640, SE])
    yfs, ybs = dint("yfs", [384, SE]), dint("ybs", [384, SE])
    xmid = dint("xmid", [1024, TC]).rearrange("(c p) t -> p c t", p=128)
    catp = dint("catp", [512, SE])
    catg = dint("catg", [1024, SE])
    xo_buf = [dint("xoA", [1024, TC]), dint("xoB", [1024, TC])]
    XTg = dint("XTg", [2048, TC])
    wscr = dint("wscr", [16, 3, 128, 8 * 768], BF16)
    v3 = lambda ap: ap.rearrange("(c p) t -> p c t", p=128)
    with ExitStack() as st:
        kb = KB(nc, st)
        P = [kb.ps("P%d" % i, [128, 512]) for i in range(7)]
        mods_bufs = [kb.sb("mods%d" % L, [128, 2, 6, 8], F32) for L in range(depth)]
        msel = kb.sb("msel", [128, 2], F32)
        kb.dma("sp", msel.t[:], msel_in, writes=[msel])
        kb.pfx = "ad_"
        emit_adaln(kb, nc, cin2, ada_w, ada_b48, mods_bufs, P)
        catg_tok = Buf(None, "catg_tok")
        xtg_tok = Buf(None, "xtg_tok")
        for L in range(depth):
            W = LW[L]
            xsrc = XTg0 if L == 0 else XTg
            xv = xsrc.rearrange("(c ph s i) t -> ph s i c t", ph=2, s=2, i=64)

            def load_x(bi, xi, xv=xv):
                for (s, e, j, t0) in _gsegs(bi * TB, (bi + 1) * TB):
                    for ph in range(2):
                        kb.dma("sp", xi.t[ph * 64:(ph + 1) * 64, :, s - bi * TB:e - bi * TB], xv[ph, j][:, :, t0:t0 + (e - s)],
                               reads=[xtg_tok], writes=[xi])

            kb.pfx = "L%dB_" % L
            if L % 2 == 0:
                emit_inproj(kb, nc, None, W["w_in"], mods_bufs[L], 0, pT, vtm, FM_E, TM_E, P, load_x=load_x)
                emit_gla(kb, nc, pT, vtm, W["wdec"], W["bdec"], W["glag"], masks, ident, ofs, obs, catp, P)
                emit_gqa(kb, nc, pT, vtm, W["qkg"], ropeR, cosT, sinT, catp, P)
                rows = EVEN_CHUNK_ROWS
            else:
                emit_inproj(kb, nc, None, W["w_in"], mods_bufs[L], 0, pT, None, FM_O, 0, P, load_x=load_x)
                emit_conv(kb, nc, pT, cT, W["convw"], W["convb"], P)
                emit_ssd(kb, nc, pT, cT, W["dtb"], W["alog"], W["dsk"], W["ng"], masks, ident, yfs, ybs, catp, P)
                emit_fnet(kb, nc, pT, W["fg"], fconst, catp, P)
                rows = ODD_CHUNK_ROWS
            for k in range(16):
                kb.collective("AllGather", catp[k * 32:(k + 1) * 32, :], catg[k * 64:(k + 1) * 64, :], PAIRS, writes=[catg_tok])

            def load_cat(bi, catb, r, vf, rows=rows):
                for j, dst in ((0, r), (1, vf)):
                    for c in range(8):
                        srank, rho0 = rows[c] // 512, rows[c] % 512
                        for (lo, hi, gc) in _csegs(bi, j):
                            for q in range(4):
                                g0 = (rho0 // 32 + q) * 64 + srank * 32
                                kb.dma("sp", dst.t[q * 32:(q + 1) * 32, c, lo:hi], catg[g0:g0 + 32, gc:gc + (hi - lo)],
                                       reads=[catg_tok], writes=[dst])
                kb.op("dve", lambda e: e.tensor_scalar(out=r.t[:], in0=r.t[:], scalar1=msel.t[:, 0:1], scalar2=None, op0=ALU.mult),
                      reads=[r, msel], writes=[r])
                kb.op("dve", lambda e: e.scalar_tensor_tensor(out=catb.t[:], in0=vf.t[:], scalar=msel.t[:, 1:2], in1=r.t[:],
                                                              op0=ALU.mult, op1=ALU.add), reads=[vf, msel, r], writes=[catb])

            last = L == depth - 1
            xo_ap = xout if last else xo_buf[L % 2]
            xin_ap = xown0 if L == 0 else xo_buf[(L - 1) % 2]
            D = dict(xT=v3(xin_ap), xo=v3(xo_ap), w_o=W["w_o"], lnp=W["lnp"], rw=rw, rbias=rbias, sel=sel_in, ident=ident,
                     wg=W["wg"], wu=W["wu"], wd=W["wd"], xmid=xmid, wscr=wscr)
            kb.pfx = "L%dC_" % L
            with ExitStack() as stc:
                p7 = Buf(stc.enter_context(nc.psum_tensor("ps_L%dC_P7" % L, [128, 512], F32)), "P7")
                xo_tok = Buf(None, "xo_tok")
                emit_stageC(kb, nc, P + [p7], D, mods_buf=mods_bufs[L], load_cat=load_cat, xo_tok=xo_tok)
            if not last:
                for k in range(16):
                    kb.collective("AllGather", xo_ap[k * 64:(k + 1) * 64, :], XTg[k * 128:(k + 1) * 128, :], PAIRS,
                                  reads=[xo_tok], writes=[xtg_tok])
        kb.finish()
        print("fused instructions:", kb.n_inst, dict(kb.cnt))
    return nc


def kernel(x, c, ctx, c_ctx, ada_w, ada_b, ln_g, ln_b, ev_w_in, ev_w_o, gla_w_decay, gla_b_decay, gla_norm_g, gqa_q_norm_g,
                 gqa_k_norm_g, od_w_in, od_w_o, ssd_conv_w, ssd_conv_b, ssd_dt_bias, ssd_a_log, ssd_d, ssd_norm_g, fnet_norm_g,
                 router_w, router_b, exp_w_gate, exp_w_up, exp_w_down, _depth=4):
    f32 = lambda a: np.ascontiguousarray(np.asarray(a), np.float32)
    x, c, ctx, c_ctx = f32(x), f32(c), f32(ctx), f32(c_ctx)
    NB, DEPTH = 4, _depth
    cores = list(range(8))
    cosT, sinT = const_rope_tables()
    shared = dict(ada_w=f32(ada_w), ada_b48=np.ascontiguousarray(f32(ada_b).reshape(4, 48, 128).transpose(0, 2, 1)),
                  masks=const_masks(), ident=np.eye(128, dtype=np.float32), ropeR=const_ropeR(), cosT=cosT, sinT=sinT,
                  rw=f32(router_w), rbias=np.ascontiguousarray(np.broadcast_to(f32(router_b)[None, :], (128, 16))), sel=const_sel())
    shared.update(const_fnet())
    for L in range(DEPTH):
        i = L // 2
        shared["w_o%d" % L] = f32(ev_w_o[i]) if L % 2 == 0 else f32(od_w_o[i])
        shared["lnp%d" % L] = np.ascontiguousarray(np.stack([pack_vec(f32(ln_g[L])), pack_vec(f32(ln_b[L]))], 1))
        shared["wg%d" % L], shared["wu%d" % L], shared["wd%d" % L] = f32(exp_w_gate[L]), f32(exp_w_up[L]), f32(exp_w_down[L])
    dummy_c = dict(masks=None, ident=None, ropeR=None, cosT=None, sinT=None, fnet={})
    ims = []
    for core in cores:
        b, r = core // 2, core % 2
        im = dict(shared)
        cc = np.stack([c_ctx, c[b]], 0)
        im["cin2"] = np.ascontiguousarray(cc.reshape(2, 8, 128).transpose(2, 1, 0))
        halves = [np.concatenate([ctx[b][j * 128:(j + 1) * 128], x[b][j * 4096:(j + 1) * 4096]], 0).T for j in range(2)]
        im["XTg0"] = np.ascontiguousarray(np.stack([h_.reshape(16, 64, TC) for h_ in halves], 1).reshape(2048, TC))
        im["xown0"] = np.ascontiguousarray(halves[r])
        im["msel"] = np.ascontiguousarray(np.broadcast_to(np.array([1.0 - r, float(r)], np.float32)[None, :], (128, 2)))
        for L in range(DEPTH):
            i = L // 2
            if L % 2 == 0:
                e = even_core_inputs(r, None, None, f32(ev_w_in[i]), f32(gla_w_decay[i]), f32(gla_b_decay[i]), f32(gla_norm_g[i]),
                                     f32(gqa_q_norm_g[i]), f32(gqa_k_norm_g[i]), dummy_c)
                for k in ("w_in", "wdec", "bdec", "glag", "qkg"):
                    im["%s%d" % (k, L)] = e[k]
            else:
                o = odd_core_inputs(r, None, None, f32(od_w_in[i]), f32(ssd_conv_w[i]), f32(ssd_conv_b[i]), f32(ssd_dt_bias[i]),
                                    f32(ssd_a_log[i]), f32(ssd_d[i]), f32(ssd_norm_g[i]), f32(fnet_norm_g[i]), dummy_c)
                for k in ("w_in", "convw", "convb", "dtb", "alog", "dsk", "ng", "fg"):
                    im["%s%d" % (k, L)] = o[k]
        ims.append(im)
    res = run_bass_kernel_spmd(_prog("fused%d" % DEPTH, lambda: build_fused(DEPTH)), ims, core_ids=cores)
    out = np.empty((NB, 8192, 1024), np.float32)
    for core in cores:
        b, r = core // 2, core % 2
        out[b, r * 4096:(r + 1) * 4096] = np.asarray(res.results[core]["xout"])[:, 128:].T
    return out
```
